# Optimizing a Trainium2 kernel written in Bass

```python
import jax
import jax.numpy as jnp
from jax import lax
import numpy as np


D_MODEL = 2048
BATCH = 4
SEQ = 4096
DEPTH = 4

PLE_DIM = 256
CONV_CH = 1024
CONV_K = 3
M_HEADS = 8
M_HD = 128
M_W = M_HEADS * M_HD
M_CHUNK = 64
A_QH = 16
A_KVH = 4
A_GROUP = A_QH // A_KVH
A_HD = 64
A_QW = A_QH * A_HD
A_KVW = A_KVH * A_HD
WINDOW = 128
A_BLOCK = 128
ROPE_THETA = 10000.0
N_BRANCH = 3
EPS = 1e-6

SPLIT_SIZES = (
    CONV_CH, CONV_CH, CONV_CH, CONV_CH,
    M_W, M_W, M_W, M_W, M_HEADS, M_HEADS, M_W,
    A_QW, A_KVW, A_KVW, A_QW,
    N_BRANCH * D_MODEL,
)
SPLIT_POINTS = [sum(SPLIT_SIZES[:i + 1]) for i in range(len(SPLIT_SIZES) - 1)]
IN_W = sum(SPLIT_SIZES)

kernel_name = 'hybrid_conv_mlstm_swa_gated_trunk'


def rmsnorm(x, g):
    xf = x.astype(jnp.float32)
    var = jnp.mean(xf * xf, axis=-1, keepdims=True)
    return (xf * lax.rsqrt(var + EPS) * g.astype(jnp.float32)).astype(x.dtype)


def rope(x, pos):
    hd = x.shape[-1]
    half = hd // 2
    inv = jnp.power(ROPE_THETA, -2.0 * jnp.arange(half, dtype=jnp.float32) / hd)
    ang = pos.astype(jnp.float32)[..., None] * inv
    cos = jnp.cos(ang)[:, :, None, :]
    sin = jnp.sin(ang)[:, :, None, :]
    xf = x.astype(jnp.float32)
    x1, x2 = xf[..., :half], xf[..., half:]
    return jnp.concatenate([x1 * cos - x2 * sin, x2 * cos + x1 * sin], axis=-1).astype(x.dtype)


def short_conv_mixer(b, c, u, w):
    v = c * u
    S = v.shape[1]
    vp = jnp.pad(v, ((0, 0), (CONV_K - 1, 0), (0, 0)))
    y = w[0] * vp[:, 0:S]
    for j in range(1, CONV_K):
        y = y + w[j] * vp[:, j:j + S]
    return b * y


def mlstm_chunkwise(q, k, v, i_pre, f_pre):
    Bsz, S = q.shape[0], q.shape[1]
    L = M_CHUNK
    nc = S // L
    f32 = jnp.float32

    def chunks(t):
        t = t.astype(f32).reshape((Bsz, nc, L) + t.shape[2:])
        return jnp.moveaxis(t, 3, 1)

    qc = chunks(q) * (M_HD ** -0.5)
    kc = chunks(k)
    vc = chunks(v)
    log_i = chunks(i_pre)
    log_f = jax.nn.log_sigmoid(chunks(f_pre))
    bcum = jnp.cumsum(log_f, axis=-1)
    b_last = bcum[..., -1]

    g = b_last[..., None] - bcum + log_i
    g_max = jnp.max(g, axis=-1)
    w_end = jnp.exp(g - g_max[..., None])
    C_loc = jnp.einsum('bhcl,bhcld,bhcle->bhcde', w_end, vc, kc)
    n_loc = jnp.einsum('bhcl,bhcle->bhce', w_end, kc)

    def step(carry, xs):
        C, n, m = carry
        Cl, nl, gm, bl = xs
        m_new = jnp.maximum(bl + m, gm)
        a = jnp.exp(bl + m - m_new)
        c = jnp.exp(gm - m_new)
        C_new = a[..., None, None] * C + c[..., None, None] * Cl
        n_new = a[..., None] * n + c[..., None] * nl
        return (C_new, n_new, m_new), (C, n, m)

    init = (jnp.zeros((Bsz, M_HEADS, M_HD, M_HD), f32),
            jnp.zeros((Bsz, M_HEADS, M_HD), f32),
            jnp.zeros((Bsz, M_HEADS), f32))
    xs = (jnp.moveaxis(C_loc, 2, 0), jnp.moveaxis(n_loc, 2, 0),
          jnp.moveaxis(g_max, 2, 0), jnp.moveaxis(b_last, 2, 0))
    _, (C_prev, n_prev, m_prev) = lax.scan(step, init, xs)
    C_prev = jnp.moveaxis(C_prev, 0, 2)
    n_prev = jnp.moveaxis(n_prev, 0, 2)
    m_prev = jnp.moveaxis(m_prev, 0, 2)

    causal = jnp.tril(jnp.ones((L, L), dtype=bool))
    D = bcum[..., :, None] - bcum[..., None, :] + log_i[..., None, :]
    D = jnp.where(causal, D, -jnp.inf)
    inter = bcum + m_prev[..., None]
    m = jnp.maximum(inter, jnp.max(D, axis=-1))
    Sw = jnp.einsum('bhcld,bhcsd->bhcls', qc, kc) * jnp.exp(D - m[..., None])
    w_inter = jnp.exp(inter - m)
    num = (jnp.einsum('bhcls,bhcsd->bhcld', Sw, vc)
           + w_inter[..., None] * jnp.einsum('bhcle,bhcde->bhcld', qc, C_prev))
    den = jnp.sum(Sw, axis=-1) + w_inter * jnp.einsum('bhcle,bhce->bhcl', qc, n_prev)
    h = num / jnp.maximum(jnp.abs(den), jnp.exp(-m))[..., None]
    h = jnp.moveaxis(h, 1, 3).reshape(Bsz, S, M_W)
    return h.astype(q.dtype)


def sliding_window_attention(q, k, v, sinks):
    Bsz, S = q.shape[0], q.shape[1]
    nb = S // A_BLOCK
    f32 = jnp.float32
    qb = q.astype(f32).reshape(Bsz, nb, A_BLOCK, A_KVH, A_GROUP, A_HD)

    def banded(t):
        tb = t.astype(f32).reshape(Bsz, nb, A_BLOCK, A_KVH, A_HD)
        prev = jnp.pad(tb, ((0, 0), (1, 0), (0, 0), (0, 0), (0, 0)))[:, :-1]
        return jnp.concatenate([prev, tb], axis=2)

    kb = banded(k)
    vb = banded(v)
    s = jnp.einsum('bnqhgd,bnkhd->bnhgqk', qb, kb) * (A_HD ** -0.5)
    qi = jnp.arange(A_BLOCK)[:, None]
    ki = jnp.arange(2 * A_BLOCK)[None, :]
    rel = qi + A_BLOCK - ki
    blk = jnp.arange(nb)[:, None, None]
    valid = (rel >= 0) & (rel < WINDOW) & (blk * A_BLOCK + ki - A_BLOCK >= 0)
    s = jnp.where(valid[None, :, None, None], s, -jnp.inf)
    sink = sinks.astype(f32).reshape(A_KVH, A_GROUP)[None, None, :, :, None, None]
    sink = jnp.broadcast_to(sink, s.shape[:-1] + (1,))
    probs = jax.nn.softmax(jnp.concatenate([s, sink], axis=-1), axis=-1)[..., :-1]
    o = jnp.einsum('bnhgqk,bnkhd->bnqhgd', probs, vb)
    return o.reshape(Bsz, S, A_QW).astype(q.dtype)


def hybrid_layer(x, p_i, positions, w_in, conv_w, b_ig, b_fg, m_norm, sinks,
                 w_up_c, w_up_m, w_up_a, w_out, pre_g, post_g, w_ple, w_ple_gate, ple_g):
    Bsz, S, _ = x.shape
    h = rmsnorm(x, pre_g)
    z = h @ w_in
    (cb, cc, cx, cg, mq, mk, mv, mo, mi, mf, mg,
     aq, ak, av, ag, gates) = jnp.split(z, SPLIT_POINTS, axis=-1)

    y_c = short_conv_mixer(cb, cc, cx, conv_w) * jax.nn.silu(cg)

    hm = mlstm_chunkwise(mq.reshape(Bsz, S, M_HEADS, M_HD), mk.reshape(Bsz, S, M_HEADS, M_HD),
                         mv.reshape(Bsz, S, M_HEADS, M_HD), mi + b_ig, mf + b_fg)
    hm = rmsnorm(hm.reshape(Bsz, S, M_HEADS, M_HD), m_norm.reshape(M_HEADS, M_HD)).reshape(Bsz, S, M_W)
    y_m = hm * jax.nn.sigmoid(mo) * jax.nn.silu(mg)

    qa = rope(aq.reshape(Bsz, S, A_QH, A_HD), positions)
    ka = rope(ak.reshape(Bsz, S, A_KVH, A_HD), positions)
    va = av.reshape(Bsz, S, A_KVH, A_HD)
    y_a = sliding_window_attention(qa, ka, va, sinks) * jax.nn.silu(ag)

    gt = jax.nn.sigmoid(gates).reshape(Bsz, S, N_BRANCH, D_MODEL)
    merged = (gt[:, :, 0] * (y_c @ w_up_c) + gt[:, :, 1] * (y_m @ w_up_m)
              + gt[:, :, 2] * (y_a @ w_up_a))
    x = x + rmsnorm(merged @ w_out, post_g)

    e = p_i @ w_ple
    x = x + rmsnorm(jax.nn.sigmoid(x @ w_ple_gate) * e, ple_g)
    return x


def setup_inputs(seed: int = 0) -> dict:
    key = jax.random.key(seed)
    ks = jax.random.split(key, 20)
    f32 = jnp.float32

    def nrm(k, shape, scale):
        return jax.random.normal(k, shape, f32) * scale

    x = nrm(ks[0], (BATCH, SEQ, D_MODEL), 1.0)
    p = nrm(ks[1], (DEPTH, BATCH, SEQ, PLE_DIM), 1.0)
    offs = jax.random.randint(ks[2], (BATCH, 1), 0, 1024, dtype=jnp.int32)
    positions = offs + jnp.arange(SEQ, dtype=jnp.int32)[None, :]
    w_in = nrm(ks[3], (DEPTH, D_MODEL, IN_W), D_MODEL ** -0.5)
    conv_w = nrm(ks[4], (DEPTH, CONV_K, CONV_CH), CONV_K ** -0.5)
    b_igate = nrm(ks[5], (DEPTH, M_HEADS), 0.1)
    b_fgate = jnp.linspace(3.0, 6.0, M_HEADS, dtype=f32)[None, :] + nrm(ks[6], (DEPTH, M_HEADS), 0.01)
    mlstm_norm = 1.0 + nrm(ks[7], (DEPTH, M_W), 0.02)
    attn_sinks = nrm(ks[8], (DEPTH, A_QH), 0.5)
    w_up_conv = nrm(ks[9], (DEPTH, CONV_CH, D_MODEL), CONV_CH ** -0.5)
    w_up_mlstm = nrm(ks[10], (DEPTH, M_W, D_MODEL), M_W ** -0.5)
    w_up_attn = nrm(ks[11], (DEPTH, A_QW, D_MODEL), A_QW ** -0.5)
    w_out = nrm(ks[12], (DEPTH, D_MODEL, D_MODEL), D_MODEL ** -0.5)
    pre_norm = 1.0 + nrm(ks[13], (DEPTH, D_MODEL), 0.02)
    post_norm = 1.0 + nrm(ks[14], (DEPTH, D_MODEL), 0.02)
    w_ple = nrm(ks[15], (DEPTH, PLE_DIM, D_MODEL), PLE_DIM ** -0.5)
    w_ple_gate = nrm(ks[16], (DEPTH, D_MODEL, D_MODEL), D_MODEL ** -0.5)
    ple_norm = 1.0 + nrm(ks[17], (DEPTH, D_MODEL), 0.02)
    return {'x': x, 'p': p, 'positions': positions, 'w_in': w_in, 'conv_w': conv_w,
            'b_igate': b_igate, 'b_fgate': b_fgate, 'mlstm_norm': mlstm_norm,
            'attn_sinks': attn_sinks, 'w_up_conv': w_up_conv, 'w_up_mlstm': w_up_mlstm,
            'w_up_attn': w_up_attn, 'w_out': w_out, 'pre_norm': pre_norm,
            'post_norm': post_norm, 'w_ple': w_ple, 'w_ple_gate': w_ple_gate,
            'ple_norm': ple_norm}


def reference(x, p, positions, w_in, conv_w, b_igate, b_fgate, mlstm_norm, attn_sinks,
              w_up_conv, w_up_mlstm, w_up_attn, w_out, pre_norm, post_norm, w_ple,
              w_ple_gate, ple_norm):
    for i in range(DEPTH):
        x = hybrid_layer(x, p[i], positions, w_in[i], conv_w[i], b_igate[i], b_fgate[i],
                         mlstm_norm[i], attn_sinks[i], w_up_conv[i], w_up_mlstm[i],
                         w_up_attn[i], w_out[i], pre_norm[i], post_norm[i], w_ple[i],
                         w_ple_gate[i], ple_norm[i])
    return x
```

```python
import numpy as np
from contextlib import ExitStack
import concourse.bass as bass
import concourse.mybir as mybir
from concourse.bass_utils import run_bass_kernel_spmd

F32 = mybir.dt.float32
BF16 = mybir.dt.bfloat16
I32 = mybir.dt.int32
AF = mybir.ActivationFunctionType
ALU = mybir.AluOpType
AX = mybir.AxisListType

D = 2048
KC = 16
IN_W = 17936
EPS = 1e-6
EPOCH = 30000
WB_ELEMS = 4096
NWB = 5
LIVE = 2

O_CB, O_CC, O_CX, O_CG = 0, 1024, 2048, 3072
O_MQ, O_MK, O_MV, O_MO, O_MI, O_MF, O_MG = 4096, 5120, 6144, 7168, 8192, 8200, 8208
O_AQ, O_AK, O_AV, O_AG = 9232, 10256, 10512, 10768
O_GT = 11792


class Buf:
    __slots__ = ("t", "w", "r", "sem", "cnt")

    def __init__(self, t=None, sem=None):
        self.t = t
        self.w = None
        self.r = {}
        self.sem = sem
        self.cnt = 0

    def __getitem__(self, k):
        return self.t[k]


class KB:
    def __init__(self, nc, es):
        self.nc = nc
        self.es = es
        self.eng = {"pe": nc.tensor, "act": nc.scalar, "dve": nc.vector,
                    "pool": nc.gpsimd, "sp": nc.sync}
        self.cnt = {e: 0 for e in self.eng}
        self.sems = {e: [] for e in self.eng}
        self.waited = {e: {} for e in self.eng}
        self.nsem = 0

    def newsem(self, name):
        self.nsem += 1
        return self.es.enter_context(self.nc.semaphore(name))

    def sb(self, name, shape, dt, dma=False):
        t = self.es.enter_context(self.nc.sbuf_tensor("sb_" + name, shape, dt))
        return Buf(t, self.newsem("s_" + name) if dma else None)

    def ps(self, name, shape, dt=F32):
        t = self.es.enter_context(self.nc.psum_tensor(name, shape, dt))
        return Buf(t)

    def _mark(self, e):
        c = self.cnt[e]
        ep = c // EPOCH
        while len(self.sems[e]) <= ep:
            self.sems[e].append(self.newsem("c_%s_%d" % (e, len(self.sems[e]))))
        self.cnt[e] = c + 1
        return (self.sems[e][ep], c - ep * EPOCH + 1)

    def _wait(self, e, dep):
        if dep is None:
            return
        sem, val = dep
        if e == "pe" and any(sem is s for s in self.sems["pe"]):
            return
        k = id(sem)
        if self.waited[e].get(k, 0) >= val:
            return
        self.waited[e][k] = val
        self.eng[e].wait_ge(sem, val)

    def op(self, e, fn, reads=(), writes=()):
        for b in reads:
            self._wait(e, b.w)
        for b in writes:
            self._wait(e, b.w)
            for d in list(b.r.values()):
                self._wait(e, d)
        ins = fn()
        m = self._mark(e)
        ins.then_inc(m[0], 1)
        for b in reads:
            b.r[id(m[0])] = m
        for b in writes:
            b.w = m
            b.r = {}
        return ins

    def dma(self, q, out_ap, in_ap, semb, reads=(), writes=()):
        for b in reads:
            self._wait(q, b.w)
        for b in writes:
            self._wait(q, b.w)
            for d in list(b.r.values()):
                self._wait(q, d)
        ins = self.eng[q].dma_start(out=out_ap, in_=in_ap)
        semb.cnt += 16
        ins.then_inc(semb.sem, 16)
        m = (semb.sem, semb.cnt)
        for b in reads:
            b.r[id(semb.sem)] = m
        for b in writes:
            b.w = m
            b.r = {}

    def wait_all(self, q, bufs):
        for b in bufs:
            self._wait(q, b.w)


class _Stop(Exception):
    pass


def build(NTOK, DEPTH, T=256, debug=None, stage=None):
    nc = bass.Bass("TRN2", target_bir_lowering=False)
    NT = NTOK // T
    NCH = T // 64
    NQB = T // 128
    dbg = {}

    def din(name, shape, dt=F32):
        return nc.dram_tensor(name, shape, dt, kind="ExternalInput").ap()

    x_in = din("x", [NTOK, D])
    p_in = din("p", [DEPTH, NTOK, 256])
    pos_in = din("pos", [NTOK], I32)
    w_in = din("w_in", [DEPTH, D, IN_W])
    w_upc = din("w_up_conv", [DEPTH, 1024, D])
    w_upm = din("w_up_mlstm", [DEPTH, 1024, D])
    w_upa = din("w_up_attn", [DEPTH, 1024, D])
    w_out = din("w_out", [DEPTH, D, D])
    w_ple = din("w_ple", [DEPTH, 256, D])
    w_pg = din("w_ple_gate", [DEPTH, D, D])
    convw_in = din("convw", [DEPTH, 128, 24])
    gains_in = din("gains", [DEPTH, 128, 48])
    big_in = din("b_igate", [DEPTH, 8])
    bfg_in = din("b_fgate", [DEPTH, 8])
    mnorm_in = din("mlstm_norm", [DEPTH, 1024])
    sinks_in = din("sinks", [DEPTH, 128, 8])
    c_ident = din("c_ident", [128, 128])
    c_rm = din("c_rm", [128, 128])
    c_sel = din("c_sel", [128, 256])
    c_vec = din("c_vec", [128, 4])
    c_causal = din("c_causal", [64, 64])
    c_amask = din("c_amask", [128, 256])
    c_onespad = din("c_onespad", [128, 256])
    y_out = nc.dram_tensor("y", [NTOK, D], F32, kind="ExternalOutput").ap()
    xT_d = nc.dram_tensor("xT_scr", [KC, 128, NTOK], F32, kind="Internal").ap()
    rope_d = nc.dram_tensor("rope_scr", [4, 128, NTOK], F32, kind="Internal").ap()
    if debug:
        for nm, shp in debug.items():
            dbg[nm] = nc.dram_tensor("dbg_" + nm, shp, F32, kind="ExternalOutput").ap()

    es = ExitStack()
    with es:
        k = KB(nc, es)
        E = {e: k.eng[e] for e in k.eng}
        xT_reg = [Buf() for _ in range(NT)]
        rope_reg = Buf()

        ident = k.sb("ident", [128, 128], F32, dma=True)
        identb = k.sb("identb", [128, 128], BF16)
        rm32 = k.sb("rm32", [128, 128], F32, dma=True)
        selb = k.sb("selb", [128, 256], BF16)
        sel32 = k.sb("sel32", [128, 256], F32, dma=True)
        cvec = k.sb("cvec", [128, 4], F32, dma=True)
        causal = k.sb("causal", [64, 64], F32, dma=True)
        amask = k.sb("amask", [128, 256], F32, dma=True)
        amask0 = k.sb("amask0", [128, 256], F32)
        onespad32 = k.sb("onespad32", [128, 256], F32, dma=True)
        onespad = k.sb("onespad", [128, 256], BF16)
        ones32 = k.sb("ones32", [128, 128], F32)
        onesb = k.sb("onesb", [128, 8], BF16)
        onesbf = k.sb("onesbf", [128, 128], BF16)
        sqb = [k.sb("sqb%d" % i, [128, T], BF16) for i in range(2)]
        for b, src in ((ident, c_ident), (rm32, c_rm), (sel32, c_sel), (cvec, c_vec),
                       (causal, c_causal), (amask, c_amask), (onespad32, c_onespad)):
            k.dma("sp", b[:], src[:], b, writes=[b])
        k.op("dve", lambda: E["dve"].tensor_copy(out=identb[:], in_=ident[:]), [ident], [identb])
        k.op("dve", lambda: E["dve"].tensor_copy(out=selb[:], in_=sel32[:]), [sel32], [selb])
        k.op("dve", lambda: E["dve"].tensor_copy(out=onespad[:], in_=onespad32[:]), [onespad32], [onespad])
        k.op("dve", lambda: E["dve"].memset(ones32[:], 1.0), [], [ones32])
        k.op("dve", lambda: E["dve"].memset(onesb[:], 1.0), [], [onesb])
        k.op("dve", lambda: E["dve"].memset(onesbf[:], 1.0), [], [onesbf])
        k.op("dve", lambda: E["dve"].tensor_copy(out=amask0[:], in_=amask[:]), [amask], [amask0])
        k.op("dve", lambda: E["dve"].memset(amask0[:, 0:128], 0.0), [], [amask0])

        PS = [k.ps("ps%d" % i, [128, 512], F32) for i in range(8)]

        WB = [k.sb("wb%d" % i, [128, WB_ELEMS], BF16, dma=True) for i in range(NWB)]
        hT = k.sb("hT", [128, KC, T], BF16)
        ycT = k.sb("ycT", [128, 8, T], BF16)
        ymT = k.sb("ymT", [128, 8, T], BF16)
        yaT = k.sb("yaT", [128, 8, T], BF16)
        XW = 16 * T
        arX = es.enter_context(nc.sbuf_tensor("arenaX", [128, XW], F32))
        YW = 5120
        arY = es.enter_context(nc.sbuf_tensor("arenaY", [128, YW], F32))

        def vw(ar, lo, hi, dt=F32, parts=128, sem=False, shape=None):
            ap = ar[0:parts, lo:hi]
            if dt is not F32:
                ap = ap.bitcast(dt)
            if shape:
                ap = ap.rearrange(shape[0], **shape[1])
            return Buf(ap, k.newsem("s_v%d" % k.nsem) if sem else None)

        def switch(new, old):
            for nv in new:
                for ov in old:
                    deps = list(ov.r.values()) + ([ov.w] if ov.w else [])
                    for d in deps:
                        kk = id(d[0])
                        if kk not in nv.r or nv.r[kk][1] < d[1]:
                            nv.r[kk] = d

        big = vw(arX, 0, 16 * T, shape=("p (a b) -> p a b", dict(b=T)))
        mgT = vw(arY, 0, 8 * T, BF16, shape=("p (a b) -> p a b", dict(b=T)))
        xs = [k.sb("xs%d" % i, [128, T], F32, dma=True) for i in range(3)]
        xo = [k.sb("xo%d" % i, [128, T], F32, dma=True) for i in range(2)]
        acc = k.sb("acc", [128, T], F32)
        sqt = k.sb("sqt", [128, T], F32)
        rstd = k.sb("rstd", [128, T], F32)
        tA = k.sb("tA", [128, T], F32)
        tB = k.sb("tB", [128, T], F32)
        tC = k.sb("tC", [128, T], F32)
        tD = k.sb("tD", [128, T], F32)
        vb = k.sb("vb", [128, T + 2], F32)
        cv = k.sb("cv", [128, 8, 2], F32)
        convw = k.sb("convw", [128, 24], F32, dma=True)
        gains = k.sb("gains", [128, 48], F32, dma=True)
        bi = k.sb("bi", [8, 1], F32, dma=True)
        bfn = k.sb("bfn", [8, 1], F32, dma=True)
        mnb = k.sb("mnb", [64, 1024], F32, dma=True)
        sinke = k.sb("sinke", [128, 8], F32, dma=True)
        h3 = ("p (a b) -> p a b", dict(b=T))
        qT = vw(arX, 0, 4 * T, BF16, shape=h3)
        kT = vw(arX, 4 * T, 8 * T, BF16, shape=h3)
        vT = vw(arX, 8 * T, 12 * T, BF16, shape=h3)
        gateT = vw(arX, 12 * T, 16 * T, BF16, shape=h3)
        rA = [vw(arY, i * T, (i + 1) * T, parts=8) for i in range(10)]
        assert 10 * T <= YW
        mus = k.sb("mus", [8, NCH], F32)
        mup = k.sb("mup", [8, NCH], F32)
        dec = k.sb("dec", [8, NCH], F32)
        dexp = k.sb("dexp", [8, NCH, 8], F32)
        dbc = k.sb("dbc", [128, NCH, 8], F32)
        cB = k.sb("cB", [8, 1], F32)
        cM = k.sb("cM", [8, 1], F32)
        cols = k.sb("cols", [64, NCH, 4, 8], F32)
        abf = k.sb("abf", [64, 8], BF16)
        t1 = vw(arY, 0, 1024, parts=64)
        t2 = vw(arY, 1024, 2048, parts=64)
        tmpS = vw(arY, 2048, 2560, parts=64)
        AT = vw(arY, 2560, 2816, BF16, parts=64)
        k_tm = vw(arY, 2816, 3328, BF16, parts=64)
        v_tm = vw(arY, 3328, 3840, BF16, parts=64)
        av_tm = vw(arY, 3840, 4352, BF16, parts=64)
        chunk_tmps = [t1, t2, tmpS, AT, k_tm, v_tm, av_tm]
        sm = [k.sb("sm%d" % i, [64, 8], F32) for i in range(6)]
        Ct = k.sb("Ct", [128, 1024], F32)
        Ctb = k.sb("Ctb", [128, 1024], BF16)
        nst = k.sb("nst", [128, 8], F32)
        nstb = k.sb("nstb", [128, 8], BF16)
        KTd = k.sb("KTd", [128, 2, 4, 128 + T], BF16)
        Vp = k.sb("Vp", [128, NQB + 1, 4, 2, 128], BF16)
        rtab = vw(arY, 0, 4 * T, sem=True, shape=("p (a b) -> p a b", dict(b=T)))
        o_ = 4 * T
        ex = vw(arY, o_, o_ + 512)
        sgT = vw(arY, o_ + 512, o_ + 512 + T)
        o_ = o_ + 512 + T
        PT = vw(arY, o_, o_ + 256, BF16)
        qr = vw(arY, o_ + 256, o_ + 256 + T // 2, BF16)
        kr = vw(arY, o_ + 256 + T // 2, o_ + 256 + T, BF16)
        o_ = o_ + 256 + T
        ex2 = vw(arY, o_, o_ + 512)
        PT2 = vw(arY, o_ + 512, o_ + 768, BF16)
        assert o_ + 768 <= YW
        exs = [ex, ex2]
        PTs = [PT, PT2]
        attn_tmps = [rtab, ex, sgT, PT, qr, kr, ex2, PT2]
        o_ = 8 * T
        p32 = vw(arY, o_, o_ + 256, sem=True)
        pbf = vw(arY, o_ + 256, o_ + 384, BF16)
        pT = vw(arY, o_ + 384, o_ + 384 + T, BF16, shape=("p (a b) -> p a b", dict(b=T)))
        assert o_ + 384 + T <= YW
        d_tmps = [mgT, p32, pbf, pT]

        def ACT(out, in_, func, reads, writes, bias=None, scale=None):
            kw = {}
            if bias is not None:
                kw["bias"] = bias
            if scale is not None:
                kw["scale"] = scale
            return k.op("act", lambda: E["act"].activation(out=out, in_=in_, func=func, **kw), reads, writes)

        def TT(out, in0, in1, op, reads, writes, e="dve"):
            return k.op(e, lambda: E[e].tensor_tensor(out=out, in0=in0, in1=in1, op=op), reads, writes)

        def TS(out, in0, s1, s2, op0, op1, reads, writes, e="dve"):
            if s2 is None:
                return k.op(e, lambda: E[e].tensor_scalar(out=out, in0=in0, scalar1=s1, scalar2=None, op0=op0), reads, writes)
            return k.op(e, lambda: E[e].tensor_scalar(out=out, in0=in0, scalar1=s1, scalar2=s2, op0=op0, op1=op1), reads, writes)

        def STT(out, in0, sc, in1, op0, op1, reads, writes, e="dve"):
            return k.op(e, lambda: E[e].scalar_tensor_tensor(out=out, in0=in0, scalar=sc, in1=in1, op0=op0, op1=op1), reads, writes)

        def CP(out, in_, reads, writes, e="dve"):
            return k.op(e, lambda: E[e].tensor_copy(out=out, in_=in_), reads, writes)

        def RECIP(out, in_, reads, writes):
            return k.op("dve", lambda: E["dve"].reciprocal(out=out, in_=in_), reads, writes)

        def MM(out, lhsT, rhs, start, stop, reads, writes):
            return k.op("pe", lambda: E["pe"].matmul(out, lhsT, rhs, start=start, stop=stop), reads, writes)

        def TR(out, in_, idn, reads, writes):
            return k.op("pe", lambda: E["pe"].transpose(out, in_, idn), reads, writes)

        def rsqrt_to(out, in_ps, scale, reads_b, tmp):
            TS(tmp[:], in_ps, scale, EPS, ALU.mult, ALU.add, reads_b, [tmp])
            ACT(tmp[:], tmp[:], AF.Sqrt, [tmp], [tmp])
            RECIP(out[:], tmp[:], [tmp], [out])

        wstate = {"specs": [], "issued": 0, "used": 0}

        def wspec(wten, K, pieces):
            wstate["specs"].append((wten, K, pieces))

        def wissue():
            i = wstate["issued"]
            wten, K, pieces = wstate["specs"][i]
            b = WB[i % NWB]
            kc = K // 128
            off = 0
            src = wten.rearrange("(kc p) c -> p kc c", p=128)
            first = True
            for (c0, n) in pieces:
                dst = b[:, off:off + kc * n].rearrange("p (k c) -> p k c", c=n)
                k.dma("pool", dst, src[:, :, c0:c0 + n], b, writes=[b] if first else [])
                if not first:
                    b.w = (b.sem, b.cnt)
                first = False
                off += kc * n
            wstate["issued"] = i + 1

        def wget():
            u = wstate["used"]
            while wstate["issued"] < min(len(wstate["specs"]), u + NWB - (LIVE - 1)):
                wissue()
            wten, K, pieces = wstate["specs"][u]
            b = WB[u % NWB]
            kc = K // 128
            views = []
            off = 0
            for (c0, n) in pieces:
                views.append(b[:, off:off + kc * n].rearrange("p (k c) -> p k c", c=n))
                off += kc * n
            wstate["used"] = u + 1
            return b, views

        def plan_weights(l):
            W = w_in[l]
            wspec(W, D, [(O_MI, 8), (O_MF, 8)])
            for o in (O_MQ, O_MK, O_MV):
                for hp in range(4):
                    wspec(W, D, [(o + hp * 256, 256)])
            for hp in range(4):
                wspec(W, D, [(O_MO + hp * 256, 256)])
                wspec(W, D, [(O_MG + hp * 256, 256)])
            for c in range(8):
                wspec(W, D, [(O_CB + c * 128, 128), (O_CC + c * 128, 128)])
                wspec(W, D, [(O_CX + c * 128, 128), (O_CG + c * 128, 128)])
            wspec(W, D, [(O_AK, 256)])
            wspec(W, D, [(O_AV, 256)])
            for j2 in range(4):
                wspec(W, D, [(O_AQ + j2 * 256, 256)])
                wspec(W, D, [(O_AG + j2 * 256, 256)])
            for fb in range(16):
                wspec(W, D, [(O_GT + j * 2048 + fb * 128, 128) for j in range(2)])
                wspec(W, D, [(O_GT + 2 * 2048 + fb * 128, 128)])
                wspec(w_upc[l], 1024, [(fb * 128, 128)])
                wspec(w_upm[l], 1024, [(fb * 128, 128)])
                wspec(w_upa[l], 1024, [(fb * 128, 128)])
            for q8 in range(8):
                wspec(w_out[l], D, [(q8 * 256, 256)])
            for q8 in range(8):
                wspec(w_pg[l], D, [(q8 * 256, 256)])
                wspec(w_ple[l], 256, [(q8 * 256, 256)])

        for l in range(DEPTH):
            for it in range(NT):
                plan_weights(l)

        def zT(psb, Wb, wv, c0, ncol, rhs_fn, rhs_bufs, kcs=KC, pslice=None):
            o = pslice if pslice is not None else psb[0:ncol, 0:T]
            for kc in range(kcs):
                MM(o, wv[:, kc, c0:c0 + ncol], rhs_fn(kc), kc == 0, kc == kcs - 1, [Wb] + rhs_bufs, [psb])

        hrhs = lambda kc: hT[:, kc, :]

        xrow = vw(arX, 0, D, sem=True)
        xcol = vw(arX, D, 2 * D, sem=True, shape=("p (a b) -> p a b", dict(b=128)))
        for blk in range(NTOK // 128):
            k.dma("sp", xrow[:], x_in[blk * 128:(blk + 1) * 128, :], xrow, writes=[xrow])
            for g4 in range(4):
                pb = PS[g4 % 2]
                for i in range(4):
                    kc = g4 * 4 + i
                    TR(pb[:, i * 128:(i + 1) * 128], xrow[:, kc * 128:(kc + 1) * 128], ident[:], [xrow, ident], [pb])
                CP(xcol[:, g4 * 4:(g4 + 1) * 4, :], pb[:].rearrange("p (a b) -> p a b", b=128), [pb], [xcol])
            treg = xT_reg[(blk * 128) // T]
            k.dma("sp", xT_d[:, :, blk * 128:(blk + 1) * 128].rearrange("k p t -> p k t"), xcol[:], xcol,
                  reads=[xcol], writes=[treg])
        posi = vw(arX, 2 * D, 2 * D + T, I32, sem=True)
        posf = vw(arX, 2 * D + T, 2 * D + 2 * T)
        for it in range(NT):
            t0 = it * T
            k.dma("sp", posi[:], pos_in[t0:t0 + T].partition_broadcast(128), posi, writes=[posi])
            CP(posf[:], posi[:], [posi], [posf])
            TS(tA[:], posf[:], cvec[:, 0:1], None, ALU.mult, None, [posf, cvec], [tA])
            def rred(dst, src, addc):
                TS(sqt[:], src[:], addc, None, ALU.add, None, [src], [sqt])
                TS(acc[:], sqt[:], 1.0 / (2.0 * np.pi), None, ALU.mult, None, [sqt], [acc])
                CP(posi[:], acc[:], [acc], [posi])
                CP(acc[:], posi[:], [posi], [acc])
                STT(dst[:], acc[:], -2.0 * np.pi, sqt[:], ALU.mult, ALU.add, [acc, sqt], [dst])
                TS(acc[:], dst[:], np.pi, -2.0 * np.pi, ALU.is_gt, ALU.mult, [dst], [acc])
                TT(dst[:], dst[:], acc[:], ALU.add, [dst, acc], [dst])
                TS(acc[:], dst[:], -np.pi, 2.0 * np.pi, ALU.is_lt, ALU.mult, [dst], [acc])
                TT(dst[:], dst[:], acc[:], ALU.add, [dst, acc], [dst])
                TS(dst[:], dst[:], -3.1415925, 3.1415925, ALU.max, ALU.min, [dst], [dst])
            rred(tB, tA, 0.0)
            ACT(tB[:], tB[:], AF.Sin, [tB], [tB])
            TS(tB[:], tB[:], -1.0, None, ALU.mult, None, [tB], [tB])
            rred(tC, tA, 0.5 * np.pi)
            ACT(tC[:], tC[:], AF.Sin, [tC], [tC])
            TS(tC[:], tC[:], -1.0, None, ALU.mult, None, [tC], [tC])
            TS(xo[0][:], tC[:], -1.0, None, ALU.mult, None, [tC], [xo[0]])
            k.dma("sp", rope_d[2, :, t0:t0 + T], xo[0][:], xo[0], reads=[xo[0]], writes=[rope_reg])
            TS(xo[1][:], tC[:], -0.125, None, ALU.mult, None, [tC], [xo[1]])
            k.dma("sp", rope_d[0, :, t0:t0 + T], xo[1][:], xo[1], reads=[xo[1]], writes=[])
            rope_reg.w = None
            TS(tD[:], tB[:], cvec[:, 1:2], -1.0, ALU.mult, ALU.mult, [tB, cvec], [tD])
            k.dma("sp", rope_d[3, :, t0:t0 + T], tD[:], xs[0], reads=[tD], writes=[])
            TS(sqt[:], tD[:], 0.125, None, ALU.mult, None, [tD], [sqt])
            k.dma("sp", rope_d[1, :, t0:t0 + T], sqt[:], xs[1], reads=[sqt], writes=[])
        rope_deps = [(xo[0].sem, xo[0].cnt), (xo[1].sem, xo[1].cnt), (xs[0].sem, xs[0].cnt), (xs[1].sem, xs[1].cnt)]

        def ck(name):
            if stage == name:
                raise _Stop()
        try:
          ck('pro')
          for l in range(DEPTH):
              k.dma("sp", convw[:], convw_in[l], convw, writes=[convw])
              k.dma("sp", gains[:], gains_in[l], gains, writes=[gains])
              k.dma("sp", bi[:], big_in[l].rearrange("(h o) -> h o", o=1), bi, writes=[bi])
              k.dma("sp", bfn[:], bfg_in[l].rearrange("(h o) -> h o", o=1), bfn, writes=[bfn])
              TS(bfn[:], bfn[:], -1.0, None, ALU.mult, None, [bfn], [bfn])
              k.dma("sp", mnb[:], mnorm_in[l].partition_broadcast(64), mnb, writes=[mnb])
              k.dma("sp", sinke[:], sinks_in[l], sinke, writes=[sinke])
              ACT(sinke[:], sinke[:], AF.Exp, [sinke], [sinke])
              for b in (cv, Ct, Ctb, nst, nstb, cB, cM, KTd, Vp):
                  k.op("dve", lambda b=b: E["dve"].memset(b[:], 0.0), [], [b])

              for it in range(NT):
                  t0 = it * T
                  treg = xT_reg[it]

                  def norm_stats(src_fn, nchunks=KC):
                      for kc in range(nchunks):
                          sbuf, sap = src_fn(kc)
                          sq = sqb[kc % 2]
                          ACT(sq[:], sap, AF.Square, [sbuf], [sq])
                          MM(PS[7][:, 0:T], onesbf[:], sq[:], kc == 0, kc == nchunks - 1, [onesbf, sq], [PS[7]])
                      rsqrt_to(rstd, PS[7][:, 0:T], 1.0 / D, [PS[7]], tA)

                  def load_x(kc):
                      b = xs[kc % 3]
                      k.dma("sp", b[:], xT_d[kc, :, t0:t0 + T], b, reads=[treg], writes=[b])
                      return b, b[:]

                  norm_stats(load_x)
                  for kc in range(KC):
                      b, ap = load_x(kc)
                      STT(hT[:, kc, :], ap, gains[:, kc:kc + 1], rstd[:], ALU.mult, ALU.mult, [b, gains, rstd], [hT])

                  ck('p0')
                  ck('A')
                  switch(rA, d_tmps)
                  Wb, wv = wget()
                  zT(PS[0], Wb, wv[0], 0, 8, hrhs, [hT])
                  zT(PS[1], Wb, wv[1], 0, 8, hrhs, [hT])
                  li, sp_, Bc, U, Mx, em, ra, rb, rw, rt = rA
                  ACT(li[:], PS[0][0:8, 0:T], AF.Identity, [PS[0], bi], [li], bias=bi[:, 0:1])
                  ACT(rt[:], PS[1][0:8, 0:T], AF.Exp, [PS[1], bfn], [rt], bias=bfn[:, 0:1], scale=-1.0)
                  ACT(sp_[:], rt[:], AF.Ln, [rt], [sp_], bias=1.0)

                  def scan(src, tmp, op):
                      cur, nxt = src, tmp
                      sh = 1
                      while sh < T:
                          TT(nxt[:, sh:T], cur[:, sh:T], cur[:, 0:T - sh], op, [cur], [nxt])
                          CP(nxt[:, 0:sh], cur[:, 0:sh], [cur], [nxt])
                          cur, nxt = nxt, cur
                          sh *= 2
                      return cur

                  cs = scan(sp_, rt, ALU.add)
                  TS(Bc[:], cs[:], -1.0, cB[:, 0:1], ALU.mult, ALU.add, [cs, cB], [Bc])
                  TT(U[:], li[:], Bc[:], ALU.subtract, [li, Bc], [U])
                  other = rt if cs is sp_ else sp_
                  CP(other[:], U[:], [U], [other])
                  other2 = sp_ if other is rt else rt
                  cm = scan(other, other2, ALU.max)
                  TS(Mx[:], cm[:], cM[:, 0:1], None, ALU.max, None, [cm, cM], [Mx])
                  TT(em[:], Bc[:], Mx[:], ALU.add, [Bc, Mx], [em])
                  ACT(em[:], em[:], AF.Exp, [em], [em], scale=-1.0)
                  Mx3 = Mx[:].rearrange("h (c s) -> h c s", s=64)
                  CP(mus[:], Mx3[:, :, 63], [Mx], [mus])
                  CP(mup[:, 0:1], cM[:], [cM], [mup])
                  if NCH > 1:
                      CP(mup[:, 1:NCH], mus[:, 0:NCH - 1], [mus], [mup])
                  CP(cM[:], mus[:, NCH - 1:NCH], [mus], [cM])
                  CP(cB[:], Bc[:, T - 1:T], [Bc], [cB])
                  musb = mus[:].unsqueeze(2).to_broadcast([8, NCH, 64])
                  mupb = mup[:].unsqueeze(2).to_broadcast([8, NCH, 64])
                  v3 = lambda b: b[:].rearrange("h (c s) -> h c s", s=64)
                  TT(v3(ra), v3(U), musb, ALU.subtract, [U, mus], [ra])
                  ACT(ra[:], ra[:], AF.Exp, [ra], [ra])
                  TT(v3(rb), musb, Mx3, ALU.subtract, [Mx, mus], [rb])
                  ACT(rb[:], rb[:], AF.Exp, [rb], [rb])
                  TT(v3(rw), mupb, Mx3, ALU.subtract, [Mx, mup], [rw])
                  ACT(rw[:], rw[:], AF.Exp, [rw], [rw])
                  TT(dec[:], mup[:], mus[:], ALU.subtract, [mup, mus], [dec])
                  ACT(dec[:], dec[:], AF.Exp, [dec], [dec])
                  TT(dexp[:], dec[:].unsqueeze(2).to_broadcast([8, NCH, 8]),
                     ident[0:8, 0:8].unsqueeze(1).to_broadcast([8, NCH, 8]), ALU.mult, [dec, ident], [dexp])
                  MM(PS[2][:, 0:NCH * 8], ones32[0:8, :], dexp[:].rearrange("h c g -> h (c g)"), True, True,
                     [ones32, dexp], [PS[2]])
                  CP(dbc[:].rearrange("p c g -> p (c g)"), PS[2][:, 0:NCH * 8], [PS[2]], [dbc])
                  for c in range(NCH):
                      for qi, rq in enumerate((ra, rb, rw, em)):
                          o0 = (c * 4 + qi) * 8
                          TR(PS[3][0:64, o0:o0 + 8], rq[:, c * 64:(c + 1) * 64], ident[0:8, 0:8], [rq, ident], [PS[3]])
                  CP(cols[:].rearrange("p c q h -> p (c q h)"), PS[3][0:64, 0:NCH * 32], [PS[3]], [cols])

                  ck('B1')
                  switch([qT, kT, vT, gateT], [big, xrow, xcol, posi, posf])
                  for dst, scale in ((qT, 128.0 ** -0.5), (kT, None), (vT, None)):
                      for hp in range(4):
                          Wb, wv = wget()
                          for hh in range(2):
                              pb = PS[4 + (hp % 2) * 2 + hh]
                              zT(pb, Wb, wv[0], hh * 128, 128, hrhs, [hT])
                              if scale is not None:
                                  ACT(dst[:, hp * 2 + hh, :], pb[:, 0:T], AF.Copy, [pb], [dst], scale=scale)
                              elif hh % 2 == 0:
                                  ACT(dst[:, hp * 2 + hh, :], pb[:, 0:T], AF.Copy, [pb], [dst])
                              else:
                                  CP(dst[:, hp * 2 + hh, :], pb[:, 0:T], [pb], [dst])
                  for hp in range(4):
                      Wb, wv = wget()
                      Wb2, wv2 = wget()
                      for hh in range(2):
                          h = hp * 2 + hh
                          zT(PS[4 + hh], Wb, wv[0], hh * 128, 128, hrhs, [hT])
                          zT(PS[6 + hh], Wb2, wv2[0], hh * 128, 128, hrhs, [hT])
                          ACT(tA[:], PS[4 + hh][:, 0:T], AF.Sigmoid, [PS[4 + hh]], [tA])
                          ACT(tB[:], PS[6 + hh][:, 0:T], AF.Silu, [PS[6 + hh]], [tB])
                          TT(gateT[:, h, :], tA[:], tB[:], ALU.mult, [tA, tB], [gateT])

                  ck('B2')
                  switch(chunk_tmps, rA)

                  def chunk_gen():
                      for c in range(NCH):
                          cs_ = slice(c * 64, (c + 1) * 64)
                          a_col = cols[:, c, 0, :]
                          b_col = cols[:, c, 1, :]
                          w_col = cols[:, c, 2, :]
                          e_col = cols[:, c, 3, :]
                          bc3 = lambda ap, n: ap.unsqueeze(2).to_broadcast([64, 8, n])
                          pkt = PS[0][:].bitcast(BF16)
                          pvt = PS[1][:].bitcast(BF16)
                          for h in range(8):
                              TR(pkt[0:64, h * 128:(h + 1) * 128], kT[:, h, cs_], identb[:], [kT, identb], [PS[0]])
                          for h in range(8):
                              TR(pvt[0:64, h * 128:(h + 1) * 128], vT[:, h, cs_], identb[:], [vT, identb], [PS[1]])
                          ACT(k_tm[:], pkt[0:64, :], AF.Copy, [PS[0]], [k_tm])
                          CP(v_tm[:], pvt[0:64, :], [PS[1]], [v_tm])
                          TT(av_tm[:].rearrange("p (h d) -> p h d", d=128), pvt[0:64, :].rearrange("p (h d) -> p h d", d=128),
                             bc3(a_col, 128), ALU.mult, [PS[1], cols], [av_tm])
                          CP(abf[:], a_col, [cols], [abf])
                          yield
                          for h in range(8):
                              MM(PS[2][0:64, h * 64:(h + 1) * 64], kT[:, h, cs_], qT[:, h, cs_], True, True, [kT, qT], [PS[2]])
                          TT(tmpS[:].rearrange("p (h l) -> p h l", l=64), PS[2][0:64, :].rearrange("p (h l) -> p h l", l=64),
                             bc3(a_col, 64), ALU.mult, [PS[2], cols], [tmpS])
                          TT(AT[:].rearrange("p (h l) -> p h l", l=64), tmpS[:].rearrange("p (h l) -> p h l", l=64),
                             causal[:].unsqueeze(1).to_broadcast([64, 8, 64]), ALU.mult, [tmpS, causal], [AT])
                          yield
                          for h in range(8):
                              pb = PS[3 + h // 4]
                              MM(pb[0:64, (h % 4) * 128:(h % 4 + 1) * 128], AT[:, h * 64:(h + 1) * 64], v_tm[:, h * 128:(h + 1) * 128],
                                 True, True, [AT, v_tm], [pb])
                          for h in range(8):
                              MM(PS[0][0:64, h:h + 1], AT[:, h * 64:(h + 1) * 64], onesb[0:64, 0:1], True, True, [AT, onesb], [PS[0]])
                          for h in range(8):
                              pb = PS[5] if h < 4 else PS[2]
                              MM(pb[0:64, (h % 4) * 128:(h % 4 + 1) * 128], qT[:, h, cs_], Ctb[:, h * 128:(h + 1) * 128],
                                 True, True, [qT, Ctb], [pb])
                          for h in range(8):
                              MM(PS[0][0:64, 8 + h:9 + h], qT[:, h, cs_], nstb[:, h:h + 1], True, True, [qT, nstb], [PS[0]])
                          for hf in range(2):
                              sl = slice(hf * 512, (hf + 1) * 512)
                              TT(t1[:, sl].rearrange("p (h d) -> p h d", d=128), PS[3 + hf][0:64, :].rearrange("p (h d) -> p h d", d=128),
                                 bc3(b_col, 128)[:, hf * 4:(hf + 1) * 4, :], ALU.mult, [PS[3 + hf], cols], [t1])
                              TT(t2[:, sl].rearrange("p (h d) -> p h d", d=128), (PS[5] if hf == 0 else PS[2])[0:64, :].rearrange("p (h d) -> p h d", d=128),
                                 bc3(w_col, 128)[:, hf * 4:(hf + 1) * 4, :], ALU.mult, [PS[5] if hf == 0 else PS[2], cols], [t2])
                          TT(t1[:], t1[:], t2[:], ALU.add, [t1, t2], [t1])
                          TT(sm[0][:], PS[0][0:64, 0:8], b_col, ALU.mult, [PS[0], cols], [sm[0]])
                          TT(sm[1][:], PS[0][0:64, 8:16], w_col, ALU.mult, [PS[0], cols], [sm[1]])
                          TT(sm[0][:], sm[0][:], sm[1][:], ALU.add, [sm[0], sm[1]], [sm[0]])
                          TS(sm[5][:], sm[0][:], -1.0, None, ALU.mult, None, [sm[0]], [sm[5]])
                          TT(sm[0][:], sm[0][:], sm[5][:], ALU.max, [sm[0], sm[5]], [sm[0]])
                          TT(sm[0][:], sm[0][:], e_col, ALU.max, [sm[0], cols], [sm[0]])
                          RECIP(sm[2][:], sm[0][:], [sm[0]], [sm[2]])
                          TT(t1[:].rearrange("p (h d) -> p h d", d=128), t1[:].rearrange("p (h d) -> p h d", d=128),
                             bc3(sm[2][:], 128), ALU.mult, [t1, sm[2]], [t1])
                          TT(t2[:], t1[:], t1[:], ALU.mult, [t1], [t2])
                          k.op("dve", lambda: E["dve"].tensor_reduce(out=sm[3][:], in_=t2[:].rearrange("p (h d) -> p h d", d=128),
                                                                     axis=AX.X, op=ALU.add), [t2], [sm[3]])
                          TS(sm[3][:], sm[3][:], 1.0 / 128, EPS, ALU.mult, ALU.add, [sm[3]], [sm[3]])
                          ACT(sm[3][:], sm[3][:], AF.Sqrt, [sm[3]], [sm[3]])
                          RECIP(sm[4][:], sm[3][:], [sm[3]], [sm[4]])
                          TT(t1[:].rearrange("p (h d) -> p h d", d=128), t1[:].rearrange("p (h d) -> p h d", d=128),
                             bc3(sm[4][:], 128), ALU.mult, [t1, sm[4]], [t1])
                          TT(t1[:], t1[:], mnb[:], ALU.mult, [t1, mnb], [t1])
                          yield
                          for h in range(8):
                              TR(PS[2][:, h * 64:(h + 1) * 64], t1[:, h * 128:(h + 1) * 128], ident[0:64, 0:64], [t1, ident], [PS[2]])
                          TT(ymT[:, :, cs_], PS[2][:].rearrange("p (h l) -> p h l", l=64), gateT[:, :, cs_], ALU.mult,
                             [PS[2], gateT], [ymT])
                          yield
                          for h in range(8):
                              pb = PS[h // 4]
                              MM(pb[:, (h % 4) * 128:(h % 4 + 1) * 128], k_tm[:, h * 128:(h + 1) * 128], av_tm[:, h * 128:(h + 1) * 128],
                                 True, True, [k_tm, av_tm], [pb])
                          for h in range(8):
                              MM(PS[2][:, h:h + 1], k_tm[:, h * 128:(h + 1) * 128], abf[:, h:h + 1], True, True, [k_tm, abf], [PS[2]])
                          dcol = dbc[:, c, :]
                          TT(Ct[:].rearrange("p (h d) -> p h d", d=128), Ct[:].rearrange("p (h d) -> p h d", d=128),
                             dcol.unsqueeze(2).to_broadcast([128, 8, 128]), ALU.mult, [Ct, dbc], [Ct])
                          for hf in range(2):
                              sl = slice(hf * 512, (hf + 1) * 512)
                              TT(Ct[:, sl], Ct[:, sl], PS[hf][:, :], ALU.add, [Ct, PS[hf]], [Ct])
                          ACT(Ctb[:], Ct[:], AF.Copy, [Ct], [Ctb])
                          TT(nst[:], nst[:], dcol, ALU.mult, [nst, dbc], [nst])
                          TT(nst[:], nst[:], PS[2][:, 0:8], ALU.add, [nst, PS[2]], [nst])
                          CP(nstb[:], nst[:], [nst], [nstb])
                          yield


                  bg = chunk_gen()

                  def tick(n=1):
                      for _ in range(n):
                          next(bg, None)

                  for c in range(8):
                      Wb, wv = wget()
                      Wb2, wv2 = wget()
                      zT(PS[6], Wb, wv[1], 0, 128, hrhs, [hT])
                      tick()
                      zT(PS[7], Wb2, wv2[0], 0, 128, hrhs, [hT])
                      tick()
                      CP(vb[:, 0:2], cv[:, c, :], [cv], [vb])
                      ACT(tA[:], PS[6][:, 0:T], AF.Copy, [PS[6]], [tA])
                      TT(vb[:, 2:2 + T], tA[:], PS[7][:, 0:T], ALU.mult, [tA, PS[7]], [vb])
                      CP(cv[:, c, :], vb[:, T:T + 2], [vb], [cv])
                      zT(PS[6], Wb, wv[0], 0, 128, hrhs, [hT])
                      tick()
                      zT(PS[7], Wb2, wv2[1], 0, 128, hrhs, [hT])
                      tick()
                      TS(tB[:], vb[:, 0:T], convw[:, c * 3:c * 3 + 1], None, ALU.mult, None, [vb, convw], [tB])
                      STT(tB[:], vb[:, 1:T + 1], convw[:, c * 3 + 1:c * 3 + 2], tB[:], ALU.mult, ALU.add, [vb, convw, tB], [tB])
                      STT(tB[:], vb[:, 2:T + 2], convw[:, c * 3 + 2:c * 3 + 3], tB[:], ALU.mult, ALU.add, [vb, convw, tB], [tB])
                      ACT(tC[:], PS[7][:, 0:T], AF.Silu, [PS[7]], [tC])
                      TT(tB[:], tB[:], PS[6][:, 0:T], ALU.mult, [tB, PS[6]], [tB])
                      TT(ycT[:, c, :], tB[:], tC[:], ALU.mult, [tB, tC], [ycT])
                  for _ in bg:
                      pass

                  ck('B3')
                  switch(attn_tmps, chunk_tmps)
                  for d in rope_deps:
                      k._wait("sp", d)
                  k.dma("sp", rtab[:], rope_d[:, :, t0:t0 + T].rearrange("f p t -> p f t"), rtab, writes=[rtab])

                  def rope(psb, cosi, sini, dst):
                      ACT(tA[:], psb[:, 0:T], AF.Copy, [psb], [tA])
                      MM(PS[7][:, 0:T], rm32[:], tA[:], True, True, [rm32, tA], [PS[7]])
                      TT(tB[:], tA[:], rtab[:, cosi, :], ALU.mult, [tA, rtab], [tB])
                      TT(tC[:], PS[7][:, 0:T], rtab[:, sini, :], ALU.mult, [PS[7], rtab], [tC])
                      TT(dst, tB[:], tC[:], ALU.add, [tB, tC], [dst_b[0]])

                  Wb, wv = wget()
                  Wv_, wvv = wget()
                  dst_b = [kr]
                  for g2 in range(2):
                      zT(PS[0], Wb, wv[0], g2 * 128, 128, hrhs, [hT])
                      rope(PS[0], 2, 3, kr[:])
                      for s in range(2):
                          MM(PS[1][:, 0:T], selb[:, s * 128:(s + 1) * 128], kr[:], True, True, [selb, kr], [PS[1]])
                          CP(KTd[0:64, 0, g2 * 2 + s, 128:128 + T], PS[1][0:64, 0:T], [PS[1]], [KTd])
                          ACT(KTd[64:128, 1, g2 * 2 + s, 128:128 + T], PS[1][64:128, 0:T], AF.Copy, [PS[1]], [KTd])
                  for qb in range(NQB):
                      for kc in range(KC):
                          MM(PS[2][:, 0:256], hT[:, kc, qb * 128:(qb + 1) * 128], wvv[0][:, kc, :], kc == 0, kc == KC - 1,
                             [hT, Wv_], [PS[2]])
                      pv = PS[2][:, 0:256].rearrange("p (g d) -> p g d", d=64)
                      CP(Vp[:, 1 + qb, :, 0, 0:64], pv, [PS[2]], [Vp])
                      ACT(Vp[:, 1 + qb, :, 1, 64:128], pv, AF.Copy, [PS[2]], [Vp])
                  dst_b = [qr]
                  for j2 in range(4):
                      Wb, wv = wget()
                      Wb2, wv2 = wget()
                      for jj in range(2):
                          j = j2 * 2 + jj
                          g = j // 2
                          zT(PS[0], Wb, wv[0], jj * 128, 128, hrhs, [hT])
                          rope(PS[0], 0, 1, qr[:])
                          zT(PS[1], Wb2, wv2[0], jj * 128, 128, hrhs, [hT])
                          ACT(sgT[:], PS[1][:, 0:T], AF.Silu, [PS[1]], [sgT])
                          def emit_scores(qb):
                              psb = PS[2] if qb % 2 == 0 else PS[5]
                              for hf in range(2):
                                  for kbi in range(2):
                                      o0 = (hf * 2 + kbi) * 128
                                      MM(psb[:, o0:o0 + 128], KTd[:, hf, g, (qb + kbi) * 128:(qb + kbi + 1) * 128],
                                         qr[:, qb * 128:(qb + 1) * 128], True, True, [KTd, qr], [psb])

                          emit_scores(0)
                          for qb in range(NQB):
                              if qb + 1 < NQB:
                                  emit_scores(qb + 1)
                              psb = PS[2] if qb % 2 == 0 else PS[5]
                              ex_, PT_ = exs[qb % 2], PTs[qb % 2]
                              ACT(ex_[:], psb[:, :], AF.Exp, [psb], [ex_])
                              mk = amask0 if (it == 0 and qb == 0) else amask
                              TT(PT_[:].rearrange("p (h r) -> p h r", r=256), ex_[:].rearrange("p (h r) -> p h r", r=256),
                                 mk[:].unsqueeze(1).to_broadcast([128, 2, 256]), ALU.mult, [ex_, mk], [PT_])
                              n = 0
                              for hf in range(2):
                                  for kbi in range(2):
                                      o0 = (hf * 2 + kbi) * 128
                                      MM(PS[3][:, 0:128], Vp[:, qb + kbi, g, hf, :], PT_[:, o0:o0 + 128], n == 0, n == 3, [Vp, PT_], [PS[3]])
                                      n += 1
                              n = 0
                              for hf in range(2):
                                  for kbi in range(2):
                                      o0 = (hf * 2 + kbi) * 128
                                      MM(PS[4][:, 0:128], onespad[:, hf * 128:(hf + 1) * 128], PT_[:, o0:o0 + 128], n == 0, n == 3,
                                         [onespad, PT_], [PS[4]])
                                      n += 1
                              TS(tD[:, 0:128], PS[4][:, 0:128], sinke[:, j:j + 1], None, ALU.add, None, [PS[4], sinke], [tD])
                              RECIP(tD[:, 0:128], tD[:, 0:128], [tD], [tD])
                              TT(tD[:, 0:128], tD[:, 0:128], PS[3][:, 0:128], ALU.mult, [tD, PS[3]], [tD])
                              TT(yaT[:, j, qb * 128:(qb + 1) * 128], tD[:, 0:128], sgT[:, qb * 128:(qb + 1) * 128], ALU.mult, [tD, sgT], [yaT])
                  CP(KTd[:, :, :, 0:128], KTd[:, :, :, T:T + 128], [KTd], [KTd])
                  CP(Vp[:, 0, :, :, :], Vp[:, NQB, :, :, :], [Vp], [Vp])

                  ck('C')
                  ys = (ycT, ymT, yaT)
                  switch(d_tmps, attn_tmps)
                  for fb in range(16):
                      Wg, wg = wget()
                      Wg2, wg2 = wget()
                      zT(PS[0], Wg, wg[0], 0, 128, hrhs, [hT])
                      zT(PS[1], Wg, wg[1], 0, 128, hrhs, [hT])
                      zT(PS[2], Wg2, wg2[0], 0, 128, hrhs, [hT])
                      for j in range(3):
                          Wu, wu = wget()
                          zT(PS[3 + j], Wu, wu[0], 0, 128, lambda kc, j=j: ys[j][:, kc, :], [ys[j]], kcs=8)
                      for j in range(3):
                          ACT(tA[:], PS[j][:, 0:T], AF.Sigmoid, [PS[j]], [tA])
                          if j == 0:
                              TT(tB[:], tA[:], PS[3][:, 0:T], ALU.mult, [tA, PS[3]], [tB])
                          else:
                              TT(tC[:], tA[:], PS[3 + j][:, 0:T], ALU.mult, [tA, PS[3 + j]], [tC])
                              if j == 1:
                                  TT(tB[:], tB[:], tC[:], ALU.add, [tB, tC], [tB])
                              else:
                                  TT(mgT[:, fb, :], tB[:], tC[:], ALU.add, [tB, tC], [mgT])
                  mrhs = lambda kc: mgT[:, kc, :]
                  switch([big], [qT, kT, vT, gateT])
                  for q8 in range(8):
                      Wb, wv = wget()
                      for i in range(2):
                          fb = q8 * 2 + i
                          pb = PS[(q8 % 2) * 2 + i]
                          zT(pb, Wb, wv[0], i * 128, 128, mrhs, [mgT])
                          if i % 2 == 0:
                              ACT(big[:, fb, :], pb[:, 0:T], AF.Copy, [pb], [big])
                          else:
                              CP(big[:, fb, :], pb[:, 0:T], [pb], [big])
                  norm_stats(lambda kc: (big, big[:, kc, :]))
                  for kc in range(KC):
                      b, ap = load_x(kc)
                      STT(tB[:], big[:, kc, :], gains[:, 16 + kc:17 + kc], rstd[:], ALU.mult, ALU.mult, [big, gains, rstd], [tB])
                      ob = xo[kc % 2]
                      TT(ob[:], tB[:], ap, ALU.add, [tB, b], [ob])
                      CP(mgT[:, kc, :], ob[:], [ob], [mgT])
                      k.dma("sp", xT_d[kc, :, t0:t0 + T], ob[:], ob, reads=[ob], writes=[treg] if kc == 0 else [])
                  st_deps = [(xo[0].sem, xo[0].cnt), (xo[1].sem, xo[1].cnt)]
                  for qb in range(NQB):
                      k.dma("sp", p32[:], p_in[l, t0 + qb * 128:t0 + (qb + 1) * 128, :], p32, writes=[p32])
                      CP(pbf[:], p32[:], [p32], [pbf])
                      ppt = PS[4][:].bitcast(BF16)
                      for c2 in range(2):
                          TR(ppt[:, c2 * 128:(c2 + 1) * 128], pbf[:, c2 * 128:(c2 + 1) * 128], identb[:], [pbf, identb], [PS[4]])
                      CP(pT[:, :, qb * 128:(qb + 1) * 128], ppt[:, 0:256].rearrange("p (c t) -> p c t", t=128), [PS[4]], [pT])
                  for q8 in range(8):
                      Wb, wv = wget()
                      Wp, wp = wget()
                      for i in range(2):
                          fb = q8 * 2 + i
                          pa = PS[(q8 % 2) * 2 + i]
                          pe_ = PS[4 + (q8 % 2) * 2 + i]
                          zT(pa, Wb, wv[0], i * 128, 128, mrhs, [mgT])
                          zT(pe_, Wp, wp[0], i * 128, 128, lambda kc: pT[:, kc, :], [pT], kcs=2)
                          ACT(tA[:], pa[:, 0:T], AF.Sigmoid, [pa], [tA])
                          TT(big[:, fb, :], tA[:], pe_[:, 0:T], ALU.mult, [tA, pe_], [big])
                  norm_stats(lambda kc: (big, big[:, kc, :]))
                  for d in st_deps:
                      k._wait("sp", d)
                  last = (l == DEPTH - 1)
                  for kc in range(KC):
                      b, ap = load_x(kc)
                      STT(tB[:], big[:, kc, :], gains[:, 32 + kc:33 + kc], rstd[:], ALU.mult, ALU.mult, [big, gains, rstd], [tB])
                      ob = xo[kc % 2]
                      TT(ob[:], tB[:], ap, ALU.add, [tB, b], [ob])
                      k.dma("sp", xT_d[kc, :, t0:t0 + T], ob[:], ob, reads=[ob], writes=[treg] if kc == 0 else [])
                  fin = [(xo[0].sem, xo[0].cnt), (xo[1].sem, xo[1].cnt)]
                  treg.w = None
                  for d in fin:
                      k._wait("sp", d)

        except _Stop:
            pass
        switch([xcol, xrow], [big, qT, kT, vT, gateT])
        for blk in range(NTOK // 128):
            k.dma("sp", xcol[:], xT_d[:, :, blk * 128:(blk + 1) * 128].rearrange("k p t -> p k t"), xcol, writes=[xcol])
            for g4 in range(4):
                pb = PS[g4 % 2]
                for i in range(4):
                    kc = g4 * 4 + i
                    TR(pb[:, i * 128:(i + 1) * 128], xcol[:, kc, :], ident[:], [xcol, ident], [pb])
                CP(xrow[:, g4 * 512:(g4 + 1) * 512], pb[:, :], [pb], [xrow])
            k.dma("sp", y_out[blk * 128:(blk + 1) * 128, :], xrow[:], xrow, reads=[xrow])
        k._wait("sp", (xrow.sem, xrow.cnt))
        for e in ("pe", "act", "dve"):
            if k.sems[e]:
                c = k.cnt[e]
                ep = (c - 1) // EPOCH
                k._wait("sp", (k.sems[e][ep], c - ep * EPOCH))
    return nc


def host_consts():
    ident = np.eye(128, dtype=np.float32)
    rm = np.zeros((128, 128), np.float32)
    for m in range(128):
        rm[(m // 64) * 64 + ((m % 64) + 32) % 64, m] = 1.0
    sel = np.zeros((128, 256), np.float32)
    for s in range(2):
        for m in range(128):
            sel[s * 64 + (m % 64), s * 128 + m] = 1.0
    vec = np.zeros((128, 4), np.float32)
    j = np.arange(128) % 32
    vec[:, 0] = np.power(np.float32(10000.0), (-2.0 * j.astype(np.float32) / 64).astype(np.float32)).astype(np.float32)
    vec[:, 1] = np.where((np.arange(128) % 64) < 32, -1.0, 1.0)
    causal = (np.arange(64)[:, None] <= np.arange(64)[None, :]).astype(np.float32)
    kk = np.arange(128)[:, None]
    qq = np.arange(128)[None, :]
    amask = np.concatenate([(kk > qq), (kk <= qq)], axis=1).astype(np.float32)
    onespad = np.zeros((128, 256), np.float32)
    onespad[:, 0:64] = 1.0
    onespad[:, 128 + 64:256] = 1.0
    return {"c_ident": ident, "c_rm": rm, "c_sel": sel, "c_vec": vec, "c_causal": causal,
            "c_amask": amask, "c_onespad": onespad}


def layout_inputs(b, x, p, positions, w_in, conv_w, b_igate, b_fgate, mlstm_norm, attn_sinks,
                  w_up_conv, w_up_mlstm, w_up_attn, w_out, pre_norm, post_norm, w_ple,
                  w_ple_gate, ple_norm, consts):
    L = w_in.shape[0]
    f = lambda a: np.ascontiguousarray(np.asarray(a, dtype=np.float32))
    convw = f(np.asarray(conv_w).reshape(L, 3, 8, 128).transpose(0, 3, 2, 1).reshape(L, 128, 24))
    g = lambda a: np.asarray(a).reshape(L, 16, 128).transpose(0, 2, 1)
    gains = f(np.concatenate([g(pre_norm), g(post_norm), g(ple_norm)], axis=2))
    sk = np.asarray(attn_sinks).reshape(L, 8, 2)
    sinks = f(np.repeat(sk.transpose(0, 2, 1), 64, axis=1))
    m = {"x": f(x[b]), "p": f(np.asarray(p)[:, b]), "pos": np.ascontiguousarray(np.asarray(positions)[b].astype(np.int32)),
         "w_in": f(w_in), "w_up_conv": f(w_up_conv), "w_up_mlstm": f(w_up_mlstm), "w_up_attn": f(w_up_attn),
         "w_out": f(w_out), "w_ple": f(w_ple), "w_ple_gate": f(w_ple_gate), "convw": convw, "gains": gains,
         "b_igate": f(b_igate), "b_fgate": f(b_fgate), "mlstm_norm": f(mlstm_norm), "sinks": sinks}
    m.update(consts)
    return m


def kernel(**inputs):
    x = np.asarray(inputs["x"])
    B, S, _ = x.shape
    L = np.asarray(inputs["w_in"]).shape[0]
    nc = build(S, L, T=512)
    consts = host_consts()
    args = [inputs[n] for n in ("x", "p", "positions", "w_in", "conv_w", "b_igate", "b_fgate", "mlstm_norm",
                                "attn_sinks", "w_up_conv", "w_up_mlstm", "w_up_attn", "w_out", "pre_norm",
                                "post_norm", "w_ple", "w_ple_gate", "ple_norm")]
    maps = [layout_inputs(c % B, *args, consts) for c in range(B)]
    in_maps = [maps[c % B] for c in range(8)]
    res = run_bass_kernel_spmd(nc, in_maps, core_ids=list(range(8)))
    return np.stack([np.asarray(res.results[b]["y"], dtype=np.float32) for b in range(B)], axis=0)
```

```python
import numpy as np
from contextlib import ExitStack
import concourse.bass as bass
import concourse.mybir as mybir
from concourse.bass_utils import run_bass_kernel_spmd

F32 = mybir.dt.float32
BF16 = mybir.dt.bfloat16
I32 = mybir.dt.int32
AF = mybir.ActivationFunctionType
ALU = mybir.AluOpType
AX = mybir.AxisListType

D = 2048
KC = 16
IN_W = 17936
EPS = 1e-6
EPOCH = 30000
WB_ELEMS = 4096
NWB = 5
LIVE = 2

O_CB, O_CC, O_CX, O_CG = 0, 1024, 2048, 3072
O_MQ, O_MK, O_MV, O_MO, O_MI, O_MF, O_MG = 4096, 5120, 6144, 7168, 8192, 8200, 8208
O_AQ, O_AK, O_AV, O_AG = 9232, 10256, 10512, 10768
O_GT = 11792


class Buf:
    __slots__ = ("t", "w", "r", "sem", "cnt")

    def __init__(self, t=None, sem=None):
        self.t = t
        self.w = None
        self.r = {}
        self.sem = sem
        self.cnt = 0

    def __getitem__(self, k):
        return self.t[k]


class KB:
    def __init__(self, nc, es):
        self.nc = nc
        self.es = es
        self.eng = {"pe": nc.tensor, "act": nc.scalar, "dve": nc.vector,
                    "pool": nc.gpsimd, "sp": nc.sync}
        self.cnt = {e: 0 for e in self.eng}
        self.sems = {e: [] for e in self.eng}
        self.waited = {e: {} for e in self.eng}
        self.nsem = 0

    def newsem(self, name):
        self.nsem += 1
        return self.es.enter_context(self.nc.semaphore(name))

    def sb(self, name, shape, dt, dma=False):
        t = self.es.enter_context(self.nc.sbuf_tensor("sb_" + name, shape, dt))
        return Buf(t, self.newsem("s_" + name) if dma else None)

    def ps(self, name, shape, dt=F32):
        t = self.es.enter_context(self.nc.psum_tensor(name, shape, dt))
        return Buf(t)

    def _mark(self, e):
        c = self.cnt[e]
        ep = c // EPOCH
        while len(self.sems[e]) <= ep:
            self.sems[e].append(self.newsem("c_%s_%d" % (e, len(self.sems[e]))))
        self.cnt[e] = c + 1
        return (self.sems[e][ep], c - ep * EPOCH + 1)

    def _wait(self, e, dep):
        if dep is None:
            return
        sem, val = dep
        if e == "pe" and any(sem is s for s in self.sems["pe"]):
            return
        k = id(sem)
        if self.waited[e].get(k, 0) >= val:
            return
        self.waited[e][k] = val
        self.eng[e].wait_ge(sem, val)

    def op(self, e, fn, reads=(), writes=()):
        for b in reads:
            self._wait(e, b.w)
        for b in writes:
            self._wait(e, b.w)
            for d in list(b.r.values()):
                self._wait(e, d)
        ins = fn()
        m = self._mark(e)
        ins.then_inc(m[0], 1)
        for b in reads:
            b.r[id(m[0])] = m
        for b in writes:
            b.w = m
            b.r = {}
        return ins

    def dma(self, q, out_ap, in_ap, semb, reads=(), writes=()):
        for b in reads:
            self._wait(q, b.w)
        for b in writes:
            self._wait(q, b.w)
            for d in list(b.r.values()):
                self._wait(q, d)
        ins = self.eng[q].dma_start(out=out_ap, in_=in_ap)
        semb.cnt += 16
        ins.then_inc(semb.sem, 16)
        m = (semb.sem, semb.cnt)
        for b in reads:
            b.r[id(semb.sem)] = m
        for b in writes:
            b.w = m
            b.r = {}

    def wait_all(self, q, bufs):
        for b in bufs:
            self._wait(q, b.w)


class _Stop(Exception):
    pass


def build(NTOK, DEPTH, T=256, debug=None, stage=None):
    nc = bass.Bass("TRN2", target_bir_lowering=False)
    NT = NTOK // T
    NCH = T // 64
    NQB = T // 128
    dbg = {}

    def din(name, shape, dt=F32):
        return nc.dram_tensor(name, shape, dt, kind="ExternalInput").ap()

    x_in = din("x", [NTOK, D])
    p_in = din("p", [DEPTH, NTOK, 256])
    pos_in = din("pos", [NTOK], I32)
    w_in = din("w_in", [DEPTH, D, IN_W])
    w_upc = din("w_up_conv", [DEPTH, 1024, D])
    w_upm = din("w_up_mlstm", [DEPTH, 1024, D])
    w_upa = din("w_up_attn", [DEPTH, 1024, D])
    w_out = din("w_out", [DEPTH, D, D])
    w_ple = din("w_ple", [DEPTH, 256, D])
    w_pg = din("w_ple_gate", [DEPTH, D, D])
    convw_in = din("convw", [DEPTH, 128, 24])
    gains_in = din("gains", [DEPTH, 128, 48])
    big_in = din("b_igate", [DEPTH, 8])
    bfg_in = din("b_fgate", [DEPTH, 8])
    mnorm_in = din("mlstm_norm", [DEPTH, 1024])
    sinks_in = din("sinks", [DEPTH, 128, 8])
    c_ident = din("c_ident", [128, 128])
    c_rm = din("c_rm", [128, 128])
    c_sel = din("c_sel", [128, 256])
    c_vec = din("c_vec", [128, 4])
    c_causal = din("c_causal", [64, 64])
    c_amask = din("c_amask", [128, 256])
    c_onespad = din("c_onespad", [128, 256])
    y_out = nc.dram_tensor("y", [NTOK, D], F32, kind="ExternalOutput").ap()
    xT_d = nc.dram_tensor("xT_scr", [KC, 128, NTOK], F32, kind="Internal").ap()
    rope_d = nc.dram_tensor("rope_scr", [4, 128, NTOK], F32, kind="Internal").ap()
    if debug:
        for nm, shp in debug.items():
            dbg[nm] = nc.dram_tensor("dbg_" + nm, shp, F32, kind="ExternalOutput").ap()

    es = ExitStack()
    with es:
        k = KB(nc, es)
        E = {e: k.eng[e] for e in k.eng}
        xT_reg = [Buf() for _ in range(NT)]
        rope_reg = Buf()

        ident = k.sb("ident", [128, 128], F32, dma=True)
        identb = k.sb("identb", [128, 128], BF16)
        rm32 = k.sb("rm32", [128, 128], F32, dma=True)
        selb = k.sb("selb", [128, 256], BF16)
        sel32 = k.sb("sel32", [128, 256], F32, dma=True)
        cvec = k.sb("cvec", [128, 4], F32, dma=True)
        causal = k.sb("causal", [64, 64], F32, dma=True)
        amask = k.sb("amask", [128, 256], F32, dma=True)
        amask0 = k.sb("amask0", [128, 256], F32)
        onespad32 = k.sb("onespad32", [128, 256], F32, dma=True)
        onespad = k.sb("onespad", [128, 256], BF16)
        ones32 = k.sb("ones32", [128, 128], F32)
        onesb = k.sb("onesb", [128, 8], BF16)
        onesbf = k.sb("onesbf", [128, 128], BF16)
        sqb = [k.sb("sqb%d" % i, [128, T], BF16) for i in range(2)]
        for b, src in ((ident, c_ident), (rm32, c_rm), (sel32, c_sel), (cvec, c_vec),
                       (causal, c_causal), (amask, c_amask), (onespad32, c_onespad)):
            k.dma("sp", b[:], src[:], b, writes=[b])
        k.op("dve", lambda: E["dve"].tensor_copy(out=identb[:], in_=ident[:]), [ident], [identb])
        k.op("dve", lambda: E["dve"].tensor_copy(out=selb[:], in_=sel32[:]), [sel32], [selb])
        k.op("dve", lambda: E["dve"].tensor_copy(out=onespad[:], in_=onespad32[:]), [onespad32], [onespad])
        k.op("dve", lambda: E["dve"].memset(ones32[:], 1.0), [], [ones32])
        k.op("dve", lambda: E["dve"].memset(onesb[:], 1.0), [], [onesb])
        k.op("dve", lambda: E["dve"].memset(onesbf[:], 1.0), [], [onesbf])
        k.op("dve", lambda: E["dve"].tensor_copy(out=amask0[:], in_=amask[:]), [amask], [amask0])
        k.op("dve", lambda: E["dve"].memset(amask0[:, 0:128], 0.0), [], [amask0])

        PS = [k.ps("ps%d" % i, [128, 512], F32) for i in range(8)]

        WB = [k.sb("wb%d" % i, [128, WB_ELEMS], BF16, dma=True) for i in range(NWB)]
        hT = k.sb("hT", [128, KC, T], BF16)
        ycT = k.sb("ycT", [128, 8, T], BF16)
        ymT = k.sb("ymT", [128, 8, T], BF16)
        yaT = k.sb("yaT", [128, 8, T], BF16)
        XW = 16 * T
        arX = es.enter_context(nc.sbuf_tensor("arenaX", [128, XW], F32))
        YW = 5120
        arY = es.enter_context(nc.sbuf_tensor("arenaY", [128, YW], F32))

        def vw(ar, lo, hi, dt=F32, parts=128, sem=False, shape=None):
            ap = ar[0:parts, lo:hi]
            if dt is not F32:
                ap = ap.bitcast(dt)
            if shape:
                ap = ap.rearrange(shape[0], **shape[1])
            return Buf(ap, k.newsem("s_v%d" % k.nsem) if sem else None)

        def switch(new, old):
            for nv in new:
                for ov in old:
                    deps = list(ov.r.values()) + ([ov.w] if ov.w else [])
                    for d in deps:
                        kk = id(d[0])
                        if kk not in nv.r or nv.r[kk][1] < d[1]:
                            nv.r[kk] = d

        big = vw(arX, 0, 16 * T, shape=("p (a b) -> p a b", dict(b=T)))
        mgT = vw(arY, 0, 8 * T, BF16, shape=("p (a b) -> p a b", dict(b=T)))
        xs = [k.sb("xs%d" % i, [128, T], F32, dma=True) for i in range(3)]
        xo = [k.sb("xo%d" % i, [128, T], F32, dma=True) for i in range(2)]
        acc = k.sb("acc", [128, T], F32)
        sqt = k.sb("sqt", [128, T], F32)
        rstd = k.sb("rstd", [128, T], F32)
        tA = k.sb("tA", [128, T], F32)
        tB = k.sb("tB", [128, T], F32)
        tC = k.sb("tC", [128, T], F32)
        tD = k.sb("tD", [128, T], F32)
        vb = k.sb("vb", [128, T + 2], F32)
        cv = k.sb("cv", [128, 8, 2], F32)
        convw = k.sb("convw", [128, 24], F32, dma=True)
        gains = k.sb("gains", [128, 48], F32, dma=True)
        bi = k.sb("bi", [8, 1], F32, dma=True)
        bfn = k.sb("bfn", [8, 1], F32, dma=True)
        mnb = k.sb("mnb", [64, 1024], F32, dma=True)
        sinke = k.sb("sinke", [128, 8], F32, dma=True)
        h3 = ("p (a b) -> p a b", dict(b=T))
        qT = vw(arX, 0, 4 * T, BF16, shape=h3)
        kT = vw(arX, 4 * T, 8 * T, BF16, shape=h3)
        vT = vw(arX, 8 * T, 12 * T, BF16, shape=h3)
        gateT = vw(arX, 12 * T, 16 * T, BF16, shape=h3)
        rA = [vw(arY, i * T, (i + 1) * T, parts=8) for i in range(10)]
        assert 10 * T <= YW
        mus = k.sb("mus", [8, NCH], F32)
        mup = k.sb("mup", [8, NCH], F32)
        dec = k.sb("dec", [8, NCH], F32)
        dexp = k.sb("dexp", [8, NCH, 8], F32)
        dbc = k.sb("dbc", [128, NCH, 8], F32)
        cB = k.sb("cB", [8, 1], F32)
        cM = k.sb("cM", [8, 1], F32)
        cols = k.sb("cols", [64, NCH, 4, 8], F32)
        abf = k.sb("abf", [64, 8], BF16)
        t1 = vw(arY, 0, 1024, parts=64)
        t2 = vw(arY, 1024, 2048, parts=64)
        tmpS = vw(arY, 2048, 2560, parts=64)
        AT = vw(arY, 2560, 2816, BF16, parts=64)
        k_tm = vw(arY, 2816, 3328, BF16, parts=64)
        v_tm = vw(arY, 3328, 3840, BF16, parts=64)
        av_tm = vw(arY, 3840, 4352, BF16, parts=64)
        chunk_tmps = [t1, t2, tmpS, AT, k_tm, v_tm, av_tm]
        sm = [k.sb("sm%d" % i, [64, 8], F32) for i in range(6)]
        Ct = k.sb("Ct", [128, 1024], F32)
        Ctb = k.sb("Ctb", [128, 1024], BF16)
        nst = k.sb("nst", [128, 8], F32)
        nstb = k.sb("nstb", [128, 8], BF16)
        KTd = k.sb("KTd", [128, 2, 4, 128 + T], BF16)
        Vp = k.sb("Vp", [128, NQB + 1, 4, 2, 128], BF16)
        rtab = vw(arY, 0, 4 * T, sem=True, shape=("p (a b) -> p a b", dict(b=T)))
        o_ = 4 * T
        ex = vw(arY, o_, o_ + 512)
        sgT = vw(arY, o_ + 512, o_ + 512 + T)
        o_ = o_ + 512 + T
        PT = vw(arY, o_, o_ + 256, BF16)
        qr = vw(arY, o_ + 256, o_ + 256 + T // 2, BF16)
        kr = vw(arY, o_ + 256 + T // 2, o_ + 256 + T, BF16)
        o_ = o_ + 256 + T
        ex2 = vw(arY, o_, o_ + 512)
        PT2 = vw(arY, o_ + 512, o_ + 768, BF16)
        assert o_ + 768 <= YW
        exs = [ex, ex2]
        PTs = [PT, PT2]
        attn_tmps = [rtab, ex, sgT, PT, qr, kr, ex2, PT2]
        o_ = 8 * T
        p32 = vw(arY, o_, o_ + 256, sem=True)
        pbf = vw(arY, o_ + 256, o_ + 384, BF16)
        pT = vw(arY, o_ + 384, o_ + 384 + T, BF16, shape=("p (a b) -> p a b", dict(b=T)))
        assert o_ + 384 + T <= YW
        d_tmps = [mgT, p32, pbf, pT]

        def ACT(out, in_, func, reads, writes, bias=None, scale=None):
            kw = {}
            if bias is not None:
                kw["bias"] = bias
            if scale is not None:
                kw["scale"] = scale
            return k.op("act", lambda: E["act"].activation(out=out, in_=in_, func=func, **kw), reads, writes)

        def TT(out, in0, in1, op, reads, writes, e="dve"):
            return k.op(e, lambda: E[e].tensor_tensor(out=out, in0=in0, in1=in1, op=op), reads, writes)

        def TS(out, in0, s1, s2, op0, op1, reads, writes, e="dve"):
            if s2 is None:
                return k.op(e, lambda: E[e].tensor_scalar(out=out, in0=in0, scalar1=s1, scalar2=None, op0=op0), reads, writes)
            return k.op(e, lambda: E[e].tensor_scalar(out=out, in0=in0, scalar1=s1, scalar2=s2, op0=op0, op1=op1), reads, writes)

        def STT(out, in0, sc, in1, op0, op1, reads, writes, e="dve"):
            return k.op(e, lambda: E[e].scalar_tensor_tensor(out=out, in0=in0, scalar=sc, in1=in1, op0=op0, op1=op1), reads, writes)

        def CP(out, in_, reads, writes, e="dve"):
            return k.op(e, lambda: E[e].tensor_copy(out=out, in_=in_), reads, writes)

        def RECIP(out, in_, reads, writes):
            return k.op("dve", lambda: E["dve"].reciprocal(out=out, in_=in_), reads, writes)

        def MM(out, lhsT, rhs, start, stop, reads, writes):
            return k.op("pe", lambda: E["pe"].matmul(out, lhsT, rhs, start=start, stop=stop), reads, writes)

        def TR(out, in_, idn, reads, writes):
            return k.op("pe", lambda: E["pe"].transpose(out, in_, idn), reads, writes)

        def rsqrt_to(out, in_ps, scale, reads_b, tmp):
            TS(tmp[:], in_ps, scale, EPS, ALU.mult, ALU.add, reads_b, [tmp])
            ACT(tmp[:], tmp[:], AF.Sqrt, [tmp], [tmp])
            RECIP(out[:], tmp[:], [tmp], [out])

        wstate = {"specs": [], "issued": 0, "used": 0}

        def wspec(wten, K, pieces):
            wstate["specs"].append((wten, K, pieces))

        def wissue():
            i = wstate["issued"]
            wten, K, pieces = wstate["specs"][i]
            b = WB[i % NWB]
            kc = K // 128
            off = 0
            src = wten.rearrange("(kc p) c -> p kc c", p=128)
            first = True
            for (c0, n) in pieces:
                dst = b[:, off:off + kc * n].rearrange("p (k c) -> p k c", c=n)
                k.dma("pool", dst, src[:, :, c0:c0 + n], b, writes=[b] if first else [])
                if not first:
                    b.w = (b.sem, b.cnt)
                first = False
                off += kc * n
            wstate["issued"] = i + 1

        def wget():
            u = wstate["used"]
            while wstate["issued"] < min(len(wstate["specs"]), u + NWB - (LIVE - 1)):
                wissue()
            wten, K, pieces = wstate["specs"][u]
            b = WB[u % NWB]
            kc = K // 128
            views = []
            off = 0
            for (c0, n) in pieces:
                views.append(b[:, off:off + kc * n].rearrange("p (k c) -> p k c", c=n))
                off += kc * n
            wstate["used"] = u + 1
            return b, views

        def plan_weights(l):
            W = w_in[l]
            wspec(W, D, [(O_MI, 8), (O_MF, 8)])
            for o in (O_MQ, O_MK, O_MV):
                for hp in range(4):
                    wspec(W, D, [(o + hp * 256, 256)])
            for hp in range(4):
                wspec(W, D, [(O_MO + hp * 256, 256)])
                wspec(W, D, [(O_MG + hp * 256, 256)])
            for c in range(8):
                wspec(W, D, [(O_CB + c * 128, 128), (O_CC + c * 128, 128)])
                wspec(W, D, [(O_CX + c * 128, 128), (O_CG + c * 128, 128)])
            wspec(W, D, [(O_AK, 256)])
            wspec(W, D, [(O_AV, 256)])
            for j2 in range(4):
                wspec(W, D, [(O_AQ + j2 * 256, 256)])
                wspec(W, D, [(O_AG + j2 * 256, 256)])
            for fb in range(16):
                wspec(W, D, [(O_GT + j * 2048 + fb * 128, 128) for j in range(2)])
                wspec(W, D, [(O_GT + 2 * 2048 + fb * 128, 128)])
                wspec(w_upc[l], 1024, [(fb * 128, 128)])
                wspec(w_upm[l], 1024, [(fb * 128, 128)])
                wspec(w_upa[l], 1024, [(fb * 128, 128)])
            for q8 in range(8):
                wspec(w_out[l], D, [(q8 * 256, 256)])
            for q8 in range(8):
                wspec(w_pg[l], D, [(q8 * 256, 256)])
                wspec(w_ple[l], 256, [(q8 * 256, 256)])

        for l in range(DEPTH):
            for it in range(NT):
                plan_weights(l)

        def zT(psb, Wb, wv, c0, ncol, rhs_fn, rhs_bufs, kcs=KC, pslice=None):
            o = pslice if pslice is not None else psb[0:ncol, 0:T]
            for kc in range(kcs):
                MM(o, wv[:, kc, c0:c0 + ncol], rhs_fn(kc), kc == 0, kc == kcs - 1, [Wb] + rhs_bufs, [psb])

        hrhs = lambda kc: hT[:, kc, :]

        xrow = vw(arX, 0, D, sem=True)
        xcol = vw(arX, D, 2 * D, sem=True, shape=("p (a b) -> p a b", dict(b=128)))
        for blk in range(NTOK // 128):
            k.dma("sp", xrow[:], x_in[blk * 128:(blk + 1) * 128, :], xrow, writes=[xrow])
            for g4 in range(4):
                pb = PS[g4 % 2]
                for i in range(4):
                    kc = g4 * 4 + i
                    TR(pb[:, i * 128:(i + 1) * 128], xrow[:, kc * 128:(kc + 1) * 128], ident[:], [xrow, ident], [pb])
                CP(xcol[:, g4 * 4:(g4 + 1) * 4, :], pb[:].rearrange("p (a b) -> p a b", b=128), [pb], [xcol])
            treg = xT_reg[(blk * 128) // T]
            k.dma("sp", xT_d[:, :, blk * 128:(blk + 1) * 128].rearrange("k p t -> p k t"), xcol[:], xcol,
                  reads=[xcol], writes=[treg])
        posi = vw(arX, 2 * D, 2 * D + T, I32, sem=True)
        posf = vw(arX, 2 * D + T, 2 * D + 2 * T)
        rs0 = Buf(None, k.newsem("s_rope0"))
        rs1 = Buf(None, k.newsem("s_rope1"))
        for it in range(NT):
            t0 = it * T
            k.dma("sp", posi[:], pos_in[t0:t0 + T].partition_broadcast(128), posi, writes=[posi])
            CP(posf[:], posi[:], [posi], [posf])
            TS(tA[:], posf[:], cvec[:, 0:1], None, ALU.mult, None, [posf, cvec], [tA])
            def rred(dst, src, addc):
                TS(sqt[:], src[:], addc, None, ALU.add, None, [src], [sqt])
                TS(acc[:], sqt[:], 1.0 / (2.0 * np.pi), None, ALU.mult, None, [sqt], [acc])
                CP(posi[:], acc[:], [acc], [posi])
                CP(acc[:], posi[:], [posi], [acc])
                STT(dst[:], acc[:], -2.0 * np.pi, sqt[:], ALU.mult, ALU.add, [acc, sqt], [dst])
                TS(acc[:], dst[:], np.pi, -2.0 * np.pi, ALU.is_gt, ALU.mult, [dst], [acc])
                TT(dst[:], dst[:], acc[:], ALU.add, [dst, acc], [dst])
                TS(acc[:], dst[:], -np.pi, 2.0 * np.pi, ALU.is_lt, ALU.mult, [dst], [acc])
                TT(dst[:], dst[:], acc[:], ALU.add, [dst, acc], [dst])
                TS(dst[:], dst[:], -3.1415925, 3.1415925, ALU.max, ALU.min, [dst], [dst])
            rred(tB, tA, 0.0)
            ACT(tB[:], tB[:], AF.Sin, [tB], [tB])
            TS(tB[:], tB[:], -1.0, None, ALU.mult, None, [tB], [tB])
            rred(tC, tA, 0.5 * np.pi)
            ACT(tC[:], tC[:], AF.Sin, [tC], [tC])
            TS(tC[:], tC[:], -1.0, None, ALU.mult, None, [tC], [tC])
            TS(xo[0][:], tC[:], -1.0, None, ALU.mult, None, [tC], [xo[0]])
            k.dma("sp", rope_d[2, :, t0:t0 + T], xo[0][:], xo[0], reads=[xo[0]], writes=[rope_reg])
            TS(xo[1][:], tC[:], -0.125, None, ALU.mult, None, [tC], [xo[1]])
            k.dma("sp", rope_d[0, :, t0:t0 + T], xo[1][:], xo[1], reads=[xo[1]], writes=[])
            rope_reg.w = None
            TS(tD[:], tB[:], cvec[:, 1:2], -1.0, ALU.mult, ALU.mult, [tB, cvec], [tD])
            k.dma("sp", rope_d[3, :, t0:t0 + T], tD[:], rs0, reads=[tD], writes=[])
            TS(sqt[:], tD[:], 0.125, None, ALU.mult, None, [tD], [sqt])
            k.dma("sp", rope_d[1, :, t0:t0 + T], sqt[:], rs1, reads=[sqt], writes=[])
        rope_deps = [(xo[0].sem, xo[0].cnt), (xo[1].sem, xo[1].cnt), (rs0.sem, rs0.cnt), (rs1.sem, rs1.cnt)]
        for d in rope_deps:
            k._wait("sp", d)

        def ck(name):
            if stage == name:
                raise _Stop()
        try:
          ck('pro')
          for l in range(DEPTH):
              k.dma("sp", convw[:], convw_in[l], convw, writes=[convw])
              k.dma("sp", gains[:], gains_in[l], gains, writes=[gains])
              k.dma("sp", bi[:], big_in[l].rearrange("(h o) -> h o", o=1), bi, writes=[bi])
              k.dma("sp", bfn[:], bfg_in[l].rearrange("(h o) -> h o", o=1), bfn, writes=[bfn])
              TS(bfn[:], bfn[:], -1.0, None, ALU.mult, None, [bfn], [bfn])
              k.dma("sp", mnb[:], mnorm_in[l].partition_broadcast(64), mnb, writes=[mnb])
              k.dma("sp", sinke[:], sinks_in[l], sinke, writes=[sinke])
              ACT(sinke[:], sinke[:], AF.Exp, [sinke], [sinke])
              for b in (cv, Ct, Ctb, nst, nstb, cB, cM, KTd, Vp):
                  k.op("dve", lambda b=b: E["dve"].memset(b[:], 0.0), [], [b])

              for it in range(NT):
                  t0 = it * T
                  treg = xT_reg[it]

                  def norm_stats(src_fn, nchunks=KC):
                      for kc in range(nchunks):
                          sbuf, sap = src_fn(kc)
                          sq = sqb[kc % 2]
                          ACT(sq[:], sap, AF.Square, [sbuf], [sq])
                          MM(PS[7][:, 0:T], onesbf[:], sq[:], kc == 0, kc == nchunks - 1, [onesbf, sq], [PS[7]])
                      rsqrt_to(rstd, PS[7][:, 0:T], 1.0 / D, [PS[7]], tA)

                  def load_x(kc):
                      b = xs[kc % 3]
                      k.dma("sp", b[:], xT_d[kc, :, t0:t0 + T], b, reads=[treg], writes=[b])
                      return b, b[:]

                  norm_stats(load_x)
                  for kc in range(KC):
                      b, ap = load_x(kc)
                      STT(hT[:, kc, :], ap, gains[:, kc:kc + 1], rstd[:], ALU.mult, ALU.mult, [b, gains, rstd], [hT])

                  ck('p0')
                  ck('A')
                  switch(rA, d_tmps)
                  Wb, wv = wget()
                  zT(PS[0], Wb, wv[0], 0, 8, hrhs, [hT])
                  zT(PS[1], Wb, wv[1], 0, 8, hrhs, [hT])
                  li, sp_, Bc, U, Mx, em, ra, rb, rw, rt = rA
                  ACT(li[:], PS[0][0:8, 0:T], AF.Identity, [PS[0], bi], [li], bias=bi[:, 0:1])
                  ACT(rt[:], PS[1][0:8, 0:T], AF.Exp, [PS[1], bfn], [rt], bias=bfn[:, 0:1], scale=-1.0)
                  ACT(sp_[:], rt[:], AF.Ln, [rt], [sp_], bias=1.0)

                  def scan(src, tmp, op):
                      cur, nxt = src, tmp
                      sh = 1
                      while sh < T:
                          TT(nxt[:, sh:T], cur[:, sh:T], cur[:, 0:T - sh], op, [cur], [nxt])
                          CP(nxt[:, 0:sh], cur[:, 0:sh], [cur], [nxt])
                          cur, nxt = nxt, cur
                          sh *= 2
                      return cur

                  cs = scan(sp_, rt, ALU.add)
                  TS(Bc[:], cs[:], -1.0, cB[:, 0:1], ALU.mult, ALU.add, [cs, cB], [Bc])
                  TT(U[:], li[:], Bc[:], ALU.subtract, [li, Bc], [U])
                  other = rt if cs is sp_ else sp_
                  CP(other[:], U[:], [U], [other])
                  other2 = sp_ if other is rt else rt
                  cm = scan(other, other2, ALU.max)
                  TS(Mx[:], cm[:], cM[:, 0:1], None, ALU.max, None, [cm, cM], [Mx])
                  TT(em[:], Bc[:], Mx[:], ALU.add, [Bc, Mx], [em])
                  ACT(em[:], em[:], AF.Exp, [em], [em], scale=-1.0)
                  Mx3 = Mx[:].rearrange("h (c s) -> h c s", s=64)
                  CP(mus[:], Mx3[:, :, 63], [Mx], [mus])
                  CP(mup[:, 0:1], cM[:], [cM], [mup])
                  if NCH > 1:
                      CP(mup[:, 1:NCH], mus[:, 0:NCH - 1], [mus], [mup])
                  CP(cM[:], mus[:, NCH - 1:NCH], [mus], [cM])
                  CP(cB[:], Bc[:, T - 1:T], [Bc], [cB])
                  musb = mus[:].unsqueeze(2).to_broadcast([8, NCH, 64])
                  mupb = mup[:].unsqueeze(2).to_broadcast([8, NCH, 64])
                  v3 = lambda b: b[:].rearrange("h (c s) -> h c s", s=64)
                  TT(v3(ra), v3(U), musb, ALU.subtract, [U, mus], [ra])
                  ACT(ra[:], ra[:], AF.Exp, [ra], [ra])
                  TT(v3(rb), musb, Mx3, ALU.subtract, [Mx, mus], [rb])
                  ACT(rb[:], rb[:], AF.Exp, [rb], [rb])
                  TT(v3(rw), mupb, Mx3, ALU.subtract, [Mx, mup], [rw])
                  ACT(rw[:], rw[:], AF.Exp, [rw], [rw])
                  TT(dec[:], mup[:], mus[:], ALU.subtract, [mup, mus], [dec])
                  ACT(dec[:], dec[:], AF.Exp, [dec], [dec])
                  TT(dexp[:], dec[:].unsqueeze(2).to_broadcast([8, NCH, 8]),
                     ident[0:8, 0:8].unsqueeze(1).to_broadcast([8, NCH, 8]), ALU.mult, [dec, ident], [dexp])
                  MM(PS[2][:, 0:NCH * 8], ones32[0:8, :], dexp[:].rearrange("h c g -> h (c g)"), True, True,
                     [ones32, dexp], [PS[2]])
                  CP(dbc[:].rearrange("p c g -> p (c g)"), PS[2][:, 0:NCH * 8], [PS[2]], [dbc])
                  for c in range(NCH):
                      for qi, rq in enumerate((ra, rb, rw, em)):
                          o0 = (c * 4 + qi) * 8
                          TR(PS[3][0:64, o0:o0 + 8], rq[:, c * 64:(c + 1) * 64], ident[0:8, 0:8], [rq, ident], [PS[3]])
                  CP(cols[:].rearrange("p c q h -> p (c q h)"), PS[3][0:64, 0:NCH * 32], [PS[3]], [cols])

                  ck('B1')
                  switch([qT, kT, vT, gateT], [big, xrow, xcol, posi, posf])
                  for dst, scale in ((qT, 128.0 ** -0.5), (kT, None), (vT, None)):
                      for hp in range(4):
                          Wb, wv = wget()
                          for hh in range(2):
                              pb = PS[4 + (hp % 2) * 2 + hh]
                              zT(pb, Wb, wv[0], hh * 128, 128, hrhs, [hT])
                              if scale is not None:
                                  ACT(dst[:, hp * 2 + hh, :], pb[:, 0:T], AF.Copy, [pb], [dst], scale=scale)
                              elif hh % 2 == 0:
                                  ACT(dst[:, hp * 2 + hh, :], pb[:, 0:T], AF.Copy, [pb], [dst])
                              else:
                                  CP(dst[:, hp * 2 + hh, :], pb[:, 0:T], [pb], [dst])
                  for hp in range(4):
                      Wb, wv = wget()
                      Wb2, wv2 = wget()
                      for hh in range(2):
                          h = hp * 2 + hh
                          zT(PS[4 + hh], Wb, wv[0], hh * 128, 128, hrhs, [hT])
                          zT(PS[6 + hh], Wb2, wv2[0], hh * 128, 128, hrhs, [hT])
                          ACT(tA[:], PS[4 + hh][:, 0:T], AF.Sigmoid, [PS[4 + hh]], [tA])
                          ACT(tB[:], PS[6 + hh][:, 0:T], AF.Silu, [PS[6 + hh]], [tB])
                          TT(gateT[:, h, :], tA[:], tB[:], ALU.mult, [tA, tB], [gateT])

                  ck('B2')
                  switch(chunk_tmps, rA)

                  def chunk_gen():
                      for c in range(NCH):
                          cs_ = slice(c * 64, (c + 1) * 64)
                          a_col = cols[:, c, 0, :]
                          b_col = cols[:, c, 1, :]
                          w_col = cols[:, c, 2, :]
                          e_col = cols[:, c, 3, :]
                          bc3 = lambda ap, n: ap.unsqueeze(2).to_broadcast([64, 8, n])
                          pkt = PS[0][:].bitcast(BF16)
                          pvt = PS[1][:].bitcast(BF16)
                          for h in range(8):
                              TR(pkt[0:64, h * 128:(h + 1) * 128], kT[:, h, cs_], identb[:], [kT, identb], [PS[0]])
                          for h in range(8):
                              TR(pvt[0:64, h * 128:(h + 1) * 128], vT[:, h, cs_], identb[:], [vT, identb], [PS[1]])
                          ACT(k_tm[:], pkt[0:64, :], AF.Copy, [PS[0]], [k_tm])
                          CP(v_tm[:], pvt[0:64, :], [PS[1]], [v_tm])
                          TT(av_tm[:].rearrange("p (h d) -> p h d", d=128), pvt[0:64, :].rearrange("p (h d) -> p h d", d=128),
                             bc3(a_col, 128), ALU.mult, [PS[1], cols], [av_tm])
                          CP(abf[:], a_col, [cols], [abf])
                          yield
                          for h in range(8):
                              MM(PS[2][0:64, h * 64:(h + 1) * 64], kT[:, h, cs_], qT[:, h, cs_], True, True, [kT, qT], [PS[2]])
                          TT(tmpS[:].rearrange("p (h l) -> p h l", l=64), PS[2][0:64, :].rearrange("p (h l) -> p h l", l=64),
                             bc3(a_col, 64), ALU.mult, [PS[2], cols], [tmpS])
                          TT(AT[:].rearrange("p (h l) -> p h l", l=64), tmpS[:].rearrange("p (h l) -> p h l", l=64),
                             causal[:].unsqueeze(1).to_broadcast([64, 8, 64]), ALU.mult, [tmpS, causal], [AT])
                          yield
                          for h in range(8):
                              pb = PS[3 + h // 4]
                              MM(pb[0:64, (h % 4) * 128:(h % 4 + 1) * 128], AT[:, h * 64:(h + 1) * 64], v_tm[:, h * 128:(h + 1) * 128],
                                 True, True, [AT, v_tm], [pb])
                          for h in range(8):
                              MM(PS[0][0:64, h:h + 1], AT[:, h * 64:(h + 1) * 64], onesb[0:64, 0:1], True, True, [AT, onesb], [PS[0]])
                          for h in range(8):
                              pb = PS[5] if h < 4 else PS[2]
                              MM(pb[0:64, (h % 4) * 128:(h % 4 + 1) * 128], qT[:, h, cs_], Ctb[:, h * 128:(h + 1) * 128],
                                 True, True, [qT, Ctb], [pb])
                          for h in range(8):
                              MM(PS[0][0:64, 8 + h:9 + h], qT[:, h, cs_], nstb[:, h:h + 1], True, True, [qT, nstb], [PS[0]])
                          for hf in range(2):
                              sl = slice(hf * 512, (hf + 1) * 512)
                              TT(t1[:, sl].rearrange("p (h d) -> p h d", d=128), PS[3 + hf][0:64, :].rearrange("p (h d) -> p h d", d=128),
                                 bc3(b_col, 128)[:, hf * 4:(hf + 1) * 4, :], ALU.mult, [PS[3 + hf], cols], [t1])
                              TT(t2[:, sl].rearrange("p (h d) -> p h d", d=128), (PS[5] if hf == 0 else PS[2])[0:64, :].rearrange("p (h d) -> p h d", d=128),
                                 bc3(w_col, 128)[:, hf * 4:(hf + 1) * 4, :], ALU.mult, [PS[5] if hf == 0 else PS[2], cols], [t2])
                          TT(t1[:], t1[:], t2[:], ALU.add, [t1, t2], [t1])
                          TT(sm[0][:], PS[0][0:64, 0:8], b_col, ALU.mult, [PS[0], cols], [sm[0]])
                          TT(sm[1][:], PS[0][0:64, 8:16], w_col, ALU.mult, [PS[0], cols], [sm[1]])
                          TT(sm[0][:], sm[0][:], sm[1][:], ALU.add, [sm[0], sm[1]], [sm[0]])
                          TS(sm[5][:], sm[0][:], -1.0, None, ALU.mult, None, [sm[0]], [sm[5]])
                          TT(sm[0][:], sm[0][:], sm[5][:], ALU.max, [sm[0], sm[5]], [sm[0]])
                          TT(sm[0][:], sm[0][:], e_col, ALU.max, [sm[0], cols], [sm[0]])
                          RECIP(sm[2][:], sm[0][:], [sm[0]], [sm[2]])
                          TT(t1[:].rearrange("p (h d) -> p h d", d=128), t1[:].rearrange("p (h d) -> p h d", d=128),
                             bc3(sm[2][:], 128), ALU.mult, [t1, sm[2]], [t1])
                          TT(t2[:], t1[:], t1[:], ALU.mult, [t1], [t2])
                          k.op("dve", lambda: E["dve"].tensor_reduce(out=sm[3][:], in_=t2[:].rearrange("p (h d) -> p h d", d=128),
                                                                     axis=AX.X, op=ALU.add), [t2], [sm[3]])
                          TS(sm[3][:], sm[3][:], 1.0 / 128, EPS, ALU.mult, ALU.add, [sm[3]], [sm[3]])
                          ACT(sm[3][:], sm[3][:], AF.Sqrt, [sm[3]], [sm[3]])
                          RECIP(sm[4][:], sm[3][:], [sm[3]], [sm[4]])
                          TT(t1[:].rearrange("p (h d) -> p h d", d=128), t1[:].rearrange("p (h d) -> p h d", d=128),
                             bc3(sm[4][:], 128), ALU.mult, [t1, sm[4]], [t1])
                          TT(t1[:], t1[:], mnb[:], ALU.mult, [t1, mnb], [t1])
                          yield
                          for h in range(8):
                              TR(PS[2][:, h * 64:(h + 1) * 64], t1[:, h * 128:(h + 1) * 128], ident[0:64, 0:64], [t1, ident], [PS[2]])
                          TT(ymT[:, :, cs_], PS[2][:].rearrange("p (h l) -> p h l", l=64), gateT[:, :, cs_], ALU.mult,
                             [PS[2], gateT], [ymT])
                          yield
                          for h in range(8):
                              pb = PS[h // 4]
                              MM(pb[:, (h % 4) * 128:(h % 4 + 1) * 128], k_tm[:, h * 128:(h + 1) * 128], av_tm[:, h * 128:(h + 1) * 128],
                                 True, True, [k_tm, av_tm], [pb])
                          for h in range(8):
                              MM(PS[2][:, h:h + 1], k_tm[:, h * 128:(h + 1) * 128], abf[:, h:h + 1], True, True, [k_tm, abf], [PS[2]])
                          dcol = dbc[:, c, :]
                          TT(Ct[:].rearrange("p (h d) -> p h d", d=128), Ct[:].rearrange("p (h d) -> p h d", d=128),
                             dcol.unsqueeze(2).to_broadcast([128, 8, 128]), ALU.mult, [Ct, dbc], [Ct])
                          for hf in range(2):
                              sl = slice(hf * 512, (hf + 1) * 512)
                              TT(Ct[:, sl], Ct[:, sl], PS[hf][:, :], ALU.add, [Ct, PS[hf]], [Ct])
                          ACT(Ctb[:], Ct[:], AF.Copy, [Ct], [Ctb])
                          TT(nst[:], nst[:], dcol, ALU.mult, [nst, dbc], [nst])
                          TT(nst[:], nst[:], PS[2][:, 0:8], ALU.add, [nst, PS[2]], [nst])
                          CP(nstb[:], nst[:], [nst], [nstb])
                          yield


                  bg = chunk_gen()

                  def tick(n=1):
                      for _ in range(n):
                          next(bg, None)

                  for c in range(8):
                      Wb, wv = wget()
                      Wb2, wv2 = wget()
                      zT(PS[6], Wb, wv[1], 0, 128, hrhs, [hT])
                      tick()
                      zT(PS[7], Wb2, wv2[0], 0, 128, hrhs, [hT])
                      tick()
                      CP(vb[:, 0:2], cv[:, c, :], [cv], [vb])
                      ACT(tA[:], PS[6][:, 0:T], AF.Copy, [PS[6]], [tA])
                      TT(vb[:, 2:2 + T], tA[:], PS[7][:, 0:T], ALU.mult, [tA, PS[7]], [vb])
                      CP(cv[:, c, :], vb[:, T:T + 2], [vb], [cv])
                      zT(PS[6], Wb, wv[0], 0, 128, hrhs, [hT])
                      tick()
                      zT(PS[7], Wb2, wv2[1], 0, 128, hrhs, [hT])
                      tick()
                      TS(tB[:], vb[:, 0:T], convw[:, c * 3:c * 3 + 1], None, ALU.mult, None, [vb, convw], [tB])
                      STT(tB[:], vb[:, 1:T + 1], convw[:, c * 3 + 1:c * 3 + 2], tB[:], ALU.mult, ALU.add, [vb, convw, tB], [tB])
                      STT(tB[:], vb[:, 2:T + 2], convw[:, c * 3 + 2:c * 3 + 3], tB[:], ALU.mult, ALU.add, [vb, convw, tB], [tB])
                      ACT(tC[:], PS[7][:, 0:T], AF.Silu, [PS[7]], [tC])
                      TT(tB[:], tB[:], PS[6][:, 0:T], ALU.mult, [tB, PS[6]], [tB])
                      TT(ycT[:, c, :], tB[:], tC[:], ALU.mult, [tB, tC], [ycT])
                  for _ in bg:
                      pass

                  ck('B3')
                  switch(attn_tmps, chunk_tmps)
                  for d in rope_deps:
                      k._wait("sp", d)
                  k.dma("sp", rtab[:], rope_d[:, :, t0:t0 + T].rearrange("f p t -> p f t"), rtab, writes=[rtab])

                  def rope(psb, cosi, sini, dst):
                      ACT(tA[:], psb[:, 0:T], AF.Copy, [psb], [tA])
                      MM(PS[7][:, 0:T], rm32[:], tA[:], True, True, [rm32, tA], [PS[7]])
                      TT(tB[:], tA[:], rtab[:, cosi, :], ALU.mult, [tA, rtab], [tB])
                      TT(tC[:], PS[7][:, 0:T], rtab[:, sini, :], ALU.mult, [PS[7], rtab], [tC])
                      TT(dst, tB[:], tC[:], ALU.add, [tB, tC], [dst_b[0]])

                  Wb, wv = wget()
                  Wv_, wvv = wget()
                  dst_b = [kr]
                  for g2 in range(2):
                      zT(PS[0], Wb, wv[0], g2 * 128, 128, hrhs, [hT])
                      rope(PS[0], 2, 3, kr[:])
                      for s in range(2):
                          MM(PS[1][:, 0:T], selb[:, s * 128:(s + 1) * 128], kr[:], True, True, [selb, kr], [PS[1]])
                          CP(KTd[0:64, 0, g2 * 2 + s, 128:128 + T], PS[1][0:64, 0:T], [PS[1]], [KTd])
                          ACT(KTd[64:128, 1, g2 * 2 + s, 128:128 + T], PS[1][64:128, 0:T], AF.Copy, [PS[1]], [KTd])
                  for qb in range(NQB):
                      for kc in range(KC):
                          MM(PS[2][:, 0:256], hT[:, kc, qb * 128:(qb + 1) * 128], wvv[0][:, kc, :], kc == 0, kc == KC - 1,
                             [hT, Wv_], [PS[2]])
                      pv = PS[2][:, 0:256].rearrange("p (g d) -> p g d", d=64)
                      CP(Vp[:, 1 + qb, :, 0, 0:64], pv, [PS[2]], [Vp])
                      ACT(Vp[:, 1 + qb, :, 1, 64:128], pv, AF.Copy, [PS[2]], [Vp])
                  dst_b = [qr]
                  for j2 in range(4):
                      Wb, wv = wget()
                      Wb2, wv2 = wget()
                      for jj in range(2):
                          j = j2 * 2 + jj
                          g = j // 2
                          zT(PS[0], Wb, wv[0], jj * 128, 128, hrhs, [hT])
                          rope(PS[0], 0, 1, qr[:])
                          zT(PS[1], Wb2, wv2[0], jj * 128, 128, hrhs, [hT])
                          ACT(sgT[:], PS[1][:, 0:T], AF.Silu, [PS[1]], [sgT])
                          def emit_scores(qb):
                              psb = PS[2] if qb % 2 == 0 else PS[5]
                              for hf in range(2):
                                  for kbi in range(2):
                                      o0 = (hf * 2 + kbi) * 128
                                      MM(psb[:, o0:o0 + 128], KTd[:, hf, g, (qb + kbi) * 128:(qb + kbi + 1) * 128],
                                         qr[:, qb * 128:(qb + 1) * 128], True, True, [KTd, qr], [psb])

                          emit_scores(0)
                          for qb in range(NQB):
                              if qb + 1 < NQB:
                                  emit_scores(qb + 1)
                              psb = PS[2] if qb % 2 == 0 else PS[5]
                              ex_, PT_ = exs[qb % 2], PTs[qb % 2]
                              ACT(ex_[:], psb[:, :], AF.Exp, [psb], [ex_])
                              mk = amask0 if (it == 0 and qb == 0) else amask
                              TT(PT_[:].rearrange("p (h r) -> p h r", r=256), ex_[:].rearrange("p (h r) -> p h r", r=256),
                                 mk[:].unsqueeze(1).to_broadcast([128, 2, 256]), ALU.mult, [ex_, mk], [PT_])
                              n = 0
                              for hf in range(2):
                                  for kbi in range(2):
                                      o0 = (hf * 2 + kbi) * 128
                                      MM(PS[3][:, 0:128], Vp[:, qb + kbi, g, hf, :], PT_[:, o0:o0 + 128], n == 0, n == 3, [Vp, PT_], [PS[3]])
                                      n += 1
                              n = 0
                              for hf in range(2):
                                  for kbi in range(2):
                                      o0 = (hf * 2 + kbi) * 128
                                      MM(PS[4][:, 0:128], onespad[:, hf * 128:(hf + 1) * 128], PT_[:, o0:o0 + 128], n == 0, n == 3,
                                         [onespad, PT_], [PS[4]])
                                      n += 1
                              TS(tD[:, 0:128], PS[4][:, 0:128], sinke[:, j:j + 1], None, ALU.add, None, [PS[4], sinke], [tD])
                              RECIP(tD[:, 0:128], tD[:, 0:128], [tD], [tD])
                              TT(tD[:, 0:128], tD[:, 0:128], PS[3][:, 0:128], ALU.mult, [tD, PS[3]], [tD])
                              TT(yaT[:, j, qb * 128:(qb + 1) * 128], tD[:, 0:128], sgT[:, qb * 128:(qb + 1) * 128], ALU.mult, [tD, sgT], [yaT])
                  CP(KTd[:, :, :, 0:128], KTd[:, :, :, T:T + 128], [KTd], [KTd])
                  CP(Vp[:, 0, :, :, :], Vp[:, NQB, :, :, :], [Vp], [Vp])

                  ck('C')
                  ys = (ycT, ymT, yaT)
                  switch(d_tmps, attn_tmps)
                  for fb in range(16):
                      Wg, wg = wget()
                      Wg2, wg2 = wget()
                      zT(PS[0], Wg, wg[0], 0, 128, hrhs, [hT])
                      zT(PS[1], Wg, wg[1], 0, 128, hrhs, [hT])
                      zT(PS[2], Wg2, wg2[0], 0, 128, hrhs, [hT])
                      for j in range(3):
                          Wu, wu = wget()
                          zT(PS[3 + j], Wu, wu[0], 0, 128, lambda kc, j=j: ys[j][:, kc, :], [ys[j]], kcs=8)
                      for j in range(3):
                          ACT(tA[:], PS[j][:, 0:T], AF.Sigmoid, [PS[j]], [tA])
                          if j == 0:
                              TT(tB[:], tA[:], PS[3][:, 0:T], ALU.mult, [tA, PS[3]], [tB])
                          else:
                              TT(tC[:], tA[:], PS[3 + j][:, 0:T], ALU.mult, [tA, PS[3 + j]], [tC])
                              if j == 1:
                                  TT(tB[:], tB[:], tC[:], ALU.add, [tB, tC], [tB])
                              else:
                                  TT(mgT[:, fb, :], tB[:], tC[:], ALU.add, [tB, tC], [mgT])
                  mrhs = lambda kc: mgT[:, kc, :]
                  switch([big], [qT, kT, vT, gateT])
                  for q8 in range(8):
                      Wb, wv = wget()
                      for i in range(2):
                          fb = q8 * 2 + i
                          pb = PS[(q8 % 2) * 2 + i]
                          zT(pb, Wb, wv[0], i * 128, 128, mrhs, [mgT])
                          if i % 2 == 0:
                              ACT(big[:, fb, :], pb[:, 0:T], AF.Copy, [pb], [big])
                          else:
                              CP(big[:, fb, :], pb[:, 0:T], [pb], [big])
                  norm_stats(lambda kc: (big, big[:, kc, :]))
                  for kc in range(KC):
                      b, ap = load_x(kc)
                      STT(tB[:], big[:, kc, :], gains[:, 16 + kc:17 + kc], rstd[:], ALU.mult, ALU.mult, [big, gains, rstd], [tB])
                      ob = xo[kc % 2]
                      TT(ob[:], tB[:], ap, ALU.add, [tB, b], [ob])
                      CP(mgT[:, kc, :], ob[:], [ob], [mgT])
                      k.dma("sp", xT_d[kc, :, t0:t0 + T], ob[:], ob, reads=[ob], writes=[treg] if kc == 0 else [])
                  st_deps = [(xo[0].sem, xo[0].cnt), (xo[1].sem, xo[1].cnt)]
                  for qb in range(NQB):
                      k.dma("sp", p32[:], p_in[l, t0 + qb * 128:t0 + (qb + 1) * 128, :], p32, writes=[p32])
                      CP(pbf[:], p32[:], [p32], [pbf])
                      ppt = PS[4][:].bitcast(BF16)
                      for c2 in range(2):
                          TR(ppt[:, c2 * 128:(c2 + 1) * 128], pbf[:, c2 * 128:(c2 + 1) * 128], identb[:], [pbf, identb], [PS[4]])
                      CP(pT[:, :, qb * 128:(qb + 1) * 128], ppt[:, 0:256].rearrange("p (c t) -> p c t", t=128), [PS[4]], [pT])
                  for q8 in range(8):
                      Wb, wv = wget()
                      Wp, wp = wget()
                      for i in range(2):
                          fb = q8 * 2 + i
                          pa = PS[(q8 % 2) * 2 + i]
                          pe_ = PS[4 + (q8 % 2) * 2 + i]
                          zT(pa, Wb, wv[0], i * 128, 128, mrhs, [mgT])
                          zT(pe_, Wp, wp[0], i * 128, 128, lambda kc: pT[:, kc, :], [pT], kcs=2)
                          ACT(tA[:], pa[:, 0:T], AF.Sigmoid, [pa], [tA])
                          TT(big[:, fb, :], tA[:], pe_[:, 0:T], ALU.mult, [tA, pe_], [big])
                  norm_stats(lambda kc: (big, big[:, kc, :]))
                  for d in st_deps:
                      k._wait("sp", d)
                  last = (l == DEPTH - 1)
                  for kc in range(KC):
                      b, ap = load_x(kc)
                      STT(tB[:], big[:, kc, :], gains[:, 32 + kc:33 + kc], rstd[:], ALU.mult, ALU.mult, [big, gains, rstd], [tB])
                      ob = xo[kc % 2]
                      TT(ob[:], tB[:], ap, ALU.add, [tB, b], [ob])
                      k.dma("sp", xT_d[kc, :, t0:t0 + T], ob[:], ob, reads=[ob], writes=[treg] if kc == 0 else [])
                  fin = [(xo[0].sem, xo[0].cnt), (xo[1].sem, xo[1].cnt)]
                  treg.w = None
                  for d in fin:
                      k._wait("sp", d)

        except _Stop:
            pass
        switch([xcol, xrow], [big, qT, kT, vT, gateT])
        for blk in range(NTOK // 128):
            k.dma("sp", xcol[:], xT_d[:, :, blk * 128:(blk + 1) * 128].rearrange("k p t -> p k t"), xcol, writes=[xcol])
            for g4 in range(4):
                pb = PS[g4 % 2]
                for i in range(4):
                    kc = g4 * 4 + i
                    TR(pb[:, i * 128:(i + 1) * 128], xcol[:, kc, :], ident[:], [xcol, ident], [pb])
                CP(xrow[:, g4 * 512:(g4 + 1) * 512], pb[:, :], [pb], [xrow])
            k.dma("sp", y_out[blk * 128:(blk + 1) * 128, :], xrow[:], xrow, reads=[xrow])
        k._wait("sp", (xrow.sem, xrow.cnt))
        for e in ("pe", "act", "dve"):
            if k.sems[e]:
                c = k.cnt[e]
                ep = (c - 1) // EPOCH
                k._wait("sp", (k.sems[e][ep], c - ep * EPOCH))
    return nc


def host_consts():
    ident = np.eye(128, dtype=np.float32)
    rm = np.zeros((128, 128), np.float32)
    for m in range(128):
        rm[(m // 64) * 64 + ((m % 64) + 32) % 64, m] = 1.0
    sel = np.zeros((128, 256), np.float32)
    for s in range(2):
        for m in range(128):
            sel[s * 64 + (m % 64), s * 128 + m] = 1.0
    vec = np.zeros((128, 4), np.float32)
    j = np.arange(128) % 32
    vec[:, 0] = np.power(np.float32(10000.0), (-2.0 * j.astype(np.float32) / 64).astype(np.float32)).astype(np.float32)
    vec[:, 1] = np.where((np.arange(128) % 64) < 32, -1.0, 1.0)
    causal = (np.arange(64)[:, None] <= np.arange(64)[None, :]).astype(np.float32)
    kk = np.arange(128)[:, None]
    qq = np.arange(128)[None, :]
    amask = np.concatenate([(kk > qq), (kk <= qq)], axis=1).astype(np.float32)
    onespad = np.zeros((128, 256), np.float32)
    onespad[:, 0:64] = 1.0
    onespad[:, 128 + 64:256] = 1.0
    return {"c_ident": ident, "c_rm": rm, "c_sel": sel, "c_vec": vec, "c_causal": causal,
            "c_amask": amask, "c_onespad": onespad}


def layout_inputs(b, x, p, positions, w_in, conv_w, b_igate, b_fgate, mlstm_norm, attn_sinks,
                  w_up_conv, w_up_mlstm, w_up_attn, w_out, pre_norm, post_norm, w_ple,
                  w_ple_gate, ple_norm, consts):
    L = w_in.shape[0]
    f = lambda a: np.ascontiguousarray(np.asarray(a, dtype=np.float32))
    convw = f(np.asarray(conv_w).reshape(L, 3, 8, 128).transpose(0, 3, 2, 1).reshape(L, 128, 24))
    g = lambda a: np.asarray(a).reshape(L, 16, 128).transpose(0, 2, 1)
    gains = f(np.concatenate([g(pre_norm), g(post_norm), g(ple_norm)], axis=2))
    sk = np.asarray(attn_sinks).reshape(L, 8, 2)
    sinks = f(np.repeat(sk.transpose(0, 2, 1), 64, axis=1))
    m = {"x": f(x[b]), "p": f(np.asarray(p)[:, b]), "pos": np.ascontiguousarray(np.asarray(positions)[b].astype(np.int32)),
         "w_in": f(w_in), "w_up_conv": f(w_up_conv), "w_up_mlstm": f(w_up_mlstm), "w_up_attn": f(w_up_attn),
         "w_out": f(w_out), "w_ple": f(w_ple), "w_ple_gate": f(w_ple_gate), "convw": convw, "gains": gains,
         "b_igate": f(b_igate), "b_fgate": f(b_fgate), "mlstm_norm": f(mlstm_norm), "sinks": sinks}
    m.update(consts)
    return m


def kernel(**inputs):
    x = np.asarray(inputs["x"])
    B, S, _ = x.shape
    L = np.asarray(inputs["w_in"]).shape[0]
    nc = build(S, L, T=512)
    consts = host_consts()
    args = [inputs[n] for n in ("x", "p", "positions", "w_in", "conv_w", "b_igate", "b_fgate", "mlstm_norm",
                                "attn_sinks", "w_up_conv", "w_up_mlstm", "w_up_attn", "w_out", "pre_norm",
                                "post_norm", "w_ple", "w_ple_gate", "ple_norm")]
    maps = [layout_inputs(c % B, *args, consts) for c in range(B)]
    in_maps = [maps[c % B] for c in range(8)]
    res = run_bass_kernel_spmd(nc, in_maps, core_ids=list(range(8)))
    return np.stack([np.asarray(res.results[b]["y"], dtype=np.float32) for b in range(B)], axis=0)
```

```python
import numpy as np
from contextlib import ExitStack
import concourse.bass as bass
import concourse.mybir as mybir
from concourse.bass_utils import run_bass_kernel_spmd

F32 = mybir.dt.float32
BF16 = mybir.dt.bfloat16
I32 = mybir.dt.int32
AF = mybir.ActivationFunctionType
ALU = mybir.AluOpType
AX = mybir.AxisListType

D = 2048
KC = 16
IN_W = 17936
EPS = 1e-6
EPOCH = 30000
WB_ELEMS = 4096
NWB = 5
LIVE = 2

O_CB, O_CC, O_CX, O_CG = 0, 1024, 2048, 3072
O_MQ, O_MK, O_MV, O_MO, O_MI, O_MF, O_MG = 4096, 5120, 6144, 7168, 8192, 8200, 8208
O_AQ, O_AK, O_AV, O_AG = 9232, 10256, 10512, 10768
O_GT = 11792


class Buf:
    __slots__ = ("t", "w", "r", "sem", "cnt")

    def __init__(self, t=None, sem=None):
        self.t = t
        self.w = None
        self.r = {}
        self.sem = sem
        self.cnt = 0

    def __getitem__(self, k):
        return self.t[k]


class KB:
    def __init__(self, nc, es):
        self.nc = nc
        self.es = es
        self.eng = {"pe": nc.tensor, "act": nc.scalar, "dve": nc.vector,
                    "pool": nc.gpsimd, "sp": nc.sync}
        self.cnt = {e: 0 for e in self.eng}
        self.sems = {e: [] for e in self.eng}
        self.waited = {e: {} for e in self.eng}
        self.nsem = 0

    def newsem(self, name):
        self.nsem += 1
        return self.es.enter_context(self.nc.semaphore(name))

    def sb(self, name, shape, dt, dma=False):
        t = self.es.enter_context(self.nc.sbuf_tensor("sb_" + name, shape, dt))
        return Buf(t, self.newsem("s_" + name) if dma else None)

    def ps(self, name, shape, dt=F32):
        t = self.es.enter_context(self.nc.psum_tensor(name, shape, dt))
        return Buf(t)

    def _mark(self, e):
        c = self.cnt[e]
        ep = c // EPOCH
        while len(self.sems[e]) <= ep:
            self.sems[e].append(self.newsem("c_%s_%d" % (e, len(self.sems[e]))))
        self.cnt[e] = c + 1
        return (self.sems[e][ep], c - ep * EPOCH + 1)

    def _wait(self, e, dep):
        if dep is None:
            return
        sem, val = dep
        if e == "pe" and any(sem is s for s in self.sems["pe"]):
            return
        k = id(sem)
        if self.waited[e].get(k, 0) >= val:
            return
        self.waited[e][k] = val
        self.eng[e].wait_ge(sem, val)

    def op(self, e, fn, reads=(), writes=()):
        for b in reads:
            self._wait(e, b.w)
        for b in writes:
            self._wait(e, b.w)
            for d in list(b.r.values()):
                self._wait(e, d)
        ins = fn()
        m = self._mark(e)
        ins.then_inc(m[0], 1)
        for b in reads:
            b.r[id(m[0])] = m
        for b in writes:
            b.w = m
            b.r = {}
        return ins

    def dma(self, q, out_ap, in_ap, semb, reads=(), writes=()):
        for b in reads:
            self._wait(q, b.w)
        for b in writes:
            self._wait(q, b.w)
            for d in list(b.r.values()):
                self._wait(q, d)
        ins = self.eng[q].dma_start(out=out_ap, in_=in_ap)
        semb.cnt += 16
        ins.then_inc(semb.sem, 16)
        m = (semb.sem, semb.cnt)
        for b in reads:
            b.r[id(semb.sem)] = m
        for b in writes:
            b.w = m
            b.r = {}

    def wait_all(self, q, bufs):
        for b in bufs:
            self._wait(q, b.w)


class _Stop(Exception):
    pass


def build(NTOK, DEPTH, T=256, debug=None, stage=None):
    nc = bass.Bass("TRN2", target_bir_lowering=False)
    NT = NTOK // T
    NCH = T // 64
    NQB = T // 128
    dbg = {}

    def din(name, shape, dt=F32):
        return nc.dram_tensor(name, shape, dt, kind="ExternalInput").ap()

    x_in = din("x", [NTOK, D])
    p_in = din("p", [DEPTH, NTOK, 256])
    pos_in = din("pos", [NTOK], I32)
    w_in = din("w_in", [DEPTH, D, IN_W])
    w_upc = din("w_up_conv", [DEPTH, 1024, D])
    w_upm = din("w_up_mlstm", [DEPTH, 1024, D])
    w_upa = din("w_up_attn", [DEPTH, 1024, D])
    w_out = din("w_out", [DEPTH, D, D])
    w_ple = din("w_ple", [DEPTH, 256, D])
    w_pg = din("w_ple_gate", [DEPTH, D, D])
    convw_in = din("convw", [DEPTH, 128, 24])
    gains_in = din("gains", [DEPTH, 128, 48])
    big_in = din("b_igate", [DEPTH, 8])
    bfg_in = din("b_fgate", [DEPTH, 8])
    mnorm_in = din("mlstm_norm", [DEPTH, 1024])
    sinks_in = din("sinks", [DEPTH, 128, 8])
    c_ident = din("c_ident", [128, 128])
    c_rm = din("c_rm", [128, 128])
    c_sel = din("c_sel", [128, 256])
    c_vec = din("c_vec", [128, 4])
    c_causal = din("c_causal", [64, 64])
    c_amask = din("c_amask", [128, 256])
    c_onespad = din("c_onespad", [128, 256])
    y_out = nc.dram_tensor("y", [NTOK, D], F32, kind="ExternalOutput").ap()
    xT_d = nc.dram_tensor("xT_scr", [KC, 128, NTOK], F32, kind="Internal").ap()
    rope_d = nc.dram_tensor("rope_scr", [4, 128, NTOK], F32, kind="Internal").ap()
    if debug:
        for nm, shp in debug.items():
            dbg[nm] = nc.dram_tensor("dbg_" + nm, shp, F32, kind="ExternalOutput").ap()

    es = ExitStack()
    with es:
        k = KB(nc, es)
        E = {e: k.eng[e] for e in k.eng}
        xT_reg = [Buf() for _ in range(NT)]
        rope_reg = Buf()

        ident = k.sb("ident", [128, 128], F32, dma=True)
        identb = k.sb("identb", [128, 128], BF16)
        rm32 = k.sb("rm32", [128, 128], F32, dma=True)
        selb = k.sb("selb", [128, 256], BF16)
        sel32 = k.sb("sel32", [128, 256], F32, dma=True)
        cvec = k.sb("cvec", [128, 4], F32, dma=True)
        causal = k.sb("causal", [64, 64], F32, dma=True)
        amask = k.sb("amask", [128, 256], F32, dma=True)
        amask0 = k.sb("amask0", [128, 256], F32)
        onespad32 = k.sb("onespad32", [128, 256], F32, dma=True)
        onespad = k.sb("onespad", [128, 256], BF16)
        ones32 = k.sb("ones32", [128, 128], F32)
        onesb = k.sb("onesb", [128, 8], BF16)
        onesbf = k.sb("onesbf", [128, 128], BF16)
        sqb = [k.sb("sqb%d" % i, [128, T], BF16) for i in range(2)]
        for b, src in ((ident, c_ident), (rm32, c_rm), (sel32, c_sel), (cvec, c_vec),
                       (causal, c_causal), (amask, c_amask), (onespad32, c_onespad)):
            k.dma("sp", b[:], src[:], b, writes=[b])
        k.op("dve", lambda: E["dve"].tensor_copy(out=identb[:], in_=ident[:]), [ident], [identb])
        k.op("dve", lambda: E["dve"].tensor_copy(out=selb[:], in_=sel32[:]), [sel32], [selb])
        k.op("dve", lambda: E["dve"].tensor_copy(out=onespad[:], in_=onespad32[:]), [onespad32], [onespad])
        k.op("dve", lambda: E["dve"].memset(ones32[:], 1.0), [], [ones32])
        k.op("dve", lambda: E["dve"].memset(onesb[:], 1.0), [], [onesb])
        k.op("dve", lambda: E["dve"].memset(onesbf[:], 1.0), [], [onesbf])
        k.op("dve", lambda: E["dve"].tensor_copy(out=amask0[:], in_=amask[:]), [amask], [amask0])
        k.op("dve", lambda: E["dve"].memset(amask0[:, 0:128], 0.0), [], [amask0])

        PS = [k.ps("ps%d" % i, [128, 512], F32) for i in range(8)]

        WB = [k.sb("wb%d" % i, [128, WB_ELEMS], BF16, dma=True) for i in range(NWB)]
        hT = k.sb("hT", [128, KC, T], BF16)
        ycT = k.sb("ycT", [128, 8, T], BF16)
        ymT = k.sb("ymT", [128, 8, T], BF16)
        yaT = k.sb("yaT", [128, 8, T], BF16)
        XW = 16 * T
        arX = es.enter_context(nc.sbuf_tensor("arenaX", [128, XW], F32))
        YW = 5120
        arY = es.enter_context(nc.sbuf_tensor("arenaY", [128, YW], F32))

        def vw(ar, lo, hi, dt=F32, parts=128, sem=False, shape=None):
            ap = ar[0:parts, lo:hi]
            if dt is not F32:
                ap = ap.bitcast(dt)
            if shape:
                ap = ap.rearrange(shape[0], **shape[1])
            return Buf(ap, k.newsem("s_v%d" % k.nsem) if sem else None)

        def switch(new, old):
            for nv in new:
                for ov in old:
                    deps = list(ov.r.values()) + ([ov.w] if ov.w else [])
                    for d in deps:
                        kk = id(d[0])
                        if kk not in nv.r or nv.r[kk][1] < d[1]:
                            nv.r[kk] = d

        big = vw(arX, 0, 16 * T, shape=("p (a b) -> p a b", dict(b=T)))
        mgT = vw(arY, 0, 8 * T, BF16, shape=("p (a b) -> p a b", dict(b=T)))
        xs = [k.sb("xs%d" % i, [128, T], F32, dma=True) for i in range(3)]
        xo = [k.sb("xo%d" % i, [128, T], F32, dma=True) for i in range(2)]
        acc = k.sb("acc", [128, T], F32)
        sqt = k.sb("sqt", [128, T], F32)
        rstd = k.sb("rstd", [128, T], F32)
        tA = k.sb("tA", [128, T], F32)
        tB = k.sb("tB", [128, T], F32)
        tC = k.sb("tC", [128, T], F32)
        tD = k.sb("tD", [128, T], F32)
        vb = k.sb("vb", [128, T + 2], F32)
        cv = k.sb("cv", [128, 8, 2], F32)
        convw = k.sb("convw", [128, 24], F32, dma=True)
        gains = k.sb("gains", [128, 48], F32, dma=True)
        bi = k.sb("bi", [8, 1], F32, dma=True)
        bfn = k.sb("bfn", [8, 1], F32, dma=True)
        mnb = k.sb("mnb", [64, 1024], F32, dma=True)
        sinke = k.sb("sinke", [128, 8], F32, dma=True)
        h3 = ("p (a b) -> p a b", dict(b=T))
        qT = vw(arX, 0, 4 * T, BF16, shape=h3)
        kT = vw(arX, 4 * T, 8 * T, BF16, shape=h3)
        vT = vw(arX, 8 * T, 12 * T, BF16, shape=h3)
        gateT = vw(arX, 12 * T, 16 * T, BF16, shape=h3)
        rA = [vw(arY, i * T, (i + 1) * T, parts=8) for i in range(10)]
        assert 10 * T <= YW
        mus = k.sb("mus", [8, NCH], F32)
        mup = k.sb("mup", [8, NCH], F32)
        dec = k.sb("dec", [8, NCH], F32)
        dexp = k.sb("dexp", [8, NCH, 8], F32)
        dbc = k.sb("dbc", [128, NCH, 8], F32)
        cB = k.sb("cB", [8, 1], F32)
        cM = k.sb("cM", [8, 1], F32)
        cols = k.sb("cols", [64, NCH, 4, 8], F32)
        abf = k.sb("abf", [64, 8], BF16)
        t1 = vw(arY, 0, 1024, parts=64)
        t2 = vw(arY, 1024, 2048, parts=64)
        tmpS = vw(arY, 2048, 2560, parts=64)
        AT = vw(arY, 2560, 2816, BF16, parts=64)
        k_tm = vw(arY, 2816, 3328, BF16, parts=64)
        v_tm = vw(arY, 3328, 3840, BF16, parts=64)
        av_tm = vw(arY, 3840, 4352, BF16, parts=64)
        chunk_tmps = [t1, t2, tmpS, AT, k_tm, v_tm, av_tm]
        sm = [k.sb("sm%d" % i, [64, 8], F32) for i in range(6)]
        Ct = k.sb("Ct", [128, 1024], F32)
        Ctb = k.sb("Ctb", [128, 1024], BF16)
        nst = k.sb("nst", [128, 8], F32)
        nstb = k.sb("nstb", [128, 8], BF16)
        KTd = k.sb("KTd", [128, 2, 4, 128 + T], BF16)
        Vp = k.sb("Vp", [128, NQB + 1, 4, 2, 128], BF16)
        rtab = vw(arY, 0, 4 * T, sem=True, shape=("p (a b) -> p a b", dict(b=T)))
        o_ = 4 * T
        ex = vw(arY, o_, o_ + 512)
        sgT = vw(arY, o_ + 512, o_ + 512 + T)
        o_ = o_ + 512 + T
        PT = vw(arY, o_, o_ + 256, BF16)
        qr = vw(arY, o_ + 256, o_ + 256 + T // 2, BF16)
        kr = vw(arY, o_ + 256 + T // 2, o_ + 256 + T, BF16)
        o_ = o_ + 256 + T
        ex2 = vw(arY, o_, o_ + 512)
        PT2 = vw(arY, o_ + 512, o_ + 768, BF16)
        assert o_ + 768 <= YW
        exs = [ex, ex2]
        PTs = [PT, PT2]
        attn_tmps = [rtab, ex, sgT, PT, qr, kr, ex2, PT2]
        o_ = 8 * T
        p32 = vw(arY, o_, o_ + 256, sem=True)
        pbf = vw(arY, o_ + 256, o_ + 384, BF16)
        pT = vw(arY, o_ + 384, o_ + 384 + T, BF16, shape=("p (a b) -> p a b", dict(b=T)))
        assert o_ + 384 + T <= YW
        d_tmps = [mgT, p32, pbf, pT]

        def ACT(out, in_, func, reads, writes, bias=None, scale=None):
            kw = {}
            if bias is not None:
                kw["bias"] = bias
            if scale is not None:
                kw["scale"] = scale
            return k.op("act", lambda: E["act"].activation(out=out, in_=in_, func=func, **kw), reads, writes)

        def TT(out, in0, in1, op, reads, writes, e="dve"):
            return k.op(e, lambda: E[e].tensor_tensor(out=out, in0=in0, in1=in1, op=op), reads, writes)

        def TS(out, in0, s1, s2, op0, op1, reads, writes, e="dve"):
            if s2 is None:
                return k.op(e, lambda: E[e].tensor_scalar(out=out, in0=in0, scalar1=s1, scalar2=None, op0=op0), reads, writes)
            return k.op(e, lambda: E[e].tensor_scalar(out=out, in0=in0, scalar1=s1, scalar2=s2, op0=op0, op1=op1), reads, writes)

        def STT(out, in0, sc, in1, op0, op1, reads, writes, e="dve"):
            return k.op(e, lambda: E[e].scalar_tensor_tensor(out=out, in0=in0, scalar=sc, in1=in1, op0=op0, op1=op1), reads, writes)

        def CP(out, in_, reads, writes, e="dve"):
            return k.op(e, lambda: E[e].tensor_copy(out=out, in_=in_), reads, writes)

        def RECIP(out, in_, reads, writes):
            return k.op("dve", lambda: E["dve"].reciprocal(out=out, in_=in_), reads, writes)

        def MM(out, lhsT, rhs, start, stop, reads, writes):
            return k.op("pe", lambda: E["pe"].matmul(out, lhsT, rhs, start=start, stop=stop), reads, writes)

        def TR(out, in_, idn, reads, writes):
            return k.op("pe", lambda: E["pe"].transpose(out, in_, idn), reads, writes)

        def rsqrt_to(out, in_ps, scale, reads_b, tmp):
            TS(tmp[:], in_ps, scale, EPS, ALU.mult, ALU.add, reads_b, [tmp])
            ACT(tmp[:], tmp[:], AF.Sqrt, [tmp], [tmp])
            RECIP(out[:], tmp[:], [tmp], [out])

        wstate = {"specs": [], "issued": 0, "used": 0}

        def wspec(wten, K, pieces):
            wstate["specs"].append((wten, K, pieces))

        def wissue():
            i = wstate["issued"]
            wten, K, pieces = wstate["specs"][i]
            b = WB[i % NWB]
            kc = K // 128
            off = 0
            src = wten.rearrange("(kc p) c -> p kc c", p=128)
            first = True
            for (c0, n) in pieces:
                dst = b[:, off:off + kc * n].rearrange("p (k c) -> p k c", c=n)
                k.dma("pool", dst, src[:, :, c0:c0 + n], b, writes=[b] if first else [])
                if not first:
                    b.w = (b.sem, b.cnt)
                first = False
                off += kc * n
            wstate["issued"] = i + 1

        def wget():
            u = wstate["used"]
            while wstate["issued"] < min(len(wstate["specs"]), u + NWB - (LIVE - 1)):
                wissue()
            wten, K, pieces = wstate["specs"][u]
            b = WB[u % NWB]
            kc = K // 128
            views = []
            off = 0
            for (c0, n) in pieces:
                views.append(b[:, off:off + kc * n].rearrange("p (k c) -> p k c", c=n))
                off += kc * n
            wstate["used"] = u + 1
            return b, views

        def plan_weights(l):
            W = w_in[l]
            wspec(W, D, [(O_MI, 8), (O_MF, 8)])
            for o in (O_MQ, O_MK, O_MV):
                for hp in range(4):
                    wspec(W, D, [(o + hp * 256, 256)])
            for hp in range(4):
                wspec(W, D, [(O_MO + hp * 256, 256)])
                wspec(W, D, [(O_MG + hp * 256, 256)])
            for c in range(8):
                wspec(W, D, [(O_CB + c * 128, 128), (O_CC + c * 128, 128)])
                wspec(W, D, [(O_CX + c * 128, 128), (O_CG + c * 128, 128)])
            wspec(W, D, [(O_AK, 256)])
            wspec(W, D, [(O_AV, 256)])
            for j2 in range(4):
                wspec(W, D, [(O_AQ + j2 * 256, 256)])
                wspec(W, D, [(O_AG + j2 * 256, 256)])
            for fb in range(16):
                wspec(W, D, [(O_GT + j * 2048 + fb * 128, 128) for j in range(2)])
                wspec(W, D, [(O_GT + 2 * 2048 + fb * 128, 128)])
                wspec(w_upc[l], 1024, [(fb * 128, 128)])
                wspec(w_upm[l], 1024, [(fb * 128, 128)])
                wspec(w_upa[l], 1024, [(fb * 128, 128)])
            for q8 in range(8):
                wspec(w_out[l], D, [(q8 * 256, 256)])
            for q8 in range(8):
                wspec(w_pg[l], D, [(q8 * 256, 256)])
                wspec(w_ple[l], 256, [(q8 * 256, 256)])

        for l in range(DEPTH):
            for it in range(NT):
                plan_weights(l)

        def zT(psb, Wb, wv, c0, ncol, rhs_fn, rhs_bufs, kcs=KC, pslice=None):
            o = pslice if pslice is not None else psb[0:ncol, 0:T]
            for kc in range(kcs):
                MM(o, wv[:, kc, c0:c0 + ncol], rhs_fn(kc), kc == 0, kc == kcs - 1, [Wb] + rhs_bufs, [psb])

        hrhs = lambda kc: hT[:, kc, :]

        xrow = vw(arX, 0, D, sem=True)
        xcol = vw(arX, D, 2 * D, sem=True, shape=("p (a b) -> p a b", dict(b=128)))
        for blk in range(NTOK // 128):
            k.dma("sp", xrow[:], x_in[blk * 128:(blk + 1) * 128, :], xrow, writes=[xrow])
            for g4 in range(4):
                pb = PS[g4 % 2]
                for i in range(4):
                    kc = g4 * 4 + i
                    TR(pb[:, i * 128:(i + 1) * 128], xrow[:, kc * 128:(kc + 1) * 128], ident[:], [xrow, ident], [pb])
                CP(xcol[:, g4 * 4:(g4 + 1) * 4, :], pb[:].rearrange("p (a b) -> p a b", b=128), [pb], [xcol])
            treg = xT_reg[(blk * 128) // T]
            k.dma("sp", xT_d[:, :, blk * 128:(blk + 1) * 128].rearrange("k p t -> p k t"), xcol[:], xcol,
                  reads=[xcol], writes=[treg])
        posi = vw(arX, 2 * D, 2 * D + T, I32, sem=True)
        posf = vw(arX, 2 * D + T, 2 * D + 2 * T)
        rs0 = Buf(None, k.newsem("s_rope0"))
        rs1 = Buf(None, k.newsem("s_rope1"))
        for it in range(NT):
            t0 = it * T
            k.dma("sp", posi[:], pos_in[t0:t0 + T].partition_broadcast(128), posi, writes=[posi])
            CP(posf[:], posi[:], [posi], [posf])
            TS(tA[:], posf[:], cvec[:, 0:1], None, ALU.mult, None, [posf, cvec], [tA])
            def rred(dst, src, addc):
                TS(sqt[:], src[:], addc, None, ALU.add, None, [src], [sqt])
                TS(acc[:], sqt[:], 1.0 / (2.0 * np.pi), None, ALU.mult, None, [sqt], [acc])
                CP(posi[:], acc[:], [acc], [posi])
                CP(acc[:], posi[:], [posi], [acc])
                STT(dst[:], acc[:], -2.0 * np.pi, sqt[:], ALU.mult, ALU.add, [acc, sqt], [dst])
                TS(acc[:], dst[:], np.pi, -2.0 * np.pi, ALU.is_gt, ALU.mult, [dst], [acc])
                TT(dst[:], dst[:], acc[:], ALU.add, [dst, acc], [dst])
                TS(acc[:], dst[:], -np.pi, 2.0 * np.pi, ALU.is_lt, ALU.mult, [dst], [acc])
                TT(dst[:], dst[:], acc[:], ALU.add, [dst, acc], [dst])
                TS(dst[:], dst[:], -3.1415925, 3.1415925, ALU.max, ALU.min, [dst], [dst])
            rred(tB, tA, 0.0)
            ACT(tB[:], tB[:], AF.Sin, [tB], [tB])
            TS(tB[:], tB[:], -1.0, None, ALU.mult, None, [tB], [tB])
            rred(tC, tA, 0.5 * np.pi)
            ACT(tC[:], tC[:], AF.Sin, [tC], [tC])
            TS(tC[:], tC[:], -1.0, None, ALU.mult, None, [tC], [tC])
            TS(xo[0][:], tC[:], -1.0, None, ALU.mult, None, [tC], [xo[0]])
            k.dma("sp", rope_d[2, :, t0:t0 + T], xo[0][:], xo[0], reads=[xo[0]], writes=[rope_reg])
            TS(xo[1][:], tC[:], -0.125, None, ALU.mult, None, [tC], [xo[1]])
            k.dma("sp", rope_d[0, :, t0:t0 + T], xo[1][:], xo[1], reads=[xo[1]], writes=[])
            rope_reg.w = None
            TS(tD[:], tB[:], cvec[:, 1:2], -1.0, ALU.mult, ALU.mult, [tB, cvec], [tD])
            k.dma("sp", rope_d[3, :, t0:t0 + T], tD[:], rs0, reads=[tD], writes=[])
            TS(sqt[:], tD[:], 0.125, None, ALU.mult, None, [tD], [sqt])
            k.dma("sp", rope_d[1, :, t0:t0 + T], sqt[:], rs1, reads=[sqt], writes=[])
        rope_deps = [(xo[0].sem, xo[0].cnt), (xo[1].sem, xo[1].cnt), (rs0.sem, rs0.cnt), (rs1.sem, rs1.cnt)]
        for d in rope_deps:
            k._wait("sp", d)

        def ck(name):
            if stage == name:
                raise _Stop()
        try:
          ck('pro')
          for l in range(DEPTH):
              k.dma("sp", convw[:], convw_in[l], convw, writes=[convw])
              k.dma("sp", gains[:], gains_in[l], gains, writes=[gains])
              k.dma("sp", bi[:], big_in[l].rearrange("(h o) -> h o", o=1), bi, writes=[bi])
              k.dma("sp", bfn[:], bfg_in[l].rearrange("(h o) -> h o", o=1), bfn, writes=[bfn])
              TS(bfn[:], bfn[:], -1.0, None, ALU.mult, None, [bfn], [bfn])
              k.dma("sp", mnb[:], mnorm_in[l].partition_broadcast(64), mnb, writes=[mnb])
              k.dma("sp", sinke[:], sinks_in[l], sinke, writes=[sinke])
              ACT(sinke[:], sinke[:], AF.Exp, [sinke], [sinke])
              for b in (cv, Ct, Ctb, nst, nstb, cB, cM, KTd, Vp):
                  k.op("dve", lambda b=b: E["dve"].memset(b[:], 0.0), [], [b])

              for it in range(NT):
                  t0 = it * T
                  treg = xT_reg[it]

                  def norm_stats(src_fn, nchunks=KC):
                      src_it = src_fn if not callable(src_fn) else (src_fn(kc) for kc in range(nchunks))
                      for kc, (sbuf, sap) in enumerate(src_it):
                          sq = sqb[kc % 2]
                          ACT(sq[:], sap, AF.Square, [sbuf], [sq])
                          MM(PS[7][:, 0:T], onesbf[:], sq[:], kc == 0, kc == nchunks - 1, [onesbf, sq], [PS[7]])
                      rsqrt_to(rstd, PS[7][:, 0:T], 1.0 / D, [PS[7]], tA)

                  def load_x(kc):
                      b = xs[kc % 3]
                      k.dma("sp", b[:], xT_d[kc, :, t0:t0 + T], b, reads=[treg], writes=[b])
                      return b, b[:]

                  def xstream(ahead=2):
                      q = []
                      nxt = 0
                      for kc in range(KC):
                          while nxt < KC and nxt <= kc + ahead:
                              q.append(load_x(nxt))
                              nxt += 1
                          yield q.pop(0)

                  norm_stats(xstream())
                  for kc, (b, ap) in enumerate(xstream()):
                      STT(hT[:, kc, :], ap, gains[:, kc:kc + 1], rstd[:], ALU.mult, ALU.mult, [b, gains, rstd], [hT])

                  ck('p0')
                  ck('A')
                  switch(rA, d_tmps)
                  Wb, wv = wget()
                  zT(PS[0], Wb, wv[0], 0, 8, hrhs, [hT])
                  zT(PS[1], Wb, wv[1], 0, 8, hrhs, [hT])
                  li, sp_, Bc, U, Mx, em, ra, rb, rw, rt = rA
                  ACT(li[:], PS[0][0:8, 0:T], AF.Identity, [PS[0], bi], [li], bias=bi[:, 0:1])
                  ACT(rt[:], PS[1][0:8, 0:T], AF.Exp, [PS[1], bfn], [rt], bias=bfn[:, 0:1], scale=-1.0)
                  ACT(sp_[:], rt[:], AF.Ln, [rt], [sp_], bias=1.0)

                  def scan(src, tmp, op):
                      cur, nxt = src, tmp
                      sh = 1
                      while sh < T:
                          TT(nxt[:, sh:T], cur[:, sh:T], cur[:, 0:T - sh], op, [cur], [nxt])
                          CP(nxt[:, 0:sh], cur[:, 0:sh], [cur], [nxt])
                          cur, nxt = nxt, cur
                          sh *= 2
                      return cur

                  cs = scan(sp_, rt, ALU.add)
                  TS(Bc[:], cs[:], -1.0, cB[:, 0:1], ALU.mult, ALU.add, [cs, cB], [Bc])
                  TT(U[:], li[:], Bc[:], ALU.subtract, [li, Bc], [U])
                  other = rt if cs is sp_ else sp_
                  CP(other[:], U[:], [U], [other])
                  other2 = sp_ if other is rt else rt
                  cm = scan(other, other2, ALU.max)
                  TS(Mx[:], cm[:], cM[:, 0:1], None, ALU.max, None, [cm, cM], [Mx])
                  TT(em[:], Bc[:], Mx[:], ALU.add, [Bc, Mx], [em])
                  ACT(em[:], em[:], AF.Exp, [em], [em], scale=-1.0)
                  Mx3 = Mx[:].rearrange("h (c s) -> h c s", s=64)
                  CP(mus[:], Mx3[:, :, 63], [Mx], [mus])
                  CP(mup[:, 0:1], cM[:], [cM], [mup])
                  if NCH > 1:
                      CP(mup[:, 1:NCH], mus[:, 0:NCH - 1], [mus], [mup])
                  CP(cM[:], mus[:, NCH - 1:NCH], [mus], [cM])
                  CP(cB[:], Bc[:, T - 1:T], [Bc], [cB])
                  musb = mus[:].unsqueeze(2).to_broadcast([8, NCH, 64])
                  mupb = mup[:].unsqueeze(2).to_broadcast([8, NCH, 64])
                  v3 = lambda b: b[:].rearrange("h (c s) -> h c s", s=64)
                  TT(v3(ra), v3(U), musb, ALU.subtract, [U, mus], [ra])
                  ACT(ra[:], ra[:], AF.Exp, [ra], [ra])
                  TT(v3(rb), musb, Mx3, ALU.subtract, [Mx, mus], [rb])
                  ACT(rb[:], rb[:], AF.Exp, [rb], [rb])
                  TT(v3(rw), mupb, Mx3, ALU.subtract, [Mx, mup], [rw])
                  ACT(rw[:], rw[:], AF.Exp, [rw], [rw])
                  TT(dec[:], mup[:], mus[:], ALU.subtract, [mup, mus], [dec])
                  ACT(dec[:], dec[:], AF.Exp, [dec], [dec])
                  TT(dexp[:], dec[:].unsqueeze(2).to_broadcast([8, NCH, 8]),
                     ident[0:8, 0:8].unsqueeze(1).to_broadcast([8, NCH, 8]), ALU.mult, [dec, ident], [dexp])
                  MM(PS[2][:, 0:NCH * 8], ones32[0:8, :], dexp[:].rearrange("h c g -> h (c g)"), True, True,
                     [ones32, dexp], [PS[2]])
                  CP(dbc[:].rearrange("p c g -> p (c g)"), PS[2][:, 0:NCH * 8], [PS[2]], [dbc])
                  for c in range(NCH):
                      for qi, rq in enumerate((ra, rb, rw, em)):
                          o0 = (c * 4 + qi) * 8
                          TR(PS[3][0:64, o0:o0 + 8], rq[:, c * 64:(c + 1) * 64], ident[0:8, 0:8], [rq, ident], [PS[3]])
                  CP(cols[:].rearrange("p c q h -> p (c q h)"), PS[3][0:64, 0:NCH * 32], [PS[3]], [cols])

                  ck('B1')
                  switch([qT, kT, vT, gateT], [big, xrow, xcol, posi, posf])
                  for dst, scale in ((qT, 128.0 ** -0.5), (kT, None), (vT, None)):
                      for hp in range(4):
                          Wb, wv = wget()
                          for hh in range(2):
                              pb = PS[4 + (hp % 2) * 2 + hh]
                              zT(pb, Wb, wv[0], hh * 128, 128, hrhs, [hT])
                              if scale is not None:
                                  ACT(dst[:, hp * 2 + hh, :], pb[:, 0:T], AF.Copy, [pb], [dst], scale=scale)
                              elif hh % 2 == 0:
                                  ACT(dst[:, hp * 2 + hh, :], pb[:, 0:T], AF.Copy, [pb], [dst])
                              else:
                                  CP(dst[:, hp * 2 + hh, :], pb[:, 0:T], [pb], [dst])
                  for hp in range(4):
                      Wb, wv = wget()
                      Wb2, wv2 = wget()
                      for hh in range(2):
                          h = hp * 2 + hh
                          zT(PS[4 + hh], Wb, wv[0], hh * 128, 128, hrhs, [hT])
                          zT(PS[6 + hh], Wb2, wv2[0], hh * 128, 128, hrhs, [hT])
                          ACT(tA[:], PS[4 + hh][:, 0:T], AF.Sigmoid, [PS[4 + hh]], [tA])
                          ACT(tB[:], PS[6 + hh][:, 0:T], AF.Silu, [PS[6 + hh]], [tB])
                          TT(gateT[:, h, :], tA[:], tB[:], ALU.mult, [tA, tB], [gateT])

                  ck('B2')
                  switch(chunk_tmps, rA)

                  def chunk_gen():
                      for c in range(NCH):
                          cs_ = slice(c * 64, (c + 1) * 64)
                          a_col = cols[:, c, 0, :]
                          b_col = cols[:, c, 1, :]
                          w_col = cols[:, c, 2, :]
                          e_col = cols[:, c, 3, :]
                          bc3 = lambda ap, n: ap.unsqueeze(2).to_broadcast([64, 8, n])
                          pkt = PS[0][:].bitcast(BF16)
                          pvt = PS[1][:].bitcast(BF16)
                          for h in range(8):
                              TR(pkt[0:64, h * 128:(h + 1) * 128], kT[:, h, cs_], identb[:], [kT, identb], [PS[0]])
                          for h in range(8):
                              TR(pvt[0:64, h * 128:(h + 1) * 128], vT[:, h, cs_], identb[:], [vT, identb], [PS[1]])
                          ACT(k_tm[:], pkt[0:64, :], AF.Copy, [PS[0]], [k_tm])
                          CP(v_tm[:], pvt[0:64, :], [PS[1]], [v_tm])
                          TT(av_tm[:].rearrange("p (h d) -> p h d", d=128), pvt[0:64, :].rearrange("p (h d) -> p h d", d=128),
                             bc3(a_col, 128), ALU.mult, [PS[1], cols], [av_tm])
                          CP(abf[:], a_col, [cols], [abf])
                          yield
                          for h in range(8):
                              MM(PS[2][0:64, h * 64:(h + 1) * 64], kT[:, h, cs_], qT[:, h, cs_], True, True, [kT, qT], [PS[2]])
                          TT(tmpS[:].rearrange("p (h l) -> p h l", l=64), PS[2][0:64, :].rearrange("p (h l) -> p h l", l=64),
                             bc3(a_col, 64), ALU.mult, [PS[2], cols], [tmpS])
                          TT(AT[:].rearrange("p (h l) -> p h l", l=64), tmpS[:].rearrange("p (h l) -> p h l", l=64),
                             causal[:].unsqueeze(1).to_broadcast([64, 8, 64]), ALU.mult, [tmpS, causal], [AT])
                          yield
                          for h in range(8):
                              pb = PS[3 + h // 4]
                              MM(pb[0:64, (h % 4) * 128:(h % 4 + 1) * 128], AT[:, h * 64:(h + 1) * 64], v_tm[:, h * 128:(h + 1) * 128],
                                 True, True, [AT, v_tm], [pb])
                          for h in range(8):
                              MM(PS[0][0:64, h:h + 1], AT[:, h * 64:(h + 1) * 64], onesb[0:64, 0:1], True, True, [AT, onesb], [PS[0]])
                          for h in range(8):
                              pb = PS[5] if h < 4 else PS[2]
                              MM(pb[0:64, (h % 4) * 128:(h % 4 + 1) * 128], qT[:, h, cs_], Ctb[:, h * 128:(h + 1) * 128],
                                 True, True, [qT, Ctb], [pb])
                          for h in range(8):
                              MM(PS[0][0:64, 8 + h:9 + h], qT[:, h, cs_], nstb[:, h:h + 1], True, True, [qT, nstb], [PS[0]])
                          for hf in range(2):
                              sl = slice(hf * 512, (hf + 1) * 512)
                              TT(t1[:, sl].rearrange("p (h d) -> p h d", d=128), PS[3 + hf][0:64, :].rearrange("p (h d) -> p h d", d=128),
                                 bc3(b_col, 128)[:, hf * 4:(hf + 1) * 4, :], ALU.mult, [PS[3 + hf], cols], [t1])
                              TT(t2[:, sl].rearrange("p (h d) -> p h d", d=128), (PS[5] if hf == 0 else PS[2])[0:64, :].rearrange("p (h d) -> p h d", d=128),
                                 bc3(w_col, 128)[:, hf * 4:(hf + 1) * 4, :], ALU.mult, [PS[5] if hf == 0 else PS[2], cols], [t2])
                          TT(t1[:], t1[:], t2[:], ALU.add, [t1, t2], [t1])
                          TT(sm[0][:], PS[0][0:64, 0:8], b_col, ALU.mult, [PS[0], cols], [sm[0]])
                          TT(sm[1][:], PS[0][0:64, 8:16], w_col, ALU.mult, [PS[0], cols], [sm[1]])
                          TT(sm[0][:], sm[0][:], sm[1][:], ALU.add, [sm[0], sm[1]], [sm[0]])
                          TS(sm[5][:], sm[0][:], -1.0, None, ALU.mult, None, [sm[0]], [sm[5]])
                          TT(sm[0][:], sm[0][:], sm[5][:], ALU.max, [sm[0], sm[5]], [sm[0]])
                          TT(sm[0][:], sm[0][:], e_col, ALU.max, [sm[0], cols], [sm[0]])
                          RECIP(sm[2][:], sm[0][:], [sm[0]], [sm[2]])
                          TT(t1[:].rearrange("p (h d) -> p h d", d=128), t1[:].rearrange("p (h d) -> p h d", d=128),
                             bc3(sm[2][:], 128), ALU.mult, [t1, sm[2]], [t1])
                          TT(t2[:], t1[:], t1[:], ALU.mult, [t1], [t2])
                          k.op("dve", lambda: E["dve"].tensor_reduce(out=sm[3][:], in_=t2[:].rearrange("p (h d) -> p h d", d=128),
                                                                     axis=AX.X, op=ALU.add), [t2], [sm[3]])
                          TS(sm[3][:], sm[3][:], 1.0 / 128, EPS, ALU.mult, ALU.add, [sm[3]], [sm[3]])
                          ACT(sm[3][:], sm[3][:], AF.Sqrt, [sm[3]], [sm[3]])
                          RECIP(sm[4][:], sm[3][:], [sm[3]], [sm[4]])
                          TT(t1[:].rearrange("p (h d) -> p h d", d=128), t1[:].rearrange("p (h d) -> p h d", d=128),
                             bc3(sm[4][:], 128), ALU.mult, [t1, sm[4]], [t1])
                          TT(t1[:], t1[:], mnb[:], ALU.mult, [t1, mnb], [t1])
                          yield
                          for h in range(8):
                              TR(PS[2][:, h * 64:(h + 1) * 64], t1[:, h * 128:(h + 1) * 128], ident[0:64, 0:64], [t1, ident], [PS[2]])
                          TT(ymT[:, :, cs_], PS[2][:].rearrange("p (h l) -> p h l", l=64), gateT[:, :, cs_], ALU.mult,
                             [PS[2], gateT], [ymT])
                          yield
                          for h in range(8):
                              pb = PS[h // 4]
                              MM(pb[:, (h % 4) * 128:(h % 4 + 1) * 128], k_tm[:, h * 128:(h + 1) * 128], av_tm[:, h * 128:(h + 1) * 128],
                                 True, True, [k_tm, av_tm], [pb])
                          for h in range(8):
                              MM(PS[2][:, h:h + 1], k_tm[:, h * 128:(h + 1) * 128], abf[:, h:h + 1], True, True, [k_tm, abf], [PS[2]])
                          dcol = dbc[:, c, :]
                          TT(Ct[:].rearrange("p (h d) -> p h d", d=128), Ct[:].rearrange("p (h d) -> p h d", d=128),
                             dcol.unsqueeze(2).to_broadcast([128, 8, 128]), ALU.mult, [Ct, dbc], [Ct])
                          for hf in range(2):
                              sl = slice(hf * 512, (hf + 1) * 512)
                              TT(Ct[:, sl], Ct[:, sl], PS[hf][:, :], ALU.add, [Ct, PS[hf]], [Ct])
                          ACT(Ctb[:], Ct[:], AF.Copy, [Ct], [Ctb])
                          TT(nst[:], nst[:], dcol, ALU.mult, [nst, dbc], [nst])
                          TT(nst[:], nst[:], PS[2][:, 0:8], ALU.add, [nst, PS[2]], [nst])
                          CP(nstb[:], nst[:], [nst], [nstb])
                          yield


                  bg = chunk_gen()

                  def tick(n=1):
                      for _ in range(n):
                          next(bg, None)

                  for c in range(8):
                      Wb, wv = wget()
                      Wb2, wv2 = wget()
                      zT(PS[6], Wb, wv[1], 0, 128, hrhs, [hT])
                      tick()
                      zT(PS[7], Wb2, wv2[0], 0, 128, hrhs, [hT])
                      tick()
                      CP(vb[:, 0:2], cv[:, c, :], [cv], [vb])
                      ACT(tA[:], PS[6][:, 0:T], AF.Copy, [PS[6]], [tA])
                      TT(vb[:, 2:2 + T], tA[:], PS[7][:, 0:T], ALU.mult, [tA, PS[7]], [vb])
                      CP(cv[:, c, :], vb[:, T:T + 2], [vb], [cv])
                      zT(PS[6], Wb, wv[0], 0, 128, hrhs, [hT])
                      tick()
                      zT(PS[7], Wb2, wv2[1], 0, 128, hrhs, [hT])
                      tick()
                      TS(tB[:], vb[:, 0:T], convw[:, c * 3:c * 3 + 1], None, ALU.mult, None, [vb, convw], [tB])
                      STT(tB[:], vb[:, 1:T + 1], convw[:, c * 3 + 1:c * 3 + 2], tB[:], ALU.mult, ALU.add, [vb, convw, tB], [tB])
                      STT(tB[:], vb[:, 2:T + 2], convw[:, c * 3 + 2:c * 3 + 3], tB[:], ALU.mult, ALU.add, [vb, convw, tB], [tB])
                      ACT(tC[:], PS[7][:, 0:T], AF.Silu, [PS[7]], [tC])
                      TT(tB[:], tB[:], PS[6][:, 0:T], ALU.mult, [tB, PS[6]], [tB])
                      TT(ycT[:, c, :], tB[:], tC[:], ALU.mult, [tB, tC], [ycT])
                  for _ in bg:
                      pass

                  ck('B3')
                  switch(attn_tmps, chunk_tmps)
                  for d in rope_deps:
                      k._wait("sp", d)
                  k.dma("sp", rtab[:], rope_d[:, :, t0:t0 + T].rearrange("f p t -> p f t"), rtab, writes=[rtab])

                  def rope(psb, cosi, sini, dst):
                      ACT(tA[:], psb[:, 0:T], AF.Copy, [psb], [tA])
                      MM(PS[7][:, 0:T], rm32[:], tA[:], True, True, [rm32, tA], [PS[7]])
                      TT(tB[:], tA[:], rtab[:, cosi, :], ALU.mult, [tA, rtab], [tB])
                      TT(tC[:], PS[7][:, 0:T], rtab[:, sini, :], ALU.mult, [PS[7], rtab], [tC])
                      TT(dst, tB[:], tC[:], ALU.add, [tB, tC], [dst_b[0]])

                  Wb, wv = wget()
                  Wv_, wvv = wget()
                  dst_b = [kr]
                  for g2 in range(2):
                      zT(PS[0], Wb, wv[0], g2 * 128, 128, hrhs, [hT])
                      rope(PS[0], 2, 3, kr[:])
                      for s in range(2):
                          MM(PS[1][:, 0:T], selb[:, s * 128:(s + 1) * 128], kr[:], True, True, [selb, kr], [PS[1]])
                          CP(KTd[0:64, 0, g2 * 2 + s, 128:128 + T], PS[1][0:64, 0:T], [PS[1]], [KTd])
                          ACT(KTd[64:128, 1, g2 * 2 + s, 128:128 + T], PS[1][64:128, 0:T], AF.Copy, [PS[1]], [KTd])
                  for qb in range(NQB):
                      for kc in range(KC):
                          MM(PS[2][:, 0:256], hT[:, kc, qb * 128:(qb + 1) * 128], wvv[0][:, kc, :], kc == 0, kc == KC - 1,
                             [hT, Wv_], [PS[2]])
                      pv = PS[2][:, 0:256].rearrange("p (g d) -> p g d", d=64)
                      CP(Vp[:, 1 + qb, :, 0, 0:64], pv, [PS[2]], [Vp])
                      ACT(Vp[:, 1 + qb, :, 1, 64:128], pv, AF.Copy, [PS[2]], [Vp])
                  dst_b = [qr]
                  for j2 in range(4):
                      Wb, wv = wget()
                      Wb2, wv2 = wget()
                      for jj in range(2):
                          j = j2 * 2 + jj
                          g = j // 2
                          zT(PS[0], Wb, wv[0], jj * 128, 128, hrhs, [hT])
                          rope(PS[0], 0, 1, qr[:])
                          zT(PS[1], Wb2, wv2[0], jj * 128, 128, hrhs, [hT])
                          ACT(sgT[:], PS[1][:, 0:T], AF.Silu, [PS[1]], [sgT])
                          def emit_scores(qb):
                              psb = PS[2] if qb % 2 == 0 else PS[5]
                              for hf in range(2):
                                  for kbi in range(2):
                                      o0 = (hf * 2 + kbi) * 128
                                      MM(psb[:, o0:o0 + 128], KTd[:, hf, g, (qb + kbi) * 128:(qb + kbi + 1) * 128],
                                         qr[:, qb * 128:(qb + 1) * 128], True, True, [KTd, qr], [psb])

                          emit_scores(0)
                          for qb in range(NQB):
                              if qb + 1 < NQB:
                                  emit_scores(qb + 1)
                              psb = PS[2] if qb % 2 == 0 else PS[5]
                              ex_, PT_ = exs[qb % 2], PTs[qb % 2]
                              ACT(ex_[:], psb[:, :], AF.Exp, [psb], [ex_])
                              mk = amask0 if (it == 0 and qb == 0) else amask
                              TT(PT_[:].rearrange("p (h r) -> p h r", r=256), ex_[:].rearrange("p (h r) -> p h r", r=256),
                                 mk[:].unsqueeze(1).to_broadcast([128, 2, 256]), ALU.mult, [ex_, mk], [PT_])
                              n = 0
                              for hf in range(2):
                                  for kbi in range(2):
                                      o0 = (hf * 2 + kbi) * 128
                                      MM(PS[3][:, 0:128], Vp[:, qb + kbi, g, hf, :], PT_[:, o0:o0 + 128], n == 0, n == 3, [Vp, PT_], [PS[3]])
                                      n += 1
                              n = 0
                              for hf in range(2):
                                  for kbi in range(2):
                                      o0 = (hf * 2 + kbi) * 128
                                      MM(PS[4][:, 0:128], onespad[:, hf * 128:(hf + 1) * 128], PT_[:, o0:o0 + 128], n == 0, n == 3,
                                         [onespad, PT_], [PS[4]])
                                      n += 1
                              TS(tD[:, 0:128], PS[4][:, 0:128], sinke[:, j:j + 1], None, ALU.add, None, [PS[4], sinke], [tD])
                              RECIP(tD[:, 0:128], tD[:, 0:128], [tD], [tD])
                              TT(tD[:, 0:128], tD[:, 0:128], PS[3][:, 0:128], ALU.mult, [tD, PS[3]], [tD])
                              TT(yaT[:, j, qb * 128:(qb + 1) * 128], tD[:, 0:128], sgT[:, qb * 128:(qb + 1) * 128], ALU.mult, [tD, sgT], [yaT])
                  CP(KTd[:, :, :, 0:128], KTd[:, :, :, T:T + 128], [KTd], [KTd])
                  CP(Vp[:, 0, :, :, :], Vp[:, NQB, :, :, :], [Vp], [Vp])

                  ck('C')
                  ys = (ycT, ymT, yaT)
                  switch(d_tmps, attn_tmps)
                  for fb in range(16):
                      Wg, wg = wget()
                      Wg2, wg2 = wget()
                      zT(PS[0], Wg, wg[0], 0, 128, hrhs, [hT])
                      zT(PS[1], Wg, wg[1], 0, 128, hrhs, [hT])
                      zT(PS[2], Wg2, wg2[0], 0, 128, hrhs, [hT])
                      for j in range(3):
                          Wu, wu = wget()
                          zT(PS[3 + j], Wu, wu[0], 0, 128, lambda kc, j=j: ys[j][:, kc, :], [ys[j]], kcs=8)
                      for j in range(3):
                          ACT(tA[:], PS[j][:, 0:T], AF.Sigmoid, [PS[j]], [tA])
                          if j == 0:
                              TT(tB[:], tA[:], PS[3][:, 0:T], ALU.mult, [tA, PS[3]], [tB])
                          else:
                              TT(tC[:], tA[:], PS[3 + j][:, 0:T], ALU.mult, [tA, PS[3 + j]], [tC])
                              if j == 1:
                                  TT(tB[:], tB[:], tC[:], ALU.add, [tB, tC], [tB])
                              else:
                                  TT(mgT[:, fb, :], tB[:], tC[:], ALU.add, [tB, tC], [mgT])
                  mrhs = lambda kc: mgT[:, kc, :]
                  switch([big], [qT, kT, vT, gateT])
                  for q8 in range(8):
                      Wb, wv = wget()
                      for i in range(2):
                          fb = q8 * 2 + i
                          pb = PS[(q8 % 2) * 2 + i]
                          zT(pb, Wb, wv[0], i * 128, 128, mrhs, [mgT])
                          if i % 2 == 0:
                              ACT(big[:, fb, :], pb[:, 0:T], AF.Copy, [pb], [big])
                          else:
                              CP(big[:, fb, :], pb[:, 0:T], [pb], [big])
                  norm_stats(lambda kc: (big, big[:, kc, :]))
                  for kc, (b, ap) in enumerate(xstream()):
                      STT(tB[:], big[:, kc, :], gains[:, 16 + kc:17 + kc], rstd[:], ALU.mult, ALU.mult, [big, gains, rstd], [tB])
                      ob = xo[kc % 2]
                      TT(ob[:], tB[:], ap, ALU.add, [tB, b], [ob])
                      CP(mgT[:, kc, :], ob[:], [ob], [mgT])
                      k.dma("sp", xT_d[kc, :, t0:t0 + T], ob[:], ob, reads=[ob], writes=[treg] if kc == 0 else [])
                  st_deps = [(xo[0].sem, xo[0].cnt), (xo[1].sem, xo[1].cnt)]
                  for qb in range(NQB):
                      k.dma("sp", p32[:], p_in[l, t0 + qb * 128:t0 + (qb + 1) * 128, :], p32, writes=[p32])
                      CP(pbf[:], p32[:], [p32], [pbf])
                      ppt = PS[4][:].bitcast(BF16)
                      for c2 in range(2):
                          TR(ppt[:, c2 * 128:(c2 + 1) * 128], pbf[:, c2 * 128:(c2 + 1) * 128], identb[:], [pbf, identb], [PS[4]])
                      CP(pT[:, :, qb * 128:(qb + 1) * 128], ppt[:, 0:256].rearrange("p (c t) -> p c t", t=128), [PS[4]], [pT])
                  for q8 in range(8):
                      Wb, wv = wget()
                      Wp, wp = wget()
                      for i in range(2):
                          fb = q8 * 2 + i
                          pa = PS[(q8 % 2) * 2 + i]
                          pe_ = PS[4 + (q8 % 2) * 2 + i]
                          zT(pa, Wb, wv[0], i * 128, 128, mrhs, [mgT])
                          zT(pe_, Wp, wp[0], i * 128, 128, lambda kc: pT[:, kc, :], [pT], kcs=2)
                          ACT(tA[:], pa[:, 0:T], AF.Sigmoid, [pa], [tA])
                          TT(big[:, fb, :], tA[:], pe_[:, 0:T], ALU.mult, [tA, pe_], [big])
                  norm_stats(lambda kc: (big, big[:, kc, :]))
                  for d in st_deps:
                      k._wait("sp", d)
                  last = (l == DEPTH - 1)
                  for kc, (b, ap) in enumerate(xstream()):
                      STT(tB[:], big[:, kc, :], gains[:, 32 + kc:33 + kc], rstd[:], ALU.mult, ALU.mult, [big, gains, rstd], [tB])
                      ob = xo[kc % 2]
                      TT(ob[:], tB[:], ap, ALU.add, [tB, b], [ob])
                      k.dma("sp", xT_d[kc, :, t0:t0 + T], ob[:], ob, reads=[ob], writes=[treg] if kc == 0 else [])
                  fin = [(xo[0].sem, xo[0].cnt), (xo[1].sem, xo[1].cnt)]
                  treg.w = None
                  for d in fin:
                      k._wait("sp", d)

        except _Stop:
            pass
        switch([xcol, xrow], [big, qT, kT, vT, gateT])
        for blk in range(NTOK // 128):
            k.dma("sp", xcol[:], xT_d[:, :, blk * 128:(blk + 1) * 128].rearrange("k p t -> p k t"), xcol, writes=[xcol])
            for g4 in range(4):
                pb = PS[g4 % 2]
                for i in range(4):
                    kc = g4 * 4 + i
                    TR(pb[:, i * 128:(i + 1) * 128], xcol[:, kc, :], ident[:], [xcol, ident], [pb])
                CP(xrow[:, g4 * 512:(g4 + 1) * 512], pb[:, :], [pb], [xrow])
            k.dma("sp", y_out[blk * 128:(blk + 1) * 128, :], xrow[:], xrow, reads=[xrow])
        k._wait("sp", (xrow.sem, xrow.cnt))
        for e in ("pe", "act", "dve"):
            if k.sems[e]:
                c = k.cnt[e]
                ep = (c - 1) // EPOCH
                k._wait("sp", (k.sems[e][ep], c - ep * EPOCH))
    return nc


def host_consts():
    ident = np.eye(128, dtype=np.float32)
    rm = np.zeros((128, 128), np.float32)
    for m in range(128):
        rm[(m // 64) * 64 + ((m % 64) + 32) % 64, m] = 1.0
    sel = np.zeros((128, 256), np.float32)
    for s in range(2):
        for m in range(128):
            sel[s * 64 + (m % 64), s * 128 + m] = 1.0
    vec = np.zeros((128, 4), np.float32)
    j = np.arange(128) % 32
    vec[:, 0] = np.power(np.float32(10000.0), (-2.0 * j.astype(np.float32) / 64).astype(np.float32)).astype(np.float32)
    vec[:, 1] = np.where((np.arange(128) % 64) < 32, -1.0, 1.0)
    causal = (np.arange(64)[:, None] <= np.arange(64)[None, :]).astype(np.float32)
    kk = np.arange(128)[:, None]
    qq = np.arange(128)[None, :]
    amask = np.concatenate([(kk > qq), (kk <= qq)], axis=1).astype(np.float32)
    onespad = np.zeros((128, 256), np.float32)
    onespad[:, 0:64] = 1.0
    onespad[:, 128 + 64:256] = 1.0
    return {"c_ident": ident, "c_rm": rm, "c_sel": sel, "c_vec": vec, "c_causal": causal,
            "c_amask": amask, "c_onespad": onespad}


def layout_inputs(b, x, p, positions, w_in, conv_w, b_igate, b_fgate, mlstm_norm, attn_sinks,
                  w_up_conv, w_up_mlstm, w_up_attn, w_out, pre_norm, post_norm, w_ple,
                  w_ple_gate, ple_norm, consts):
    L = w_in.shape[0]
    f = lambda a: np.ascontiguousarray(np.asarray(a, dtype=np.float32))
    convw = f(np.asarray(conv_w).reshape(L, 3, 8, 128).transpose(0, 3, 2, 1).reshape(L, 128, 24))
    g = lambda a: np.asarray(a).reshape(L, 16, 128).transpose(0, 2, 1)
    gains = f(np.concatenate([g(pre_norm), g(post_norm), g(ple_norm)], axis=2))
    sk = np.asarray(attn_sinks).reshape(L, 8, 2)
    sinks = f(np.repeat(sk.transpose(0, 2, 1), 64, axis=1))
    m = {"x": f(x[b]), "p": f(np.asarray(p)[:, b]), "pos": np.ascontiguousarray(np.asarray(positions)[b].astype(np.int32)),
         "w_in": f(w_in), "w_up_conv": f(w_up_conv), "w_up_mlstm": f(w_up_mlstm), "w_up_attn": f(w_up_attn),
         "w_out": f(w_out), "w_ple": f(w_ple), "w_ple_gate": f(w_ple_gate), "convw": convw, "gains": gains,
         "b_igate": f(b_igate), "b_fgate": f(b_fgate), "mlstm_norm": f(mlstm_norm), "sinks": sinks}
    m.update(consts)
    return m


def kernel(**inputs):
    x = np.asarray(inputs["x"])
    B, S, _ = x.shape
    L = np.asarray(inputs["w_in"]).shape[0]
    nc = build(S, L, T=512)
    consts = host_consts()
    args = [inputs[n] for n in ("x", "p", "positions", "w_in", "conv_w", "b_igate", "b_fgate", "mlstm_norm",
                                "attn_sinks", "w_up_conv", "w_up_mlstm", "w_up_attn", "w_out", "pre_norm",
                                "post_norm", "w_ple", "w_ple_gate", "ple_norm")]
    maps = [layout_inputs(c % B, *args, consts) for c in range(B)]
    in_maps = [maps[c % B] for c in range(8)]
    res = run_bass_kernel_spmd(nc, in_maps, core_ids=list(range(8)))
    return np.stack([np.asarray(res.results[b]["y"], dtype=np.float32) for b in range(B)], axis=0)
```

```python
import numpy as np
from contextlib import ExitStack
import concourse.bass as bass
import concourse.mybir as mybir
from concourse.bass_utils import run_bass_kernel_spmd

F32 = mybir.dt.float32
BF16 = mybir.dt.bfloat16
I32 = mybir.dt.int32
AF = mybir.ActivationFunctionType
ALU = mybir.AluOpType
AX = mybir.AxisListType

D = 2048
KC = 16
IN_W = 17936
EPS = 1e-6
EPOCH = 30000
WB_ELEMS = 4096
NWB = 5
LIVE = 2

O_CB, O_CC, O_CX, O_CG = 0, 1024, 2048, 3072
O_MQ, O_MK, O_MV, O_MO, O_MI, O_MF, O_MG = 4096, 5120, 6144, 7168, 8192, 8200, 8208
O_AQ, O_AK, O_AV, O_AG = 9232, 10256, 10512, 10768
O_GT = 11792


class Buf:
    __slots__ = ("t", "w", "r", "sem", "cnt")

    def __init__(self, t=None, sem=None):
        self.t = t
        self.w = None
        self.r = {}
        self.sem = sem
        self.cnt = 0

    def __getitem__(self, k):
        return self.t[k]


class KB:
    def __init__(self, nc, es):
        self.nc = nc
        self.es = es
        self.eng = {"pe": nc.tensor, "act": nc.scalar, "dve": nc.vector,
                    "pool": nc.gpsimd, "sp": nc.sync}
        self.cnt = {e: 0 for e in self.eng}
        self.sems = {e: [] for e in self.eng}
        self.waited = {e: {} for e in self.eng}
        self.nsem = 0

    def newsem(self, name):
        self.nsem += 1
        return self.es.enter_context(self.nc.semaphore(name))

    def sb(self, name, shape, dt, dma=False):
        t = self.es.enter_context(self.nc.sbuf_tensor("sb_" + name, shape, dt))
        return Buf(t, self.newsem("s_" + name) if dma else None)

    def ps(self, name, shape, dt=F32):
        t = self.es.enter_context(self.nc.psum_tensor(name, shape, dt))
        return Buf(t)

    def _mark(self, e):
        c = self.cnt[e]
        ep = c // EPOCH
        while len(self.sems[e]) <= ep:
            self.sems[e].append(self.newsem("c_%s_%d" % (e, len(self.sems[e]))))
        self.cnt[e] = c + 1
        return (self.sems[e][ep], c - ep * EPOCH + 1)

    def _wait(self, e, dep):
        if dep is None:
            return
        sem, val = dep
        if e == "pe" and any(sem is s for s in self.sems["pe"]):
            return
        k = id(sem)
        if self.waited[e].get(k, 0) >= val:
            return
        self.waited[e][k] = val
        self.eng[e].wait_ge(sem, val)

    def op(self, e, fn, reads=(), writes=()):
        for b in reads:
            self._wait(e, b.w)
        for b in writes:
            self._wait(e, b.w)
            for d in list(b.r.values()):
                self._wait(e, d)
        ins = fn()
        m = self._mark(e)
        ins.then_inc(m[0], 1)
        for b in reads:
            b.r[id(m[0])] = m
        for b in writes:
            b.w = m
            b.r = {}
        return ins

    def dma(self, q, out_ap, in_ap, semb, reads=(), writes=()):
        for b in reads:
            self._wait(q, b.w)
        for b in writes:
            self._wait(q, b.w)
            for d in list(b.r.values()):
                self._wait(q, d)
        ins = self.eng[q].dma_start(out=out_ap, in_=in_ap)
        semb.cnt += 16
        ins.then_inc(semb.sem, 16)
        m = (semb.sem, semb.cnt)
        for b in reads:
            b.r[id(semb.sem)] = m
        for b in writes:
            b.w = m
            b.r = {}

    def wait_all(self, q, bufs):
        for b in bufs:
            self._wait(q, b.w)


class _Stop(Exception):
    pass


def build(NTOK, DEPTH, T=256, debug=None, stage=None):
    nc = bass.Bass("TRN2", target_bir_lowering=False)
    NT = NTOK // T
    NCH = T // 64
    NQB = T // 128
    dbg = {}

    def din(name, shape, dt=F32):
        return nc.dram_tensor(name, shape, dt, kind="ExternalInput").ap()

    x_in = din("x", [NTOK, D])
    p_in = din("p", [DEPTH, NTOK, 256])
    pos_in = din("pos", [NTOK], I32)
    w_in = din("w_in", [DEPTH, D, IN_W])
    w_upc = din("w_up_conv", [DEPTH, 1024, D])
    w_upm = din("w_up_mlstm", [DEPTH, 1024, D])
    w_upa = din("w_up_attn", [DEPTH, 1024, D])
    w_out = din("w_out", [DEPTH, D, D])
    w_ple = din("w_ple", [DEPTH, 256, D])
    w_pg = din("w_ple_gate", [DEPTH, D, D])
    convw_in = din("convw", [DEPTH, 128, 24])
    gains_in = din("gains", [DEPTH, 128, 48])
    big_in = din("b_igate", [DEPTH, 8])
    bfg_in = din("b_fgate", [DEPTH, 8])
    mnorm_in = din("mlstm_norm", [DEPTH, 1024])
    sinks_in = din("sinks", [DEPTH, 128, 8])
    c_ident = din("c_ident", [128, 128])
    c_rm = din("c_rm", [128, 128])
    c_sel = din("c_sel", [128, 256])
    c_vec = din("c_vec", [128, 4])
    c_causal = din("c_causal", [64, 64])
    c_amask = din("c_amask", [128, 256])
    c_onespad = din("c_onespad", [128, 256])
    y_out = nc.dram_tensor("y", [NTOK, D], F32, kind="ExternalOutput").ap()
    xT_d = nc.dram_tensor("xT_scr", [KC, 128, NTOK], F32, kind="Internal").ap()
    rope_d = nc.dram_tensor("rope_scr", [4, 128, NTOK], F32, kind="Internal").ap()
    if debug:
        for nm, shp in debug.items():
            dbg[nm] = nc.dram_tensor("dbg_" + nm, shp, F32, kind="ExternalOutput").ap()

    es = ExitStack()
    with es:
        k = KB(nc, es)
        E = {e: k.eng[e] for e in k.eng}
        xT_reg = [Buf() for _ in range(NT)]
        rope_reg = Buf()

        ident = k.sb("ident", [128, 128], F32, dma=True)
        identb = k.sb("identb", [128, 128], BF16)
        rm32 = k.sb("rm32", [128, 128], F32, dma=True)
        selb = k.sb("selb", [128, 256], BF16)
        sel32 = k.sb("sel32", [128, 256], F32, dma=True)
        cvec = k.sb("cvec", [128, 4], F32, dma=True)
        causal = k.sb("causal", [64, 64], F32, dma=True)
        amask = k.sb("amask", [128, 256], F32, dma=True)
        amask0 = k.sb("amask0", [128, 256], F32)
        onespad32 = k.sb("onespad32", [128, 256], F32, dma=True)
        onespad = k.sb("onespad", [128, 256], BF16)
        ones32 = k.sb("ones32", [128, 128], F32)
        onesb = k.sb("onesb", [128, 8], BF16)
        onesbf = k.sb("onesbf", [128, 128], BF16)
        sqb = [k.sb("sqb%d" % i, [128, T], BF16) for i in range(2)]
        for b, src in ((ident, c_ident), (rm32, c_rm), (sel32, c_sel), (cvec, c_vec),
                       (causal, c_causal), (amask, c_amask), (onespad32, c_onespad)):
            k.dma("sp", b[:], src[:], b, writes=[b])
        k.op("dve", lambda: E["dve"].tensor_copy(out=identb[:], in_=ident[:]), [ident], [identb])
        k.op("dve", lambda: E["dve"].tensor_copy(out=selb[:], in_=sel32[:]), [sel32], [selb])
        k.op("dve", lambda: E["dve"].tensor_copy(out=onespad[:], in_=onespad32[:]), [onespad32], [onespad])
        k.op("dve", lambda: E["dve"].memset(ones32[:], 1.0), [], [ones32])
        k.op("dve", lambda: E["dve"].memset(onesb[:], 1.0), [], [onesb])
        k.op("dve", lambda: E["dve"].memset(onesbf[:], 1.0), [], [onesbf])
        k.op("dve", lambda: E["dve"].tensor_copy(out=amask0[:], in_=amask[:]), [amask], [amask0])
        k.op("dve", lambda: E["dve"].memset(amask0[:, 0:128], 0.0), [], [amask0])

        PS = [k.ps("ps%d" % i, [128, 512], F32) for i in range(8)]

        WB = [k.sb("wb%d" % i, [128, WB_ELEMS], BF16, dma=True) for i in range(NWB)]
        hT = k.sb("hT", [128, KC, T], BF16)
        ycT = k.sb("ycT", [128, 8, T], BF16)
        ymT = k.sb("ymT", [128, 8, T], BF16)
        yaT = k.sb("yaT", [128, 8, T], BF16)
        XW = 16 * T
        arX = es.enter_context(nc.sbuf_tensor("arenaX", [128, XW], F32))
        YW = 5120
        arY = es.enter_context(nc.sbuf_tensor("arenaY", [128, YW], F32))

        def vw(ar, lo, hi, dt=F32, parts=128, sem=False, shape=None):
            ap = ar[0:parts, lo:hi]
            if dt is not F32:
                ap = ap.bitcast(dt)
            if shape:
                ap = ap.rearrange(shape[0], **shape[1])
            return Buf(ap, k.newsem("s_v%d" % k.nsem) if sem else None)

        def switch(new, old):
            for nv in new:
                for ov in old:
                    deps = list(ov.r.values()) + ([ov.w] if ov.w else [])
                    for d in deps:
                        kk = id(d[0])
                        if kk not in nv.r or nv.r[kk][1] < d[1]:
                            nv.r[kk] = d

        big = vw(arX, 0, 16 * T, shape=("p (a b) -> p a b", dict(b=T)))
        mgT = vw(arY, 0, 8 * T, BF16, shape=("p (a b) -> p a b", dict(b=T)))
        xs = [k.sb("xs%d" % i, [128, T], F32, dma=True) for i in range(3)]
        xo = [k.sb("xo%d" % i, [128, T], F32, dma=True) for i in range(2)]
        acc = k.sb("acc", [128, T], F32)
        sqt = k.sb("sqt", [128, T], F32)
        rstd = k.sb("rstd", [128, T], F32)
        tA = k.sb("tA", [128, T], F32)
        tB = k.sb("tB", [128, T], F32)
        tC = k.sb("tC", [128, T], F32)
        tD = k.sb("tD", [128, T], F32)
        vb = k.sb("vb", [128, T + 2], F32)
        cv = k.sb("cv", [128, 8, 2], F32)
        convw = k.sb("convw", [128, 24], F32, dma=True)
        gains = k.sb("gains", [128, 48], F32, dma=True)
        bi = k.sb("bi", [8, 1], F32, dma=True)
        bfn = k.sb("bfn", [8, 1], F32, dma=True)
        mnb = k.sb("mnb", [64, 1024], F32, dma=True)
        sinke = k.sb("sinke", [128, 8], F32, dma=True)
        h3 = ("p (a b) -> p a b", dict(b=T))
        qT = vw(arX, 0, 4 * T, BF16, shape=h3)
        kT = vw(arX, 4 * T, 8 * T, BF16, shape=h3)
        vT = vw(arX, 8 * T, 12 * T, BF16, shape=h3)
        gateT = vw(arX, 12 * T, 16 * T, BF16, shape=h3)
        rA = [vw(arY, i * T, (i + 1) * T, parts=8) for i in range(10)]
        assert 10 * T <= YW
        mus = k.sb("mus", [8, NCH], F32)
        mup = k.sb("mup", [8, NCH], F32)
        dec = k.sb("dec", [8, NCH], F32)
        dexp = k.sb("dexp", [8, NCH, 8], F32)
        dbc = k.sb("dbc", [128, NCH, 8], F32)
        cB = k.sb("cB", [8, 1], F32)
        cM = k.sb("cM", [8, 1], F32)
        cols = k.sb("cols", [64, NCH, 4, 8], F32)
        abf = k.sb("abf", [64, 8], BF16)
        t1 = vw(arY, 0, 1024, parts=64)
        t2 = vw(arY, 1024, 2048, parts=64)
        tmpS = vw(arY, 2048, 2560, parts=64)
        AT = vw(arY, 2560, 2816, BF16, parts=64)
        k_tm = vw(arY, 2816, 3328, BF16, parts=64)
        v_tm = vw(arY, 3328, 3840, BF16, parts=64)
        av_tm = vw(arY, 3840, 4352, BF16, parts=64)
        chunk_tmps = [t1, t2, tmpS, AT, k_tm, v_tm, av_tm]
        sm = [k.sb("sm%d" % i, [64, 8], F32) for i in range(6)]
        Ct = k.sb("Ct", [128, 1024], F32)
        Ctb = k.sb("Ctb", [128, 1024], BF16)
        nst = k.sb("nst", [128, 8], F32)
        nstb = k.sb("nstb", [128, 8], BF16)
        KTd = k.sb("KTd", [128, 2, 4, 128 + T], BF16)
        Vp = k.sb("Vp", [128, NQB + 1, 4, 2, 128], BF16)
        rtab = vw(arY, 0, 4 * T, sem=True, shape=("p (a b) -> p a b", dict(b=T)))
        o_ = 4 * T
        ex = vw(arY, o_, o_ + 512)
        sgT = vw(arY, o_ + 512, o_ + 512 + T)
        o_ = o_ + 512 + T
        PT = vw(arY, o_, o_ + 256, BF16)
        qr = vw(arY, o_ + 256, o_ + 256 + T // 2, BF16)
        kr = vw(arY, o_ + 256 + T // 2, o_ + 256 + T, BF16)
        o_ = o_ + 256 + T
        ex2 = vw(arY, o_, o_ + 512)
        PT2 = vw(arY, o_ + 512, o_ + 768, BF16)
        assert o_ + 768 <= YW
        exs = [ex, ex2]
        PTs = [PT, PT2]
        attn_tmps = [rtab, ex, sgT, PT, qr, kr, ex2, PT2]
        o_ = 8 * T
        p32 = vw(arY, o_, o_ + 256, sem=True)
        pbf = vw(arY, o_ + 256, o_ + 384, BF16)
        pT = vw(arY, o_ + 384, o_ + 384 + T, BF16, shape=("p (a b) -> p a b", dict(b=T)))
        assert o_ + 384 + T <= YW
        d_tmps = [mgT, p32, pbf, pT]

        def ACT(out, in_, func, reads, writes, bias=None, scale=None):
            kw = {}
            if bias is not None:
                kw["bias"] = bias
            if scale is not None:
                kw["scale"] = scale
            return k.op("act", lambda: E["act"].activation(out=out, in_=in_, func=func, **kw), reads, writes)

        def TT(out, in0, in1, op, reads, writes, e="dve"):
            return k.op(e, lambda: E[e].tensor_tensor(out=out, in0=in0, in1=in1, op=op), reads, writes)

        def TS(out, in0, s1, s2, op0, op1, reads, writes, e="dve"):
            if s2 is None:
                return k.op(e, lambda: E[e].tensor_scalar(out=out, in0=in0, scalar1=s1, scalar2=None, op0=op0), reads, writes)
            return k.op(e, lambda: E[e].tensor_scalar(out=out, in0=in0, scalar1=s1, scalar2=s2, op0=op0, op1=op1), reads, writes)

        def STT(out, in0, sc, in1, op0, op1, reads, writes, e="dve"):
            return k.op(e, lambda: E[e].scalar_tensor_tensor(out=out, in0=in0, scalar=sc, in1=in1, op0=op0, op1=op1), reads, writes)

        def CP(out, in_, reads, writes, e="dve"):
            return k.op(e, lambda: E[e].tensor_copy(out=out, in_=in_), reads, writes)

        def RECIP(out, in_, reads, writes):
            return k.op("dve", lambda: E["dve"].reciprocal(out=out, in_=in_), reads, writes)

        def MM(out, lhsT, rhs, start, stop, reads, writes):
            return k.op("pe", lambda: E["pe"].matmul(out, lhsT, rhs, start=start, stop=stop), reads, writes)

        def TR(out, in_, idn, reads, writes):
            return k.op("pe", lambda: E["pe"].transpose(out, in_, idn), reads, writes)

        def rsqrt_to(out, in_ps, scale, reads_b, tmp):
            TS(tmp[:], in_ps, scale, EPS, ALU.mult, ALU.add, reads_b, [tmp])
            ACT(tmp[:], tmp[:], AF.Sqrt, [tmp], [tmp])
            RECIP(out[:], tmp[:], [tmp], [out])

        wstate = {"specs": [], "issued": 0, "used": 0}

        def wspec(wten, K, pieces):
            wstate["specs"].append((wten, K, pieces))

        def wissue():
            i = wstate["issued"]
            wten, K, pieces = wstate["specs"][i]
            b = WB[i % NWB]
            kc = K // 128
            off = 0
            src = wten.rearrange("(kc p) c -> p kc c", p=128)
            first = True
            for (c0, n) in pieces:
                dst = b[:, off:off + kc * n].rearrange("p (k c) -> p k c", c=n)
                k.dma("pool", dst, src[:, :, c0:c0 + n], b, writes=[b] if first else [])
                if not first:
                    b.w = (b.sem, b.cnt)
                first = False
                off += kc * n
            wstate["issued"] = i + 1

        def wget():
            u = wstate["used"]
            while wstate["issued"] < min(len(wstate["specs"]), u + NWB - (LIVE - 1)):
                wissue()
            wten, K, pieces = wstate["specs"][u]
            b = WB[u % NWB]
            kc = K // 128
            views = []
            off = 0
            for (c0, n) in pieces:
                views.append(b[:, off:off + kc * n].rearrange("p (k c) -> p k c", c=n))
                off += kc * n
            wstate["used"] = u + 1
            return b, views

        def plan_weights(l):
            W = w_in[l]
            wspec(W, D, [(O_MI, 8), (O_MF, 8)])
            for o in (O_MQ, O_MK, O_MV):
                for hp in range(4):
                    wspec(W, D, [(o + hp * 256, 256)])
            for hp in range(4):
                wspec(W, D, [(O_MO + hp * 256, 256)])
                wspec(W, D, [(O_MG + hp * 256, 256)])
            for c in range(8):
                wspec(W, D, [(O_CB + c * 128, 128), (O_CC + c * 128, 128)])
                wspec(W, D, [(O_CX + c * 128, 128), (O_CG + c * 128, 128)])
            wspec(W, D, [(O_AK, 256)])
            wspec(W, D, [(O_AV, 256)])
            for j2 in range(4):
                wspec(W, D, [(O_AQ + j2 * 256, 256)])
                wspec(W, D, [(O_AG + j2 * 256, 256)])
            for fb in range(16):
                wspec(W, D, [(O_GT + j * 2048 + fb * 128, 128) for j in range(2)])
                wspec(W, D, [(O_GT + 2 * 2048 + fb * 128, 128)])
                wspec(w_upc[l], 1024, [(fb * 128, 128)])
                wspec(w_upm[l], 1024, [(fb * 128, 128)])
                wspec(w_upa[l], 1024, [(fb * 128, 128)])
            for q8 in range(8):
                wspec(w_out[l], D, [(q8 * 256, 256)])
            for q8 in range(8):
                wspec(w_pg[l], D, [(q8 * 256, 256)])
                wspec(w_ple[l], 256, [(q8 * 256, 256)])

        for l in range(DEPTH):
            for it in range(NT):
                plan_weights(l)

        def zT(psb, Wb, wv, c0, ncol, rhs_fn, rhs_bufs, kcs=KC, pslice=None):
            o = pslice if pslice is not None else psb[0:ncol, 0:T]
            for kc in range(kcs):
                MM(o, wv[:, kc, c0:c0 + ncol], rhs_fn(kc), kc == 0, kc == kcs - 1, [Wb] + rhs_bufs, [psb])

        hrhs = lambda kc: hT[:, kc, :]

        xrow = vw(arX, 0, D, sem=True)
        xcol = vw(arX, D, 2 * D, sem=True, shape=("p (a b) -> p a b", dict(b=128)))
        for blk in range(NTOK // 128):
            k.dma("sp", xrow[:], x_in[blk * 128:(blk + 1) * 128, :], xrow, writes=[xrow])
            for g4 in range(4):
                pb = PS[g4 % 2]
                for i in range(4):
                    kc = g4 * 4 + i
                    TR(pb[:, i * 128:(i + 1) * 128], xrow[:, kc * 128:(kc + 1) * 128], ident[:], [xrow, ident], [pb])
                CP(xcol[:, g4 * 4:(g4 + 1) * 4, :], pb[:].rearrange("p (a b) -> p a b", b=128), [pb], [xcol])
            treg = xT_reg[(blk * 128) // T]
            k.dma("sp", xT_d[:, :, blk * 128:(blk + 1) * 128].rearrange("k p t -> p k t"), xcol[:], xcol,
                  reads=[xcol], writes=[treg])
        posi = vw(arX, 2 * D, 2 * D + T, I32, sem=True)
        posf = vw(arX, 2 * D + T, 2 * D + 2 * T)
        rs0 = Buf(None, k.newsem("s_rope0"))
        rs1 = Buf(None, k.newsem("s_rope1"))
        for it in range(NT):
            t0 = it * T
            k.dma("sp", posi[:], pos_in[t0:t0 + T].partition_broadcast(128), posi, writes=[posi])
            CP(posf[:], posi[:], [posi], [posf])
            TS(tA[:], posf[:], cvec[:, 0:1], None, ALU.mult, None, [posf, cvec], [tA])
            def rred(dst, src, addc):
                TS(sqt[:], src[:], addc, None, ALU.add, None, [src], [sqt])
                TS(acc[:], sqt[:], 1.0 / (2.0 * np.pi), None, ALU.mult, None, [sqt], [acc])
                CP(posi[:], acc[:], [acc], [posi])
                CP(acc[:], posi[:], [posi], [acc])
                STT(dst[:], acc[:], -2.0 * np.pi, sqt[:], ALU.mult, ALU.add, [acc, sqt], [dst])
                TS(acc[:], dst[:], np.pi, -2.0 * np.pi, ALU.is_gt, ALU.mult, [dst], [acc])
                TT(dst[:], dst[:], acc[:], ALU.add, [dst, acc], [dst])
                TS(acc[:], dst[:], -np.pi, 2.0 * np.pi, ALU.is_lt, ALU.mult, [dst], [acc])
                TT(dst[:], dst[:], acc[:], ALU.add, [dst, acc], [dst])
                TS(dst[:], dst[:], -3.1415925, 3.1415925, ALU.max, ALU.min, [dst], [dst])
            rred(tB, tA, 0.0)
            ACT(tB[:], tB[:], AF.Sin, [tB], [tB])
            TS(tB[:], tB[:], -1.0, None, ALU.mult, None, [tB], [tB])
            rred(tC, tA, 0.5 * np.pi)
            ACT(tC[:], tC[:], AF.Sin, [tC], [tC])
            TS(tC[:], tC[:], -1.0, None, ALU.mult, None, [tC], [tC])
            TS(xo[0][:], tC[:], -1.0, None, ALU.mult, None, [tC], [xo[0]])
            k.dma("sp", rope_d[2, :, t0:t0 + T], xo[0][:], xo[0], reads=[xo[0]], writes=[rope_reg])
            TS(xo[1][:], tC[:], -0.125, None, ALU.mult, None, [tC], [xo[1]])
            k.dma("sp", rope_d[0, :, t0:t0 + T], xo[1][:], xo[1], reads=[xo[1]], writes=[])
            rope_reg.w = None
            TS(tD[:], tB[:], cvec[:, 1:2], -1.0, ALU.mult, ALU.mult, [tB, cvec], [tD])
            k.dma("sp", rope_d[3, :, t0:t0 + T], tD[:], rs0, reads=[tD], writes=[])
            TS(sqt[:], tD[:], 0.125, None, ALU.mult, None, [tD], [sqt])
            k.dma("sp", rope_d[1, :, t0:t0 + T], sqt[:], rs1, reads=[sqt], writes=[])
        rope_deps = [(xo[0].sem, xo[0].cnt), (xo[1].sem, xo[1].cnt), (rs0.sem, rs0.cnt), (rs1.sem, rs1.cnt)]
        for d in rope_deps:
            k._wait("sp", d)

        def ck(name):
            if stage == name:
                raise _Stop()
        try:
          ck('pro')
          for l in range(DEPTH):
              k.dma("sp", convw[:], convw_in[l], convw, writes=[convw])
              k.dma("sp", gains[:], gains_in[l], gains, writes=[gains])
              k.dma("sp", bi[:], big_in[l].rearrange("(h o) -> h o", o=1), bi, writes=[bi])
              k.dma("sp", bfn[:], bfg_in[l].rearrange("(h o) -> h o", o=1), bfn, writes=[bfn])
              TS(bfn[:], bfn[:], -1.0, None, ALU.mult, None, [bfn], [bfn])
              k.dma("sp", mnb[:], mnorm_in[l].partition_broadcast(64), mnb, writes=[mnb])
              k.dma("sp", sinke[:], sinks_in[l], sinke, writes=[sinke])
              ACT(sinke[:], sinke[:], AF.Exp, [sinke], [sinke])
              for b in (cv, Ct, Ctb, nst, nstb, cB, cM, KTd, Vp):
                  k.op("dve", lambda b=b: E["dve"].memset(b[:], 0.0), [], [b])

              for it in range(NT):
                  t0 = it * T
                  treg = xT_reg[it]

                  def norm_stats(src_fn, nchunks=KC):
                      src_it = src_fn if not callable(src_fn) else (src_fn(kc) for kc in range(nchunks))
                      for kc, (sbuf, sap) in enumerate(src_it):
                          sq = sqb[kc % 2]
                          ACT(sq[:], sap, AF.Square, [sbuf], [sq])
                          MM(PS[7][:, 0:T], onesbf[:], sq[:], kc == 0, kc == nchunks - 1, [onesbf, sq], [PS[7]])
                      rsqrt_to(rstd, PS[7][:, 0:T], 1.0 / D, [PS[7]], tA)

                  def load_x(kc):
                      b = xs[kc % 3]
                      k.dma("sp", b[:], xT_d[kc, :, t0:t0 + T], b, reads=[treg], writes=[b])
                      return b, b[:]

                  def xstream(ahead=2):
                      q = []
                      nxt = 0
                      for kc in range(KC):
                          while nxt < KC and nxt <= kc + ahead:
                              q.append(load_x(nxt))
                              nxt += 1
                          yield q.pop(0)

                  norm_stats(xstream())
                  for kc, (b, ap) in enumerate(xstream()):
                      STT(hT[:, kc, :], ap, gains[:, kc:kc + 1], rstd[:], ALU.mult, ALU.mult, [b, gains, rstd], [hT])

                  ck('p0')
                  ck('A')
                  switch(rA, d_tmps)
                  Wb, wv = wget()
                  zT(PS[0], Wb, wv[0], 0, 8, hrhs, [hT])
                  zT(PS[1], Wb, wv[1], 0, 8, hrhs, [hT])
                  li, sp_, Bc, U, Mx, em, ra, rb, rw, rt = rA
                  ACT(li[:], PS[0][0:8, 0:T], AF.Identity, [PS[0], bi], [li], bias=bi[:, 0:1])
                  ACT(rt[:], PS[1][0:8, 0:T], AF.Exp, [PS[1], bfn], [rt], bias=bfn[:, 0:1], scale=-1.0)
                  ACT(sp_[:], rt[:], AF.Ln, [rt], [sp_], bias=1.0)

                  def scan(src, tmp, op):
                      cur, nxt = src, tmp
                      sh = 1
                      while sh < T:
                          TT(nxt[:, sh:T], cur[:, sh:T], cur[:, 0:T - sh], op, [cur], [nxt])
                          CP(nxt[:, 0:sh], cur[:, 0:sh], [cur], [nxt])
                          cur, nxt = nxt, cur
                          sh *= 2
                      return cur

                  cs = scan(sp_, rt, ALU.add)
                  TS(Bc[:], cs[:], -1.0, cB[:, 0:1], ALU.mult, ALU.add, [cs, cB], [Bc])
                  TT(U[:], li[:], Bc[:], ALU.subtract, [li, Bc], [U])
                  other = rt if cs is sp_ else sp_
                  CP(other[:], U[:], [U], [other])
                  other2 = sp_ if other is rt else rt
                  cm = scan(other, other2, ALU.max)
                  TS(Mx[:], cm[:], cM[:, 0:1], None, ALU.max, None, [cm, cM], [Mx])
                  TT(em[:], Bc[:], Mx[:], ALU.add, [Bc, Mx], [em])
                  ACT(em[:], em[:], AF.Exp, [em], [em], scale=-1.0)
                  Mx3 = Mx[:].rearrange("h (c s) -> h c s", s=64)
                  CP(mus[:], Mx3[:, :, 63], [Mx], [mus])
                  CP(mup[:, 0:1], cM[:], [cM], [mup])
                  if NCH > 1:
                      CP(mup[:, 1:NCH], mus[:, 0:NCH - 1], [mus], [mup])
                  CP(cM[:], mus[:, NCH - 1:NCH], [mus], [cM])
                  CP(cB[:], Bc[:, T - 1:T], [Bc], [cB])
                  musb = mus[:].unsqueeze(2).to_broadcast([8, NCH, 64])
                  mupb = mup[:].unsqueeze(2).to_broadcast([8, NCH, 64])
                  v3 = lambda b: b[:].rearrange("h (c s) -> h c s", s=64)
                  TT(v3(ra), v3(U), musb, ALU.subtract, [U, mus], [ra])
                  ACT(ra[:], ra[:], AF.Exp, [ra], [ra])
                  TT(v3(rb), musb, Mx3, ALU.subtract, [Mx, mus], [rb])
                  ACT(rb[:], rb[:], AF.Exp, [rb], [rb])
                  TT(v3(rw), mupb, Mx3, ALU.subtract, [Mx, mup], [rw])
                  ACT(rw[:], rw[:], AF.Exp, [rw], [rw])
                  TT(dec[:], mup[:], mus[:], ALU.subtract, [mup, mus], [dec])
                  ACT(dec[:], dec[:], AF.Exp, [dec], [dec])
                  TT(dexp[:], dec[:].unsqueeze(2).to_broadcast([8, NCH, 8]),
                     ident[0:8, 0:8].unsqueeze(1).to_broadcast([8, NCH, 8]), ALU.mult, [dec, ident], [dexp])
                  MM(PS[2][:, 0:NCH * 8], ones32[0:8, :], dexp[:].rearrange("h c g -> h (c g)"), True, True,
                     [ones32, dexp], [PS[2]])
                  CP(dbc[:].rearrange("p c g -> p (c g)"), PS[2][:, 0:NCH * 8], [PS[2]], [dbc])
                  for c in range(NCH):
                      for qi, rq in enumerate((ra, rb, rw, em)):
                          o0 = (c * 4 + qi) * 8
                          TR(PS[3][0:64, o0:o0 + 8], rq[:, c * 64:(c + 1) * 64], ident[0:8, 0:8], [rq, ident], [PS[3]])
                  CP(cols[:].rearrange("p c q h -> p (c q h)"), PS[3][0:64, 0:NCH * 32], [PS[3]], [cols])

                  ck('B1')
                  switch([qT, kT, vT, gateT], [big, xrow, xcol, posi, posf])
                  for dst, scale in ((qT, 128.0 ** -0.5), (kT, None), (vT, None)):
                      for hp in range(4):
                          Wb, wv = wget()
                          for hh in range(2):
                              pb = PS[4 + (hp % 2) * 2 + hh]
                              zT(pb, Wb, wv[0], hh * 128, 128, hrhs, [hT])
                              if scale is not None:
                                  ACT(dst[:, hp * 2 + hh, :], pb[:, 0:T], AF.Copy, [pb], [dst], scale=scale)
                              elif hh % 2 == 0:
                                  ACT(dst[:, hp * 2 + hh, :], pb[:, 0:T], AF.Copy, [pb], [dst])
                              else:
                                  CP(dst[:, hp * 2 + hh, :], pb[:, 0:T], [pb], [dst])
                  for hp in range(4):
                      Wb, wv = wget()
                      Wb2, wv2 = wget()
                      for hh in range(2):
                          h = hp * 2 + hh
                          zT(PS[4 + hh], Wb, wv[0], hh * 128, 128, hrhs, [hT])
                          zT(PS[6 + hh], Wb2, wv2[0], hh * 128, 128, hrhs, [hT])
                          ACT(tA[:], PS[4 + hh][:, 0:T], AF.Sigmoid, [PS[4 + hh]], [tA])
                          ACT(tB[:], PS[6 + hh][:, 0:T], AF.Silu, [PS[6 + hh]], [tB])
                          TT(gateT[:, h, :], tA[:], tB[:], ALU.mult, [tA, tB], [gateT])

                  ck('B2')
                  switch(chunk_tmps, rA)

                  def chunk_gen():
                      for c in range(NCH):
                          cs_ = slice(c * 64, (c + 1) * 64)
                          a_col = cols[:, c, 0, :]
                          b_col = cols[:, c, 1, :]
                          w_col = cols[:, c, 2, :]
                          e_col = cols[:, c, 3, :]
                          bc3 = lambda ap, n: ap.unsqueeze(2).to_broadcast([64, 8, n])
                          pkt = PS[0][:].bitcast(BF16)
                          pvt = PS[1][:].bitcast(BF16)
                          for h in range(8):
                              TR(pkt[0:64, h * 128:(h + 1) * 128], kT[:, h, cs_], identb[:], [kT, identb], [PS[0]])
                          for h in range(8):
                              TR(pvt[0:64, h * 128:(h + 1) * 128], vT[:, h, cs_], identb[:], [vT, identb], [PS[1]])
                          ACT(k_tm[:], pkt[0:64, :], AF.Copy, [PS[0]], [k_tm])
                          CP(v_tm[:], pvt[0:64, :], [PS[1]], [v_tm])
                          TT(av_tm[:].rearrange("p (h d) -> p h d", d=128), pvt[0:64, :].rearrange("p (h d) -> p h d", d=128),
                             bc3(a_col, 128), ALU.mult, [PS[1], cols], [av_tm])
                          CP(abf[:], a_col, [cols], [abf])
                          yield
                          for h in range(8):
                              MM(PS[2][0:64, h * 64:(h + 1) * 64], kT[:, h, cs_], qT[:, h, cs_], True, True, [kT, qT], [PS[2]])
                          TT(tmpS[:].rearrange("p (h l) -> p h l", l=64), PS[2][0:64, :].rearrange("p (h l) -> p h l", l=64),
                             bc3(a_col, 64), ALU.mult, [PS[2], cols], [tmpS])
                          TT(AT[:].rearrange("p (h l) -> p h l", l=64), tmpS[:].rearrange("p (h l) -> p h l", l=64),
                             causal[:].unsqueeze(1).to_broadcast([64, 8, 64]), ALU.mult, [tmpS, causal], [AT])
                          yield
                          for h in range(8):
                              pb = PS[3 + h // 4]
                              MM(pb[0:64, (h % 4) * 128:(h % 4 + 1) * 128], AT[:, h * 64:(h + 1) * 64], v_tm[:, h * 128:(h + 1) * 128],
                                 True, True, [AT, v_tm], [pb])
                          for h in range(8):
                              MM(PS[0][0:64, h:h + 1], AT[:, h * 64:(h + 1) * 64], onesb[0:64, 0:1], True, True, [AT, onesb], [PS[0]])
                          for h in range(8):
                              pb = PS[5] if h < 4 else PS[2]
                              MM(pb[0:64, (h % 4) * 128:(h % 4 + 1) * 128], qT[:, h, cs_], Ctb[:, h * 128:(h + 1) * 128],
                                 True, True, [qT, Ctb], [pb])
                          for h in range(8):
                              MM(PS[0][0:64, 8 + h:9 + h], qT[:, h, cs_], nstb[:, h:h + 1], True, True, [qT, nstb], [PS[0]])
                          for hf in range(2):
                              sl = slice(hf * 512, (hf + 1) * 512)
                              TT(t1[:, sl].rearrange("p (h d) -> p h d", d=128), PS[3 + hf][0:64, :].rearrange("p (h d) -> p h d", d=128),
                                 bc3(b_col, 128)[:, hf * 4:(hf + 1) * 4, :], ALU.mult, [PS[3 + hf], cols], [t1])
                              TT(t2[:, sl].rearrange("p (h d) -> p h d", d=128), (PS[5] if hf == 0 else PS[2])[0:64, :].rearrange("p (h d) -> p h d", d=128),
                                 bc3(w_col, 128)[:, hf * 4:(hf + 1) * 4, :], ALU.mult, [PS[5] if hf == 0 else PS[2], cols], [t2])
                          TT(t1[:], t1[:], t2[:], ALU.add, [t1, t2], [t1])
                          TT(sm[0][:], PS[0][0:64, 0:8], b_col, ALU.mult, [PS[0], cols], [sm[0]])
                          TT(sm[1][:], PS[0][0:64, 8:16], w_col, ALU.mult, [PS[0], cols], [sm[1]])
                          TT(sm[0][:], sm[0][:], sm[1][:], ALU.add, [sm[0], sm[1]], [sm[0]])
                          TS(sm[5][:], sm[0][:], -1.0, None, ALU.mult, None, [sm[0]], [sm[5]])
                          TT(sm[0][:], sm[0][:], sm[5][:], ALU.max, [sm[0], sm[5]], [sm[0]])
                          TT(sm[0][:], sm[0][:], e_col, ALU.max, [sm[0], cols], [sm[0]])
                          RECIP(sm[2][:], sm[0][:], [sm[0]], [sm[2]])
                          TT(t1[:].rearrange("p (h d) -> p h d", d=128), t1[:].rearrange("p (h d) -> p h d", d=128),
                             bc3(sm[2][:], 128), ALU.mult, [t1, sm[2]], [t1])
                          TT(t2[:], t1[:], t1[:], ALU.mult, [t1], [t2])
                          k.op("dve", lambda: E["dve"].tensor_reduce(out=sm[3][:], in_=t2[:].rearrange("p (h d) -> p h d", d=128),
                                                                     axis=AX.X, op=ALU.add), [t2], [sm[3]])
                          TS(sm[3][:], sm[3][:], 1.0 / 128, EPS, ALU.mult, ALU.add, [sm[3]], [sm[3]])
                          ACT(sm[3][:], sm[3][:], AF.Sqrt, [sm[3]], [sm[3]])
                          RECIP(sm[4][:], sm[3][:], [sm[3]], [sm[4]])
                          TT(t1[:].rearrange("p (h d) -> p h d", d=128), t1[:].rearrange("p (h d) -> p h d", d=128),
                             bc3(sm[4][:], 128), ALU.mult, [t1, sm[4]], [t1])
                          TT(t1[:], t1[:], mnb[:], ALU.mult, [t1, mnb], [t1])
                          yield
                          for h in range(8):
                              TR(PS[2][:, h * 64:(h + 1) * 64], t1[:, h * 128:(h + 1) * 128], ident[0:64, 0:64], [t1, ident], [PS[2]])
                          TT(ymT[:, :, cs_], PS[2][:].rearrange("p (h l) -> p h l", l=64), gateT[:, :, cs_], ALU.mult,
                             [PS[2], gateT], [ymT])
                          yield
                          for h in range(8):
                              pb = PS[h // 4]
                              MM(pb[:, (h % 4) * 128:(h % 4 + 1) * 128], k_tm[:, h * 128:(h + 1) * 128], av_tm[:, h * 128:(h + 1) * 128],
                                 True, True, [k_tm, av_tm], [pb])
                          for h in range(8):
                              MM(PS[2][:, h:h + 1], k_tm[:, h * 128:(h + 1) * 128], abf[:, h:h + 1], True, True, [k_tm, abf], [PS[2]])
                          dcol = dbc[:, c, :]
                          TT(Ct[:].rearrange("p (h d) -> p h d", d=128), Ct[:].rearrange("p (h d) -> p h d", d=128),
                             dcol.unsqueeze(2).to_broadcast([128, 8, 128]), ALU.mult, [Ct, dbc], [Ct])
                          for hf in range(2):
                              sl = slice(hf * 512, (hf + 1) * 512)
                              TT(Ct[:, sl], Ct[:, sl], PS[hf][:, :], ALU.add, [Ct, PS[hf]], [Ct])
                          ACT(Ctb[:], Ct[:], AF.Copy, [Ct], [Ctb])
                          TT(nst[:], nst[:], dcol, ALU.mult, [nst, dbc], [nst])
                          TT(nst[:], nst[:], PS[2][:, 0:8], ALU.add, [nst, PS[2]], [nst])
                          CP(nstb[:], nst[:], [nst], [nstb])
                          yield


                  bg = chunk_gen()

                  def tick(n=1):
                      for _ in range(n):
                          next(bg, None)

                  for c in range(8):
                      Wb, wv = wget()
                      Wb2, wv2 = wget()
                      zT(PS[6], Wb, wv[1], 0, 128, hrhs, [hT])
                      tick()
                      zT(PS[7], Wb2, wv2[0], 0, 128, hrhs, [hT])
                      tick()
                      CP(vb[:, 0:2], cv[:, c, :], [cv], [vb])
                      ACT(tA[:], PS[6][:, 0:T], AF.Copy, [PS[6]], [tA])
                      TT(vb[:, 2:2 + T], tA[:], PS[7][:, 0:T], ALU.mult, [tA, PS[7]], [vb])
                      CP(cv[:, c, :], vb[:, T:T + 2], [vb], [cv])
                      zT(PS[6], Wb, wv[0], 0, 128, hrhs, [hT])
                      tick()
                      zT(PS[7], Wb2, wv2[1], 0, 128, hrhs, [hT])
                      tick()
                      TS(tB[:], vb[:, 0:T], convw[:, c * 3:c * 3 + 1], None, ALU.mult, None, [vb, convw], [tB])
                      STT(tB[:], vb[:, 1:T + 1], convw[:, c * 3 + 1:c * 3 + 2], tB[:], ALU.mult, ALU.add, [vb, convw, tB], [tB])
                      STT(tB[:], vb[:, 2:T + 2], convw[:, c * 3 + 2:c * 3 + 3], tB[:], ALU.mult, ALU.add, [vb, convw, tB], [tB])
                      ACT(tC[:], PS[7][:, 0:T], AF.Silu, [PS[7]], [tC])
                      TT(tB[:], tB[:], PS[6][:, 0:T], ALU.mult, [tB, PS[6]], [tB])
                      TT(ycT[:, c, :], tB[:], tC[:], ALU.mult, [tB, tC], [ycT])
                  for _ in bg:
                      pass

                  ck('B3')
                  switch(attn_tmps, chunk_tmps)
                  for d in rope_deps:
                      k._wait("sp", d)
                  k.dma("sp", rtab[:], rope_d[:, :, t0:t0 + T].rearrange("f p t -> p f t"), rtab, writes=[rtab])

                  def rope(psb, cosi, sini, dst):
                      ACT(tA[:], psb[:, 0:T], AF.Copy, [psb], [tA])
                      MM(PS[7][:, 0:T], rm32[:], tA[:], True, True, [rm32, tA], [PS[7]])
                      TT(tB[:], tA[:], rtab[:, cosi, :], ALU.mult, [tA, rtab], [tB])
                      TT(tC[:], PS[7][:, 0:T], rtab[:, sini, :], ALU.mult, [PS[7], rtab], [tC])
                      TT(dst, tB[:], tC[:], ALU.add, [tB, tC], [dst_b[0]])

                  Wb, wv = wget()
                  Wv_, wvv = wget()
                  dst_b = [kr]
                  for g2 in range(2):
                      zT(PS[0], Wb, wv[0], g2 * 128, 128, hrhs, [hT])
                      rope(PS[0], 2, 3, kr[:])
                      for s in range(2):
                          MM(PS[1][:, 0:T], selb[:, s * 128:(s + 1) * 128], kr[:], True, True, [selb, kr], [PS[1]])
                          CP(KTd[0:64, 0, g2 * 2 + s, 128:128 + T], PS[1][0:64, 0:T], [PS[1]], [KTd])
                          ACT(KTd[64:128, 1, g2 * 2 + s, 128:128 + T], PS[1][64:128, 0:T], AF.Copy, [PS[1]], [KTd])
                  for qb in range(NQB):
                      for kc in range(KC):
                          MM(PS[2][:, 0:256], hT[:, kc, qb * 128:(qb + 1) * 128], wvv[0][:, kc, :], kc == 0, kc == KC - 1,
                             [hT, Wv_], [PS[2]])
                      pv = PS[2][:, 0:256].rearrange("p (g d) -> p g d", d=64)
                      CP(Vp[:, 1 + qb, :, 0, 0:64], pv, [PS[2]], [Vp])
                      ACT(Vp[:, 1 + qb, :, 1, 64:128], pv, AF.Copy, [PS[2]], [Vp])
                  dst_b = [qr]
                  for j2 in range(4):
                      Wb, wv = wget()
                      Wb2, wv2 = wget()
                      for jj in range(2):
                          j = j2 * 2 + jj
                          g = j // 2
                          zT(PS[0], Wb, wv[0], jj * 128, 128, hrhs, [hT])
                          rope(PS[0], 0, 1, qr[:])
                          zT(PS[1], Wb2, wv2[0], jj * 128, 128, hrhs, [hT])
                          ACT(sgT[:], PS[1][:, 0:T], AF.Silu, [PS[1]], [sgT])
                          def emit_scores(qb):
                              psb = PS[2] if qb % 2 == 0 else PS[5]
                              for hf in range(2):
                                  for kbi in range(2):
                                      o0 = (hf * 2 + kbi) * 128
                                      MM(psb[:, o0:o0 + 128], KTd[:, hf, g, (qb + kbi) * 128:(qb + kbi + 1) * 128],
                                         qr[:, qb * 128:(qb + 1) * 128], True, True, [KTd, qr], [psb])

                          emit_scores(0)
                          for qb in range(NQB):
                              if qb + 1 < NQB:
                                  emit_scores(qb + 1)
                              psb = PS[2] if qb % 2 == 0 else PS[5]
                              ex_, PT_ = exs[qb % 2], PTs[qb % 2]
                              ACT(ex_[:], psb[:, :], AF.Exp, [psb], [ex_])
                              mk = amask0 if (it == 0 and qb == 0) else amask
                              TT(PT_[:].rearrange("p (h r) -> p h r", r=256), ex_[:].rearrange("p (h r) -> p h r", r=256),
                                 mk[:].unsqueeze(1).to_broadcast([128, 2, 256]), ALU.mult, [ex_, mk], [PT_])
                              n = 0
                              for hf in range(2):
                                  for kbi in range(2):
                                      o0 = (hf * 2 + kbi) * 128
                                      MM(PS[3][:, 0:128], Vp[:, qb + kbi, g, hf, :], PT_[:, o0:o0 + 128], n == 0, n == 3, [Vp, PT_], [PS[3]])
                                      n += 1
                              n = 0
                              for hf in range(2):
                                  for kbi in range(2):
                                      o0 = (hf * 2 + kbi) * 128
                                      MM(PS[4][:, 0:128], onespad[:, hf * 128:(hf + 1) * 128], PT_[:, o0:o0 + 128], n == 0, n == 3,
                                         [onespad, PT_], [PS[4]])
                                      n += 1
                              TS(tD[:, 0:128], PS[4][:, 0:128], sinke[:, j:j + 1], None, ALU.add, None, [PS[4], sinke], [tD])
                              RECIP(tD[:, 0:128], tD[:, 0:128], [tD], [tD])
                              TT(tD[:, 0:128], tD[:, 0:128], PS[3][:, 0:128], ALU.mult, [tD, PS[3]], [tD])
                              TT(yaT[:, j, qb * 128:(qb + 1) * 128], tD[:, 0:128], sgT[:, qb * 128:(qb + 1) * 128], ALU.mult, [tD, sgT], [yaT])
                  CP(KTd[:, :, :, 0:128], KTd[:, :, :, T:T + 128], [KTd], [KTd])
                  CP(Vp[:, 0, :, :, :], Vp[:, NQB, :, :, :], [Vp], [Vp])

                  ck('C')
                  ys = (ycT, ymT, yaT)
                  switch(d_tmps, attn_tmps)
                  for fb in range(16):
                      Wg, wg = wget()
                      Wg2, wg2 = wget()
                      zT(PS[0], Wg, wg[0], 0, 128, hrhs, [hT])
                      zT(PS[1], Wg, wg[1], 0, 128, hrhs, [hT])
                      zT(PS[2], Wg2, wg2[0], 0, 128, hrhs, [hT])
                      for j in range(3):
                          Wu, wu = wget()
                          zT(PS[3 + j], Wu, wu[0], 0, 128, lambda kc, j=j: ys[j][:, kc, :], [ys[j]], kcs=8)
                      for j in range(3):
                          ACT(tA[:], PS[j][:, 0:T], AF.Sigmoid, [PS[j]], [tA])
                          if j == 0:
                              TT(tB[:], tA[:], PS[3][:, 0:T], ALU.mult, [tA, PS[3]], [tB])
                          else:
                              TT(tC[:], tA[:], PS[3 + j][:, 0:T], ALU.mult, [tA, PS[3 + j]], [tC])
                              if j == 1:
                                  TT(tB[:], tB[:], tC[:], ALU.add, [tB, tC], [tB])
                              else:
                                  TT(mgT[:, fb, :], tB[:], tC[:], ALU.add, [tB, tC], [mgT])
                  mrhs = lambda kc: mgT[:, kc, :]
                  switch([big], [qT, kT, vT, gateT])
                  for q8 in range(8):
                      Wb, wv = wget()
                      for i in range(2):
                          fb = q8 * 2 + i
                          pb = PS[(q8 % 2) * 2 + i]
                          zT(pb, Wb, wv[0], i * 128, 128, mrhs, [mgT])
                          if i % 2 == 0:
                              ACT(big[:, fb, :], pb[:, 0:T], AF.Copy, [pb], [big])
                          else:
                              CP(big[:, fb, :], pb[:, 0:T], [pb], [big])
                  norm_stats(lambda kc: (big, big[:, kc, :]))
                  for kc, (b, ap) in enumerate(xstream()):
                      STT(tB[:], big[:, kc, :], gains[:, 16 + kc:17 + kc], rstd[:], ALU.mult, ALU.mult, [big, gains, rstd], [tB])
                      ob = xo[kc % 2]
                      TT(ob[:], tB[:], ap, ALU.add, [tB, b], [ob])
                      CP(mgT[:, kc, :], ob[:], [ob], [mgT])
                      k.dma("act", xT_d[kc, :, t0:t0 + T], ob[:], ob, reads=[ob], writes=[treg] if kc == 0 else [])
                  st_deps = [(xo[0].sem, xo[0].cnt), (xo[1].sem, xo[1].cnt)]
                  for qb in range(NQB):
                      k.dma("sp", p32[:], p_in[l, t0 + qb * 128:t0 + (qb + 1) * 128, :], p32, writes=[p32])
                      CP(pbf[:], p32[:], [p32], [pbf])
                      ppt = PS[4][:].bitcast(BF16)
                      for c2 in range(2):
                          TR(ppt[:, c2 * 128:(c2 + 1) * 128], pbf[:, c2 * 128:(c2 + 1) * 128], identb[:], [pbf, identb], [PS[4]])
                      CP(pT[:, :, qb * 128:(qb + 1) * 128], ppt[:, 0:256].rearrange("p (c t) -> p c t", t=128), [PS[4]], [pT])
                  for q8 in range(8):
                      Wb, wv = wget()
                      Wp, wp = wget()
                      for i in range(2):
                          fb = q8 * 2 + i
                          pa = PS[(q8 % 2) * 2 + i]
                          pe_ = PS[4 + (q8 % 2) * 2 + i]
                          zT(pa, Wb, wv[0], i * 128, 128, mrhs, [mgT])
                          zT(pe_, Wp, wp[0], i * 128, 128, lambda kc: pT[:, kc, :], [pT], kcs=2)
                          ACT(tA[:], pa[:, 0:T], AF.Sigmoid, [pa], [tA])
                          TT(big[:, fb, :], tA[:], pe_[:, 0:T], ALU.mult, [tA, pe_], [big])
                  norm_stats(lambda kc: (big, big[:, kc, :]))
                  for d in st_deps:
                      k._wait("sp", d)
                  last = (l == DEPTH - 1)
                  for kc, (b, ap) in enumerate(xstream()):
                      STT(tB[:], big[:, kc, :], gains[:, 32 + kc:33 + kc], rstd[:], ALU.mult, ALU.mult, [big, gains, rstd], [tB])
                      ob = xo[kc % 2]
                      TT(ob[:], tB[:], ap, ALU.add, [tB, b], [ob])
                      k.dma("act", xT_d[kc, :, t0:t0 + T], ob[:], ob, reads=[ob], writes=[treg] if kc == 0 else [])
                  fin = [(xo[0].sem, xo[0].cnt), (xo[1].sem, xo[1].cnt)]
                  treg.w = None
                  for d in fin:
                      k._wait("sp", d)

        except _Stop:
            pass
        switch([xcol, xrow], [big, qT, kT, vT, gateT])
        for blk in range(NTOK // 128):
            k.dma("sp", xcol[:], xT_d[:, :, blk * 128:(blk + 1) * 128].rearrange("k p t -> p k t"), xcol, writes=[xcol])
            for g4 in range(4):
                pb = PS[g4 % 2]
                for i in range(4):
                    kc = g4 * 4 + i
                    TR(pb[:, i * 128:(i + 1) * 128], xcol[:, kc, :], ident[:], [xcol, ident], [pb])
                CP(xrow[:, g4 * 512:(g4 + 1) * 512], pb[:, :], [pb], [xrow])
            k.dma("sp", y_out[blk * 128:(blk + 1) * 128, :], xrow[:], xrow, reads=[xrow])
        k._wait("sp", (xrow.sem, xrow.cnt))
        for e in ("pe", "act", "dve"):
            if k.sems[e]:
                c = k.cnt[e]
                ep = (c - 1) // EPOCH
                k._wait("sp", (k.sems[e][ep], c - ep * EPOCH))
    return nc


def host_consts():
    ident = np.eye(128, dtype=np.float32)
    rm = np.zeros((128, 128), np.float32)
    for m in range(128):
        rm[(m // 64) * 64 + ((m % 64) + 32) % 64, m] = 1.0
    sel = np.zeros((128, 256), np.float32)
    for s in range(2):
        for m in range(128):
            sel[s * 64 + (m % 64), s * 128 + m] = 1.0
    vec = np.zeros((128, 4), np.float32)
    j = np.arange(128) % 32
    vec[:, 0] = np.power(np.float32(10000.0), (-2.0 * j.astype(np.float32) / 64).astype(np.float32)).astype(np.float32)
    vec[:, 1] = np.where((np.arange(128) % 64) < 32, -1.0, 1.0)
    causal = (np.arange(64)[:, None] <= np.arange(64)[None, :]).astype(np.float32)
    kk = np.arange(128)[:, None]
    qq = np.arange(128)[None, :]
    amask = np.concatenate([(kk > qq), (kk <= qq)], axis=1).astype(np.float32)
    onespad = np.zeros((128, 256), np.float32)
    onespad[:, 0:64] = 1.0
    onespad[:, 128 + 64:256] = 1.0
    return {"c_ident": ident, "c_rm": rm, "c_sel": sel, "c_vec": vec, "c_causal": causal,
            "c_amask": amask, "c_onespad": onespad}


def layout_inputs(b, x, p, positions, w_in, conv_w, b_igate, b_fgate, mlstm_norm, attn_sinks,
                  w_up_conv, w_up_mlstm, w_up_attn, w_out, pre_norm, post_norm, w_ple,
                  w_ple_gate, ple_norm, consts):
    L = w_in.shape[0]
    f = lambda a: np.ascontiguousarray(np.asarray(a, dtype=np.float32))
    convw = f(np.asarray(conv_w).reshape(L, 3, 8, 128).transpose(0, 3, 2, 1).reshape(L, 128, 24))
    g = lambda a: np.asarray(a).reshape(L, 16, 128).transpose(0, 2, 1)
    gains = f(np.concatenate([g(pre_norm), g(post_norm), g(ple_norm)], axis=2))
    sk = np.asarray(attn_sinks).reshape(L, 8, 2)
    sinks = f(np.repeat(sk.transpose(0, 2, 1), 64, axis=1))
    m = {"x": f(x[b]), "p": f(np.asarray(p)[:, b]), "pos": np.ascontiguousarray(np.asarray(positions)[b].astype(np.int32)),
         "w_in": f(w_in), "w_up_conv": f(w_up_conv), "w_up_mlstm": f(w_up_mlstm), "w_up_attn": f(w_up_attn),
         "w_out": f(w_out), "w_ple": f(w_ple), "w_ple_gate": f(w_ple_gate), "convw": convw, "gains": gains,
         "b_igate": f(b_igate), "b_fgate": f(b_fgate), "mlstm_norm": f(mlstm_norm), "sinks": sinks}
    m.update(consts)
    return m


def kernel(**inputs):
    x = np.asarray(inputs["x"])
    B, S, _ = x.shape
    L = np.asarray(inputs["w_in"]).shape[0]
    nc = build(S, L, T=512)
    consts = host_consts()
    args = [inputs[n] for n in ("x", "p", "positions", "w_in", "conv_w", "b_igate", "b_fgate", "mlstm_norm",
                                "attn_sinks", "w_up_conv", "w_up_mlstm", "w_up_attn", "w_out", "pre_norm",
                                "post_norm", "w_ple", "w_ple_gate", "ple_norm")]
    maps = [layout_inputs(c % B, *args, consts) for c in range(B)]
    in_maps = [maps[c % B] for c in range(8)]
    res = run_bass_kernel_spmd(nc, in_maps, core_ids=list(range(8)))
    return np.stack([np.asarray(res.results[b]["y"], dtype=np.float32) for b in range(B)], axis=0)
```
